# Optimizing a Trainium2 kernel written in Bass

```python
import jax, jax.numpy as jnp
from jax import lax
import numpy as np

D_MODEL = 1024
BATCH = 2
SEQ = 8192
DEPTH = 1

N_MEM = 256
EPS = 1e-6
MLSTM_HEADS = 4
MLSTM_HEAD_DIM = D_MODEL // MLSTM_HEADS
MLSTM_DIM = MLSTM_HEADS * MLSTM_HEAD_DIM
MLSTM_CHUNK = 128
CONV_WIDTH = 4
SGU_GROUPS = 8
SGU_GROUP_DIM = D_MODEL // SGU_GROUPS
SGU_DIM = SGU_GROUPS * SGU_GROUP_DIM
SGU_CHUNK = 128
N_BRANCH = 2
IN_WIDTHS = (2 * MLSTM_DIM, MLSTM_DIM, MLSTM_DIM, MLSTM_HEADS, MLSTM_HEADS, SGU_DIM, SGU_DIM, N_BRANCH * D_MODEL)
IN_DIM = sum(IN_WIDTHS)
SPLIT_POINTS = tuple(int(s) for s in np.cumsum(IN_WIDTHS)[:-1])
XATTN_HEADS = 4
XATTN_HEAD_DIM = D_MODEL // XATTN_HEADS
XATTN_DIM = XATTN_HEADS * XATTN_HEAD_DIM
MOE_GROUPS = 4
EXPERTS_PER_GROUP = 8
N_EXPERTS = MOE_GROUPS * EXPERTS_PER_GROUP
MOE_TOPK = 2
EXPERT_FF = D_MODEL // 2
MOE_BLOCK = 128

kernel_name = 'hybrid_mlstm_sgu_xattn_hmoe'


def rmsnorm(x, g):
    x32 = x.astype(jnp.float32)
    y = x32 * lax.rsqrt(jnp.mean(x32 * x32, axis=-1, keepdims=True) + EPS)
    return (y * g.astype(jnp.float32)).astype(x.dtype)


def causal_depthwise_conv(x, w, b):
    c = x.shape[-1]
    y = lax.conv_general_dilated(x, w[:, None, :].astype(x.dtype), window_strides=(1,),
                                 padding=[(CONV_WIDTH - 1, 0)],
                                 dimension_numbers=('NWC', 'WIO', 'NWC'),
                                 feature_group_count=c)
    return y + b.astype(x.dtype)


def mlstm_chunkwise(q, k, v, i_pre, f_pre):
    B, S, H, dh = q.shape
    L = MLSTM_CHUNK
    nc = S // L
    f32 = jnp.float32

    def to_chunks(a):
        return a.astype(f32).reshape(B, nc, L, H, dh).transpose(1, 0, 3, 2, 4)

    qc = to_chunks(q)
    kc = to_chunks(k) * (dh ** -0.5)
    vc = to_chunks(v)
    logf = jax.nn.log_sigmoid(f_pre.astype(f32)).reshape(B, nc, L, H).transpose(1, 0, 3, 2)
    ig = i_pre.astype(f32).reshape(B, nc, L, H).transpose(1, 0, 3, 2)
    causal = jnp.tril(jnp.ones((L, L), dtype=bool))

    def step(carry, inp):
        C, n, m = carry
        q_, k_, v_, lf, ic = inp
        b = jnp.cumsum(lf, axis=-1)
        dmat = jnp.where(causal, b[..., :, None] - b[..., None, :] + ic[..., None, :], -jnp.inf)
        inter = b + m[..., None]
        m_t = jnp.maximum(inter, jnp.max(dmat, axis=-1))
        dexp = jnp.exp(dmat - m_t[..., None])
        inter_w = jnp.exp(inter - m_t)
        s = jnp.einsum('bhtd,bhsd->bhts', q_, k_) * dexp
        num = jnp.einsum('bhts,bhsd->bhtd', s, v_) + inter_w[..., None] * jnp.einsum('bhtd,bhde->bhte', q_, C)
        den = jnp.sum(s, axis=-1) + inter_w * jnp.einsum('bhtd,bhd->bht', q_, n)
        h = num / jnp.maximum(jnp.abs(den), jnp.exp(-m_t))[..., None]
        b_last = b[..., -1]
        w_log = b_last[..., None] - b + ic
        m_new = jnp.maximum(b_last + m, jnp.max(w_log, axis=-1))
        decay = jnp.exp(b_last + m - m_new)
        ws = jnp.exp(w_log - m_new[..., None])
        C_new = decay[..., None, None] * C + jnp.einsum('bhs,bhsd,bhse->bhde', ws, k_, v_)
        n_new = decay[..., None] * n + jnp.einsum('bhs,bhsd->bhd', ws, k_)
        return (C_new, n_new, m_new), h

    init = (jnp.zeros((B, H, dh, dh), f32), jnp.zeros((B, H, dh), f32), jnp.zeros((B, H), f32))
    _, hs = lax.scan(step, init, (qc, kc, vc, logf, ig))
    return hs.transpose(1, 0, 3, 2, 4).reshape(B, S, H, dh).astype(q.dtype)


def spatial_gating(u, v, sgu_norm_g, w_s, b_s):
    B, S, _ = u.shape
    nc = S // SGU_CHUNK
    u = jax.nn.gelu(u)
    v = rmsnorm(jax.nn.gelu(v), sgu_norm_g)
    vb = v.reshape(B, nc, SGU_CHUNK, SGU_GROUPS, SGU_GROUP_DIM)
    w_causal = jnp.tril(w_s).astype(v.dtype)
    mixed = jnp.einsum('gts,bcsgd->bctgd', w_causal, vb) + b_s.T.astype(v.dtype)[:, :, None]
    return u * mixed.reshape(B, S, SGU_DIM)


def hybrid_mixer(xn, w_in, b_gate, b_if, conv_w, conv_b, mh_norm_g, sgu_norm_g, w_s, b_s, w_out):
    B, S, _ = xn.shape
    proj = xn @ w_in
    qk, v, o, i_pre, f_pre, u, sv, gates = jnp.split(proj, SPLIT_POINTS, axis=-1)
    qk = jax.nn.silu(causal_depthwise_conv(qk, conv_w, conv_b))
    q, k = jnp.split(qk, 2, axis=-1)
    b_i, b_f = jnp.split(b_if, 2)
    hd = (B, S, MLSTM_HEADS, MLSTM_HEAD_DIM)
    h = mlstm_chunkwise(q.reshape(hd), k.reshape(hd), v.reshape(hd), i_pre + b_i, f_pre + b_f)
    y_a = rmsnorm(h, mh_norm_g.reshape(MLSTM_HEADS, MLSTM_HEAD_DIM)).reshape(B, S, MLSTM_DIM) * jax.nn.sigmoid(o)
    y_b = spatial_gating(u, sv, sgu_norm_g, w_s, b_s)
    g_a, g_b = jnp.split(jax.nn.sigmoid(gates + b_gate), 2, axis=-1)
    return (g_a * y_a + g_b * y_b) @ w_out


def memory_cross_attention(xn, mn, w_xq, w_xkv, w_xo):
    B, S, _ = xn.shape
    M = mn.shape[1]
    q = (xn @ w_xq).reshape(B, S, XATTN_HEADS, XATTN_HEAD_DIM)
    k, v = jnp.split(mn @ w_xkv, 2, axis=-1)
    k = k.reshape(B, M, XATTN_HEADS, XATTN_HEAD_DIM)
    v = v.reshape(B, M, XATTN_HEADS, XATTN_HEAD_DIM)
    scores = jnp.einsum('bshd,bmhd->bhsm', q, k).astype(jnp.float32) * (XATTN_HEAD_DIM ** -0.5)
    p = jax.nn.softmax(scores, axis=-1).astype(v.dtype)
    out = jnp.einsum('bhsm,bmhd->bshd', p, v).reshape(B, S, XATTN_DIM)
    return out @ w_xo


def hierarchical_moe(xn, w_rg, b_rg, w_re, b_re, w1, w3, w2):
    B, S, D = xn.shape
    N = B * S
    xt = xn.reshape(N, D)
    g_logits = (xt @ w_rg + b_rg).astype(jnp.float32)
    g_idx = jnp.argmax(g_logits, axis=-1)
    p_g = jnp.take_along_axis(jax.nn.softmax(g_logits, axis=-1), g_idx[:, None], axis=1)
    e_logits = (xt @ w_re + b_re).astype(jnp.float32).reshape(N, MOE_GROUPS, EXPERTS_PER_GROUP)
    e_in_group = jnp.take_along_axis(e_logits, g_idx[:, None, None], axis=1)[:, 0]
    top_v, top_i = lax.top_k(e_in_group, MOE_TOPK)
    gate = p_g * jax.nn.softmax(top_v, axis=-1)
    expert = g_idx[:, None].astype(jnp.int32) * EXPERTS_PER_GROUP + top_i.astype(jnp.int32)

    A = N * MOE_TOPK
    e_flat = expert.reshape(A)
    tok_flat = jnp.repeat(jnp.arange(N, dtype=jnp.int32), MOE_TOPK)
    order = jnp.argsort(e_flat)
    e_sorted = e_flat[order]
    counts = jnp.bincount(e_flat, length=N_EXPERTS)
    starts = jnp.cumsum(counts) - counts
    padded = (counts + MOE_BLOCK - 1) // MOE_BLOCK * MOE_BLOCK
    pends = jnp.cumsum(padded)
    pstarts = pends - padded
    dest_sorted = (pstarts[e_sorted] + jnp.arange(A, dtype=jnp.int32) - starts[e_sorted]).astype(jnp.int32)
    n_blocks = -(-A // MOE_BLOCK) + N_EXPERTS
    P = n_blocks * MOE_BLOCK
    buf_tok = jnp.zeros((P,), jnp.int32).at[dest_sorted].set(tok_flat[order])
    block_expert = jnp.minimum(
        jnp.searchsorted(pends, jnp.arange(n_blocks, dtype=jnp.int32) * MOE_BLOCK, side='right'),
        N_EXPERTS - 1).astype(jnp.int32)

    def block_ffn(args):
        tok, e = args
        xb = xt[tok]
        hb = jax.nn.silu(xb @ w1[e]) * (xb @ w3[e])
        return hb @ w2[e]

    out_buf = lax.map(block_ffn, (buf_tok.reshape(n_blocks, MOE_BLOCK), block_expert)).reshape(P, D)
    dest = jnp.zeros((A,), jnp.int32).at[order].set(dest_sorted)
    y_assign = out_buf[dest].reshape(N, MOE_TOPK, D)
    y = jnp.einsum('nk,nkd->nd', gate.astype(xt.dtype), y_assign)
    return y.reshape(B, S, D)


def setup_inputs(seed: int = 0) -> dict:
    key = jax.random.key(seed)
    ks = jax.random.split(key, 32)
    D, L = D_MODEL, DEPTH

    def nrm(k, shape, scale):
        return jax.random.normal(k, shape, jnp.float32) * scale

    def gain(k, shape):
        return 1.0 + 0.02 * jax.random.normal(k, shape, jnp.float32)

    b_i = nrm(ks[4], (L, MLSTM_HEADS), 0.1)
    b_f = jnp.linspace(3.0, 6.0, MLSTM_HEADS, dtype=jnp.float32)[None, :] + nrm(ks[5], (L, MLSTM_HEADS), 0.1)
    return {
        'x': nrm(ks[0], (BATCH, SEQ, D), 1.0),
        'mem': nrm(ks[1], (BATCH, N_MEM, D), 1.0),
        'norm_mix_g': gain(ks[2], (L, D)),
        'w_in': nrm(ks[3], (L, D, IN_DIM), D ** -0.5),
        'b_gate': nrm(ks[6], (L, N_BRANCH * D), 0.02),
        'b_if': jnp.concatenate([b_i, b_f], axis=-1),
        'conv_w': nrm(ks[7], (L, CONV_WIDTH, 2 * MLSTM_DIM), CONV_WIDTH ** -0.5),
        'conv_b': nrm(ks[8], (L, 2 * MLSTM_DIM), 0.02),
        'mh_norm_g': gain(ks[9], (L, MLSTM_DIM)),
        'sgu_norm_g': gain(ks[10], (L, SGU_DIM)),
        'w_s': nrm(ks[11], (L, SGU_GROUPS, SGU_CHUNK, SGU_CHUNK), SGU_CHUNK ** -0.5),
        'b_s': gain(ks[12], (L, SGU_GROUPS, SGU_CHUNK)),
        'w_out': nrm(ks[13], (L, D, D), D ** -0.5),
        'norm_x_g': gain(ks[14], (L, D)),
        'norm_mem_g': gain(ks[15], (L, D)),
        'w_xq': nrm(ks[16], (L, D, XATTN_DIM), D ** -0.5),
        'w_xkv': nrm(ks[17], (L, D, 2 * XATTN_DIM), D ** -0.5),
        'w_xo': nrm(ks[18], (L, XATTN_DIM, D), XATTN_DIM ** -0.5),
        'norm_moe_g': gain(ks[19], (L, D)),
        'w_rg': nrm(ks[20], (L, D, MOE_GROUPS), D ** -0.5),
        'b_rg': nrm(ks[21], (L, MOE_GROUPS), 0.01),
        'w_re': nrm(ks[22], (L, D, N_EXPERTS), D ** -0.5),
        'b_re': nrm(ks[23], (L, N_EXPERTS), 0.01),
        'w1': nrm(ks[24], (L, N_EXPERTS, D, EXPERT_FF), D ** -0.5),
        'w3': nrm(ks[25], (L, N_EXPERTS, D, EXPERT_FF), D ** -0.5),
        'w2': nrm(ks[26], (L, N_EXPERTS, EXPERT_FF, D), EXPERT_FF ** -0.5),
        'norm_f_g': gain(ks[27], (D,)),
    }


def reference(x, mem, norm_mix_g, w_in, b_gate, b_if, conv_w, conv_b, mh_norm_g, sgu_norm_g, w_s, b_s, w_out,
              norm_x_g, norm_mem_g, w_xq, w_xkv, w_xo, norm_moe_g, w_rg, b_rg, w_re, b_re, w1, w3, w2, norm_f_g):
    for l in range(DEPTH):
        x = x + hybrid_mixer(rmsnorm(x, norm_mix_g[l]), w_in[l], b_gate[l], b_if[l], conv_w[l], conv_b[l],
                             mh_norm_g[l], sgu_norm_g[l], w_s[l], b_s[l], w_out[l])
        x = x + memory_cross_attention(rmsnorm(x, norm_x_g[l]), rmsnorm(mem, norm_mem_g[l]),
                                       w_xq[l], w_xkv[l], w_xo[l])
        x = x + hierarchical_moe(rmsnorm(x, norm_moe_g[l]), w_rg[l], b_rg[l], w_re[l], b_re[l],
                                 w1[l], w3[l], w2[l])
    return rmsnorm(x, norm_f_g)
```

```python
import numpy as np
import concourse.bass as bass
import concourse.mybir as mybir
from concourse.bass_utils import run_bass_kernel_spmd
from contextlib import ExitStack

F32 = mybir.dt.float32
BF16 = mybir.dt.bfloat16
ALU = mybir.AluOpType
AF = mybir.ActivationFunctionType
AX = mybir.AxisListType

NDS = 24
NCORES = 8
D = 1024
NCH = 16
NPRE = 48
NIT = NCH + NPRE
IN_DIM = 8200
NE = 32
EPS = 1e-6
RELAXED_SAME_ENGINE = False


class Buf:
    __slots__ = ("w", "r")

    def __init__(self):
        self.w = None
        self.r = {}


class Q:
    def __init__(self, S, name, eng):
        self.eng = eng
        self.name = name
        self.sem = S.new_sem("q_" + name)
        self.count = 0
        self.waited = {}


class Sched:
    def __init__(self, nc, es):
        self.nc = nc
        self.es = es
        self.T = Q(self, "pe", nc.tensor)
        self.V = Q(self, "dve", nc.vector)
        self.A = Q(self, "act", nc.scalar)
        self.G = Q(self, "pool", nc.gpsimd)
        self.SP = Q(self, "sp", nc.sync)
        self.dma_sems = [self.new_sem("dma%d" % i) for i in range(NDS)]
        self.dma_uses = [0] * NDS
        self.dma_next = 0
        self.dma_next_g = 0
        self._wv = None
        self._force = None
        self.pools = []
        self.noswitch = False

    def weave(self, fns):
        import threading
        if len(fns) == 1:
            fns[0][0]()
            return
        n = len(fns)
        evs = [threading.Event() for _ in range(n)]
        alive = [True] * n
        errs = []
        done = threading.Event()
        st = {"cur": 0, "cnt": 0}
        quanta = [q for _, q in fns]

        def next_alive(k):
            for d in range(1, n + 1):
                j = (k + d) % n
                if alive[j] and j != k:
                    return j
            return None

        def runner(k, fn):
            evs[k].wait()
            try:
                fn()
            except BaseException as e:
                errs.append(e)
            for pl in self.pools:
                pl.release_mine()
            alive[k] = False
            j = next_alive(k)
            if j is None:
                done.set()
            else:
                st["cur"] = j
                st["cnt"] = 0
                evs[j].set()

        def switch():
            k = st["cur"]
            st["cnt"] += 1
            if st["cnt"] < quanta[k]:
                return
            j = next_alive(k)
            if j is None:
                st["cnt"] = 0
                return
            st["cur"] = j
            st["cnt"] = 0
            evs[k].clear()
            evs[j].set()
            evs[k].wait()

        def force():
            k = st["cur"]
            j = next_alive(k)
            if j is None:
                raise RuntimeError("weave: stream blocked on a PSUM bank with no other stream alive")
            st["cur"] = j
            st["cnt"] = 0
            evs[k].clear()
            evs[j].set()
            evs[k].wait()

        self._wv = switch
        self._force = force
        ths = [threading.Thread(target=runner, args=(k, fn)) for k, (fn, _) in enumerate(fns)]
        for t in ths:
            t.start()
        evs[0].set()
        done.wait()
        for t in ths:
            t.join()
        self._wv = None
        self._force = None
        for pl in self.pools:
            pl.free = list(range(len(pl.items)))
            pl.owned = {}
        if errs:
            raise errs[0]

    def yield_now(self):
        self._force()

    def _sw(self):
        if self._wv is not None and not self.noswitch:
            self._wv()

    def new_sem(self, name):
        return self.es.enter_context(self.nc.semaphore(name))

    def _wait(self, q, tok):
        sem, val = tok
        k = id(sem)
        if q.waited.get(k, 0) >= val:
            return
        q.eng.wait_ge(sem, val)
        q.waited[k] = val

    def _deps(self, q, reads, writes):
        best = {}

        def add(tok):
            k = id(tok[0])
            if k not in best or best[k][1] < tok[1]:
                best[k] = tok

        for b in reads:
            if b.w is not None:
                add(b.w)
        relaxed = RELAXED_SAME_ENGINE and q in (self.T, self.V, self.A)
        for b in writes:
            if b.w is not None and not (relaxed and b.w[0] is q.sem):
                add(b.w)
            for t in b.r.values():
                if not (relaxed and t[0] is q.sem):
                    add(t)
        for tok in best.values():
            if RELAXED_SAME_ENGINE and q is self.T and tok[0] is q.sem:
                continue
            self._wait(q, tok)

    def _commit(self, tok, reads, writes):
        for b in writes:
            b.w = tok
            b.r = {}
        k = id(tok[0])
        for b in reads:
            if b not in writes:
                b.r[k] = tok

    def op(self, q, fn, reads=(), writes=()):
        self._deps(q, reads, writes)
        ins = fn(q.eng)
        q.count += 1
        ins.then_inc(q.sem, 1)
        self._commit((q.sem, q.count), reads, writes)
        self._sw()

    def mm(self, groups, reads=(), writes=()):
        q = self.T
        self._deps(q, reads, writes)
        ins = None
        for out, pairs in groups:
            n = len(pairs)
            for i, (l, r) in enumerate(pairs):
                ins = q.eng.matmul(out, lhsT=l, rhs=r, start=(i == 0), stop=(i == n - 1))
        q.count += 1
        ins.then_inc(q.sem, 1)
        self._commit((q.sem, q.count), reads, writes)
        self._sw()

    def tr(self, items, ident, reads=(), writes=()):
        q = self.T
        self._deps(q, reads, writes)
        ins = None
        for out, in_ in items:
            ins = q.eng.transpose(out=out, in_=in_, identity=ident)
        q.count += 1
        ins.then_inc(q.sem, 1)
        self._commit((q.sem, q.count), reads, writes)
        self._sw()

    def dma(self, q, out, in_, reads=(), writes=()):
        self._deps(q, reads, writes)
        half = NDS // 2
        if q is self.G:
            i = half + self.dma_next_g
            self.dma_next_g = (self.dma_next_g + 1) % half
        else:
            i = self.dma_next
            self.dma_next = (self.dma_next + 1) % half
        sem = self.dma_sems[i]
        if self.dma_uses[i] > 0:
            self._wait(q, (sem, 16 * self.dma_uses[i]))
        q.eng.dma_start(out=out, in_=in_).then_inc(sem, 16)
        self.dma_uses[i] += 1
        tok = (sem, 16 * self.dma_uses[i])
        self._commit(tok, reads, writes)
        self._sw()

    def idma(self, q, reads=(), writes=(), **kw):
        self._deps(q, reads, writes)
        half = NDS // 2
        i = half + self.dma_next_g
        self.dma_next_g = (self.dma_next_g + 1) % half
        sem = self.dma_sems[i]
        if self.dma_uses[i] > 0:
            self._wait(q, (sem, 16 * self.dma_uses[i]))
        q.eng.indirect_dma_start(**kw).then_inc(sem, 16)
        self.dma_uses[i] += 1
        self._commit((sem, 16 * self.dma_uses[i]), reads, writes)
        self._sw()

    def barrier(self):
        qs = [self.T, self.V, self.A, self.G, self.SP]
        toks = [(q.sem, q.count) for q in qs if q.count > 0]
        toks += [(self.dma_sems[i], 16 * self.dma_uses[i]) for i in range(NDS) if self.dma_uses[i] > 0]
        for q in qs:
            for tok in toks:
                if tok[0] is q.sem:
                    continue
                self._wait(q, tok)

    def wait_all(self, q, bufs):
        for b in bufs:
            if b.w is not None:
                self._wait(q, b.w)


class Pool:
    def __init__(self, items, S=None, hold=1):
        self.items = items
        self.i = 0
        self.S = S
        self.hold = hold
        self.free = list(range(len(items)))
        self.owned = {}
        if S is not None:
            S.pools.append(self)

    def next(self):
        S = self.S
        if S is None or S._wv is None:
            it = self.items[self.i]
            self.i = (self.i + 1) % len(self.items)
            return it
        import threading
        tid = threading.get_ident()
        mine = self.owned.setdefault(tid, [])
        while len(mine) >= self.hold:
            self.free.append(mine.pop(0))
        while not self.free:
            S.yield_now()
        k = self.free.pop(0)
        mine.append(k)
        return self.items[k]

    def release_mine(self):
        import threading
        mine = self.owned.get(threading.get_ident(), [])
        while mine:
            self.free.append(mine.pop(0))


def build(stage=3, sseq_in=None, srec=None):
    if srec is None:
        srec = []
    nc = bass.Bass("TRN2", target_bir_lowering=False)

    def din(name, shape):
        return nc.dram_tensor(name, list(shape), F32, kind="ExternalInput").ap()

    xseq = din("xseq", [NIT * 128, D])
    pmask_d = din("pmask", [128, NIT])
    mem_d = din("mem_b", [256, D])
    w_in_d = din("w_in", [D, IN_DIM])
    w_out_d = din("w_out", [D, D])
    w_xq_d = din("w_xq", [D, D])
    w_xkv_d = din("w_xkv", [D, 2 * D])
    w_xo_d = din("w_xo", [D, D])
    w_r_d = din("w_r", [D, 36])
    w1_d = din("w1r", [NE * 2 * 128, 2048])
    w3_d = din("w3r", [NE * 2 * 128, 2048])
    w2_d = din("w2r", [NE * 2 * 128, 2048])
    thr_d = din("thr16", [128, 16])
    biota_d = din("biota", [128, 64])
    pidx_d = din("pidx", [128, 1])
    ident_d = din("ident", [128, 128])
    tri_d = din("tri", [128, 128])
    gmix_d = din("gmix_t", [128, D])
    gx_d = din("gx_t", [128, D])
    gmem_d = din("gmem_t", [128, D])
    gmoe_d = din("gmoe_t", [128, D])
    gf_d = din("gf_t", [128, D])
    mhg_d = din("mhg_t", [128, D])
    sgug_d = din("sgug_t", [128, D])
    bs_d = din("bs_t", [128, D])
    bgate_d = din("bgate_t", [128, 2 * D])
    bif_d = din("bif_t", [128, 8])
    br_d = din("br_t", [128, 36])
    convw_d = din("convw_t", [128, 16, 4])
    convb_d = din("convb_t", [128, 16])
    wsT_d = din("wsT", [128, 8, 128])
    y_d = nc.dram_tensor("y", [NCH * 128, D], F32, kind="ExternalOutput").ap()
    x1s = nc.dram_tensor("x1s", [NCH * 128, D], F32, kind="Internal").ap()
    wst = nc.dram_tensor("wst", [22, 128, 8, 512], BF16, kind="Internal").ap()
    BS = 256
    NT = BS // 128
    NBLK = (NCH * 128 * 2) // BS + NE
    xbuf = nc.dram_tensor("xbuf", [NBLK * BS, D], BF16, kind="Internal").ap()
    obuf = nc.dram_tensor("obuf", [NBLK * BS, D], F32, kind="Internal").ap()
    I32 = mybir.dt.int32

    es = ExitStack()
    with es:
        S = Sched(nc, es)
        T, V, A, G, SP = S.T, S.V, S.A, S.G, S.SP

        def sbt(stack, name, shape, dt):
            return stack.enter_context(nc.sbuf_tensor("s_" + name, list(shape), dt)), Buf()

        def pst(stack, name, shape, dt):
            return stack.enter_context(nc.psum_tensor(name, list(shape), dt)), Buf()

        identf, b_identf = sbt(es, "identf", [128, 128], F32)
        identb, b_identb = sbt(es, "identb", [128, 128], BF16)
        trif, b_trif = sbt(es, "trif", [128, 128], F32)
        trib, b_trib = sbt(es, "trib", [128, 128], BF16)
        onesf, b_onesf = sbt(es, "onesf", [128, 128], F32)
        S.dma(SP, identf[:], ident_d, writes=[b_identf])
        S.dma(SP, trif[:], tri_d, writes=[b_trif])
        S.op(V, lambda e: e.tensor_copy(out=identb[:], in_=identf[:]), reads=[b_identf], writes=[b_identb])
        S.op(V, lambda e: e.tensor_copy(out=trib[:], in_=trif[:]), reads=[b_trif], writes=[b_trib])
        S.op(V, lambda e: e.memset(onesf[:], 1.0), writes=[b_onesf])
        epsc, b_epsc = sbt(es, "epsc", [128, 1], F32)
        S.op(V, lambda e: e.memset(epsc[:], EPS), writes=[b_epsc])

        banks = []
        for i in range(8):
            t, b = pst(es, "bank%d" % i, [128, 512], F32)
            banks.append((t, b))

        def rms_rstd(q_sq, xin_ap, b_xin, junk, b_junk, st, b_st, n):
            S.op(A, lambda e: e.activation(out=junk, in_=xin_ap, func=AF.Square, accum_out=st[:, 0:1]),
                 reads=[b_xin], writes=[b_junk, b_st])
            S.op(A, lambda e: e.activation(out=st[:, 3:4], in_=st[:, 0:1], func=AF.Sqrt, scale=1.0 / n, bias=epsc[:, 0:1]),
                 reads=[b_st, b_epsc], writes=[b_st])
            S.op(V, lambda e: e.reciprocal(out=st[:, 2:3], in_=st[:, 3:4]), reads=[b_st], writes=[b_st])

        pa = ExitStack()
        with pa:
            w_in_v = w_in_d.rearrange("(k p) n -> p k n", p=128)
            Wk, b_wk = sbt(pa, "Wk", [128, 8, 1024], BF16)
            Wv, b_wv = sbt(pa, "Wv", [128, 8, 1024], BF16)
            Wif, b_wif = sbt(pa, "Wif", [128, 8, 8], BF16)
            S.dma(G, Wk[:], w_in_v[:, :, 1024:2048], writes=[b_wk])
            S.dma(G, Wv[:], w_in_v[:, :, 2048:3072], writes=[b_wv])
            S.dma(G, Wif[:], w_in_v[:, :, 4096:4104], writes=[b_wif])
            GCOLS = [0, 512, 3072, 3584, 4104, 4616, 5128, 5640, 6152, 6664, 7176, 7688]
            NSG = 14
            w_out_v = w_out_d.rearrange("(k p) n -> p k n", p=128)
            NSB = 3
            wsb = [sbt(pa, "wsb%d" % i, [128, 8, 512], BF16) for i in range(NSB)]
            NSTG = NSG + 8
            b_wst = [Buf() for _ in range(NSTG)]
            w_xq_v = w_xq_d.rearrange("(k p) n -> p k n", p=128)
            w_xo_v = w_xo_d.rearrange("(k p) n -> p k n", p=128)
            w_xkv_v0 = w_xkv_d.rearrange("(k p) n -> p k n", p=128)
            S.op(V, lambda e: e.memset(wsb[0][0][:].rearrange("p a b -> p (a b)"), 0.0), writes=[wsb[0][1]])
            xbuf_z = xbuf.rearrange("(g p r) d -> g p (r d)", p=128, r=4)
            for zg in range(xbuf_z.shape[0]):
                S.dma(G, xbuf_z[zg], wsb[0][0][:].rearrange("p a b -> p (a b)"), reads=[wsb[0][1]], writes=[Buf()])

            def stage_group(g):
                sg_t, b_sg = wsb[g % NSB]
                if g < 12:
                    src = w_in_v[:, :, GCOLS[g]:GCOLS[g] + 512]
                elif g < 14:
                    src = w_out_v[:, :, (g - 12) * 512:(g - 11) * 512]
                elif g < 16:
                    src = w_xq_v[:, :, (g - 14) * 512:(g - 13) * 512]
                elif g < 18:
                    src = w_xo_v[:, :, (g - 16) * 512:(g - 15) * 512]
                else:
                    src = w_xkv_v0[:, :, (g - 18) * 512:(g - 17) * 512]
                S.dma(G, sg_t[:], src, writes=[b_sg])
                S.dma(SP, wst[g], sg_t[:], reads=[b_sg], writes=[b_wst[g]])
            sstate = {"issued": 0, "used": 0}
            sseq = list(sseq_in) if sseq_in is not None else None

            def stream_issue(upto):
                while sstate["issued"] < min(upto, len(sseq)):
                    n = sstate["issued"]
                    t, b = wsb[n % NSB]
                    S.dma(SP, t[:], wst[sseq[n]], reads=[b_wst[sseq[n]]], writes=[b])
                    sstate["issued"] += 1

            def stream_next(g):
                n = sstate["used"]
                S.noswitch = True
                if sseq is None:
                    srec.append(g)
                    t, b = wsb[n % NSB]
                    S.dma(SP, t[:], wst[g], reads=[b_wst[g]], writes=[b])
                else:
                    assert sseq[n] == g, (n, g, sseq[n])
                    stream_issue(n + NSB)
                S.noswitch = False
                sstate["used"] += 1
                return wsb[n % NSB]

            def cload(name, shape, src, stack=pa):
                t, b = sbt(stack, name, shape, F32)
                S.dma(SP, t[:], src, writes=[b])
                return t, b

            gmix, b_gmix = cload("gmix", [128, D], gmix_d)
            mhg, b_mhg = cload("mhg", [128, D], mhg_d)
            sgug, b_sgug = cload("sgug", [128, D], sgug_d)
            bst, b_bst = cload("bst", [128, D], bs_d)
            bgate, b_bgate = cload("bgate", [128, 2 * D], bgate_d)
            bif, b_bif = cload("bif", [128, 8], bif_d)
            convw, b_convw = cload("convw", [128, 16, 4], convw_d)
            convb, b_convb = cload("convb", [128, 16], convb_d)
            pmask, b_pmask = cload("pmask", [128, NIT], pmask_d)
            wsTf, b_wsTf = cload("wsTf", [128, 8, 128], wsT_d)
            wsT, b_wsT = sbt(pa, "wsT", [128, 8, 128], BF16)
            for g in range(8):
                S.op(V, lambda e: e.tensor_tensor(out=wsT[:, g, :], in0=wsTf[:, g, :], in1=trif[:], op=ALU.mult),
                     reads=[b_wsTf, b_trif], writes=[b_wsT])

            R3 = 4
            RG = 3
            xt = [sbt(pa, "xt%d" % i, [128, D], F32) for i in range(R3)]
            xT = [sbt(pa, "xT%d" % i, [128, 8, 128], BF16) for i in range(R3)]
            st = [sbt(pa, "st%d" % i, [128, 4], F32) for i in range(2)]
            junk, b_junk = sbt(pa, "junk", [128, D], BF16)
            junk3, b_junk3 = sbt(pa, "junk3", [128, D], BF16)
            xn, b_xn = sbt(pa, "xn", [128, D], BF16)
            pre_t, b_pre = sbt(pa, "pre", [128, 16, 131], F32)
            halo, b_halo = sbt(pa, "halo", [128, 16, 3], F32)
            cacc, _ = sbt(pa, "cacc", [128, 16, 128], F32)
            b_cacc = [Buf() for _ in range(16)]
            qkT_r = [sbt(pa, "qkT%d" % i, [128, 16, 128], BF16) for i in range(2)]
            vaug_r = [sbt(pa, "vaug%d" % i, [128, 4, 257], F32) for i in range(2)]
            gif_r = [sbt(pa, "gif%d" % i, [128, 8], F32) for i in range(RG)]
            lsp_r = [sbt(pa, "lsp%d" % i, [128, 8], F32) for i in range(RG)]
            anb_r = [sbt(pa, "anb%d" % i, [128, 8], F32) for i in range(RG)]
            sm_r = [sbt(pa, "sm%d" % i, [4, 16], F32) for i in range(RG)]
            vw, _ = sbt(pa, "vw", [128, 4, 257], BF16)
            b_vw = [Buf() for _ in range(4)]
            eac, b_eac = sbt(pa, "eac", [128, 8], F32)
            bc, b_bc = sbt(pa, "bc", [128, 8], F32)
            mst, b_mst = sbt(pa, "mst", [4, 1], F32)
            rhs8, b_rhs8 = sbt(pa, "rhs8", [4, 8], F32)
            Cm, b_Cm0 = sbt(pa, "Cm", [128, 4, 2, 257], F32)
            b_Cm = [Buf() for _ in range(4)]
            Cb, b_Cb = sbt(pa, "Cb", [128, 2, 257], BF16)
            ktok, b_ktok = sbt(pa, "ktok", [128, 256], BF16)
            PT, b_PT = sbt(pa, "PT", [128, 128], BF16)
            ya_r = [sbt(pa, "ya%d" % i, [128, D], F32) for i in range(2)]
            hs_r = [sbt(pa, "hs%d" % i, [128, 16], F32) for i in range(2)]
            st3, b_st3 = sbt(pa, "st3", [128, 4], F32)
            so, b_so = sbt(pa, "so", [128, D], F32)
            ub, b_ub = sbt(pa, "ub", [128, D], F32)
            svb, b_svb = sbt(pa, "svb", [128, D], F32)
            svn, b_svn = sbt(pa, "svn", [128, D], BF16)
            gt, b_gt = sbt(pa, "gt", [128, 2 * D], F32)
            t1, b_t1 = sbt(pa, "t1", [128, 512], F32)
            t2, b_t2 = sbt(pa, "t2", [128, 512], F32)
            t3, b_t3 = sbt(pa, "t3", [128, 512], F32)
            yb, b_yb = sbt(pa, "yb", [128, D], F32)
            zb, b_zb = sbt(pa, "zb", [128, D], BF16)
            zT, b_zT = sbt(pa, "zT", [128, 8, 128], BF16)

            S.op(V, lambda e: e.memset(Cm[:].rearrange("p a b c -> p (a b c)"), 0.0), writes=b_Cm)
            S.op(V, lambda e: e.memset(mst[:], 0.0), writes=[b_mst])
            for i in range(2):
                S.op(V, lambda e: e.memset(vaug_r[i][0][:].rearrange("p a b -> p (a b)"), 1.0), writes=[vaug_r[i][1]])
            S.op(V, lambda e: e.memset(halo[:].rearrange("p a b -> p (a b)"), 0.0), writes=[b_halo])

            pp = Pool(banks[0:3], S, hold=1)
            trp = Pool(banks[3:4], S, hold=1)
            bk_tr, b_tr = banks[3]
            bk_s, b_s = banks[4]
            bk_num, b_num = banks[5]
            bk_kv0, b_kv0 = banks[6]
            bk_kv1, b_kv1 = banks[7]
            trv = bk_tr[:].bitcast(BF16)
            kv1_bf = bk_kv1[:].bitcast(BF16)

            b_x1s = [Buf() for _ in range(NCH)]

            def gelu_group(src_ps, b_src, dst, b_dst):
                S.op(A, lambda e: e.copy(out=t1[:], in_=src_ps), reads=[b_src], writes=[b_t1])
                S.op(A, lambda e: e.activation(out=t2[:], in_=t1[:], func=AF.Square), reads=[b_t1], writes=[b_t2])
                S.op(V, lambda e: e.tensor_scalar(out=t2[:], in0=t2[:], scalar1=0.044715, scalar2=1.0,
                                                  op0=ALU.mult, op1=ALU.add), reads=[b_t2], writes=[b_t2])
                S.op(V, lambda e: e.tensor_tensor(out=t3[:], in0=t2[:], in1=t1[:], op=ALU.mult),
                     reads=[b_t2, b_t1], writes=[b_t3])
                S.op(A, lambda e: e.activation(out=t3[:], in_=t3[:], func=AF.Sigmoid, scale=1.5957691216057308),
                     reads=[b_t3], writes=[b_t3])
                S.op(V, lambda e: e.tensor_tensor(out=dst, in0=t3[:], in1=t1[:], op=ALU.mult),
                     reads=[b_t3, b_t1], writes=[b_dst])

            def proj_w(xT_t, b_xTt, wt_, b_wt, c0, ncols):
                bk, b_bk = pp.next()
                S.mm([(bk[:, 0:ncols], [(xT_t[:, kc, :], wt_[:, kc, c0:c0 + ncols]) for kc in range(8)])],
                     reads=[b_xTt, b_wt], writes=[b_bk])
                return bk, b_bk

            def P1a(it):
                r2 = it % 2
                xt_t, b_xt = xt[it % R3]
                xT_t, b_xTt = xT[it % R3]
                st_t, b_stt = st[r2]
                gif, b_gif = gif_r[it % RG]
                lsp, b_lsp = lsp_r[it % RG]
                anb, b_anb = anb_r[it % RG]
                sm, b_sm = sm_r[it % RG]
                S.dma(SP, xt_t[:], xseq[it * 128:(it + 1) * 128, :], writes=[b_xt])
                rms_rstd(A, xt_t[:], b_xt, junk[:], b_junk, st_t, b_stt, float(D))
                S.op(V, lambda e: e.scalar_tensor_tensor(out=xn[:], in0=xt_t[:], scalar=st_t[:, 2:3], in1=gmix[:],
                                                         op0=ALU.mult, op1=ALU.mult),
                     reads=[b_xt, b_stt, b_gmix], writes=[b_xn])
                trp.next()
                S.tr([(trv[:, k * 128:(k + 1) * 128], xn[:, k * 128:(k + 1) * 128]) for k in range(8)], identb[:],
                     reads=[b_xn, b_identb], writes=[b_tr])
                S.op(A, lambda e: e.copy(out=xT_t[:].rearrange("p k t -> p (k t)"), in_=trv), reads=[b_tr], writes=[b_xTt])
                trp.release_mine()
                bk, b_bk = proj_w(xT_t, b_xTt, Wif, b_wif, 0, 8)
                S.op(V, lambda e: e.tensor_tensor(out=gif[:], in0=bk[:, 0:8], in1=bif[:], op=ALU.add),
                     reads=[b_bk, b_bif], writes=[b_gif])
                S.op(A, lambda e: e.activation(out=lsp[:, 0:4], in_=gif[:, 4:8], func=AF.Exp, scale=-1.0),
                     reads=[b_gif], writes=[b_lsp])
                S.op(A, lambda e: e.activation(out=lsp[:, 4:8], in_=lsp[:, 0:4], func=AF.Ln, bias=1.0),
                     reads=[b_lsp], writes=[b_lsp])
                bkg, b_bkg = pp.next()
                S.mm([(bkg[:, 128:132], [(trif[:], lsp[:, 4:8])]),
                      (bkg[0:4, 136:137], [(lsp[:, 4:8], onesf[:, 0:1])])],
                     reads=[b_trif, b_lsp, b_onesf], writes=[b_bkg])
                S.op(V, lambda e: e.tensor_copy(out=anb[:, 4:8], in_=bkg[:, 128:132]), reads=[b_bkg], writes=[b_anb])
                S.op(V, lambda e: e.tensor_tensor(out=anb[:, 0:4], in0=bkg[:, 128:132], in1=gif[:, 0:4], op=ALU.add),
                     reads=[b_bkg, b_gif], writes=[b_anb])
                S.op(V, lambda e: e.tensor_copy(out=sm[:, 0:1], in_=bkg[0:4, 136:137]), reads=[b_bkg], writes=[b_sm])
                S.tr([(bkg[0:4, 256:384], anb[:, 0:4])], identf[:], reads=[b_anb, b_identf], writes=[b_bkg])
                S.op(V, lambda e: e.tensor_reduce(out=sm[:, 1:2], in_=bkg[0:4, 256:384], axis=AX.X, op=ALU.max),
                     reads=[b_bkg], writes=[b_sm])
                pp.release_mine()

            def P1b(it):
                main = it >= NPRE
                r2 = it % 2
                xT_t, b_xTt = xT[it % R3]
                qkT, b_qkT = qkT_r[r2]
                vaug, b_vaug = vaug_r[r2]
                nlist = list(range(16)) if (main or it == NPRE - 1) else list(range(8, 16))
                n0 = nlist[0]
                S.op(G, lambda e: e.tensor_copy(out=pre_t[:, n0:16, 0:3], in_=halo[:, n0:16, :]),
                     reads=[b_halo], writes=[b_pre])
                for g0 in range(0, len(nlist), 4):
                    grp = nlist[g0:g0 + 4]
                    bk, b_bk = pp.next()
                    if grp[0] >= 8:
                        wt_, b_wt = Wk, b_wk
                        off = (grp[0] - 8) * 128
                    else:
                        wt_, b_wt = stream_next(grp[0] // 4)
                        off = 0
                    S.mm([(bk[:, j * 128:(j + 1) * 128],
                           [(wt_[:, kc, off + j * 128:off + (j + 1) * 128], xT_t[:, kc, :]) for kc in range(8)])
                          for j in range(4)],
                         reads=[b_xTt, b_wt], writes=[b_bk])
                    S.op(A, lambda e: e.copy(out=pre_t[:, grp[0]:grp[0] + 4, 3:131],
                                             in_=bk[:].rearrange("p (a b) -> p a b", a=4)),
                         reads=[b_bk], writes=[b_pre])
                S.op(G, lambda e: e.tensor_copy(out=halo[:, n0:16, :], in_=pre_t[:, n0:16, 128:131]),
                     reads=[b_pre], writes=[b_halo])
                for i in nlist:
                    S.op(A, lambda e: e.activation(out=cacc[:, i, :], in_=pre_t[:, i, 3:131], func=AF.Identity,
                                                   scale=convw[:, i, 3:4], bias=convb[:, i:i + 1]),
                         reads=[b_pre, b_convw, b_convb], writes=[b_cacc[i]])
                for j in range(3):
                    for i in nlist:
                        S.op(V, lambda e: e.scalar_tensor_tensor(out=cacc[:, i, :], in0=pre_t[:, i, j:j + 128],
                                                                 scalar=convw[:, i, j:j + 1], in1=cacc[:, i, :],
                                                                 op0=ALU.mult, op1=ALU.add),
                             reads=[b_pre, b_convw, b_cacc[i]], writes=[b_cacc[i]])
                S.op(A, lambda e: e.activation(out=qkT[:, n0:16, :].rearrange("p a b -> p (a b)"),
                                               in_=cacc[:, n0:16, :].rearrange("p a b -> p (a b)"), func=AF.Silu),
                     reads=[b_cacc[i] for i in nlist], writes=[b_qkT])
                for hgp in range(2):
                    bk, b_bk = proj_w(xT_t, b_xTt, Wv, b_wv, hgp * 512, 512)
                    S.op(A, lambda e: e.copy(out=vaug[:, 2 * hgp:2 * hgp + 2, 0:256],
                                             in_=bk[:].rearrange("p (a b) -> p a b", a=2)),
                         reads=[b_bk], writes=[b_vaug])
                pp.release_mine()

            def P2(it):
                main = it >= NPRE
                r2 = it % 2
                qkT, b_qkT = qkT_r[r2]
                vaug, b_vaug = vaug_r[r2]
                gif, b_gif = gif_r[it % RG]
                anb, b_anb = anb_r[it % RG]
                sm, b_sm = sm_r[it % RG]
                hbuf, b_hbuf = ya_r[r2]
                hs, b_hs = hs_r[r2]
                S.op(V, lambda e: e.tensor_tensor(out=sm[:, 2:3], in0=sm[:, 1:2], in1=mst[:], op=ALU.max),
                     reads=[b_sm, b_mst], writes=[b_sm])
                S.op(V, lambda e: e.tensor_scalar(out=sm[:, 3:4], in0=sm[:, 2:3], scalar1=-1.0, scalar2=None, op0=ALU.mult),
                     reads=[b_sm], writes=[b_sm])
                S.op(A, lambda e: e.activation(out=sm[:, 4:5], in_=mst[:], func=AF.Exp, bias=sm[:, 3:4]),
                     reads=[b_sm, b_mst], writes=[b_sm])
                S.op(V, lambda e: e.tensor_tensor(out=mst[:], in0=sm[:, 2:3], in1=sm[:, 0:1], op=ALU.subtract),
                     reads=[b_sm], writes=[b_mst])
                S.op(V, lambda e: e.tensor_scalar(out=rhs8[:, 0:4], in0=identf[0:4, 0:4], scalar1=sm[:, 3:4], scalar2=None,
                                                  op0=ALU.mult), reads=[b_sm, b_identf], writes=[b_rhs8])
                S.op(V, lambda e: e.tensor_scalar(out=rhs8[:, 4:8], in0=identf[0:4, 0:4], scalar1=sm[:, 4:5], scalar2=None,
                                                  op0=ALU.mult), reads=[b_sm, b_identf], writes=[b_rhs8])
                S.mm([(bk_s[:, 144:152], [(onesf[0:4, :], rhs8[:])])], reads=[b_onesf, b_rhs8], writes=[b_s])
                S.op(V, lambda e: e.tensor_copy(out=bc[:], in_=bk_s[:, 144:152]), reads=[b_s], writes=[b_bc])
                S.op(V, lambda e: e.tensor_tensor(out=eac[:, 0:4], in0=anb[:, 0:4], in1=bc[:, 0:4], op=ALU.add),
                     reads=[b_anb, b_bc], writes=[b_eac])
                S.op(V, lambda e: e.tensor_tensor(out=eac[:, 4:8], in0=anb[:, 4:8], in1=bc[:, 0:4], op=ALU.add),
                     reads=[b_anb, b_bc], writes=[b_eac])
                S.op(A, lambda e: e.activation(out=eac[:], in_=eac[:], func=AF.Exp), reads=[b_eac], writes=[b_eac])
                if not main:
                    S.op(V, lambda e: e.tensor_scalar(out=eac[:, 0:4], in0=eac[:, 0:4], scalar1=pmask[:, it:it + 1],
                                                      scalar2=None, op0=ALU.mult),
                         reads=[b_eac, b_pmask], writes=[b_eac])
                for h in range(4):
                    S.op(V, lambda e: e.tensor_scalar(out=vw[:, h, :], in0=vaug[:, h, :], scalar1=eac[:, h:h + 1],
                                                      scalar2=None, op0=ALU.mult),
                         reads=[b_vaug, b_eac], writes=[b_vw[h]])
                for h in range(4):
                    S.tr([(kv1_bf[:, 640 + dc * 128:640 + (dc + 1) * 128], qkT[:, 8 + 2 * h + dc, :]) for dc in range(2)],
                         identb[:], reads=[b_qkT, b_identb], writes=[b_kv1])
                    S.op(A, lambda e: e.mul(out=ktok[:], in_=kv1_bf[:, 640:896], mul=0.0625), reads=[b_kv1], writes=[b_ktok])
                    if main:
                        S.mm([(bk_s[:, 0:128], [(qkT[:, 8 + 2 * h + dc, :], qkT[:, 2 * h + dc, :]) for dc in range(2)])],
                             reads=[b_qkT], writes=[b_s])
                        S.op(V, lambda e: e.scalar_tensor_tensor(out=PT[:], in0=bk_s[:, 0:128], scalar=0.0625, in1=trif[:],
                                                                 op0=ALU.mult, op1=ALU.mult),
                             reads=[b_s, b_trif], writes=[b_PT])
                        S.op(V, lambda e: e.tensor_scalar(out=Cb[:].rearrange("p a b -> p (a b)"),
                                                          in0=Cm[:, h, :, :].rearrange("p a b -> p (a b)"),
                                                          scalar1=bc[:, 4 + h:5 + h], scalar2=None, op0=ALU.mult),
                             reads=[b_Cm[h], b_bc], writes=[b_Cb])
                        S.mm([(bk_num[:, 0:257], [(PT[:], vw[:, h, :]),
                                                  (qkT[:, 2 * h, :], Cb[:, 0, :]),
                                                  (qkT[:, 2 * h + 1, :], Cb[:, 1, :])])],
                             reads=[b_PT, b_vw[h], b_qkT, b_Cb], writes=[b_num])
                    S.mm([(bk_kv0[:, 0:257], [(ktok[:, 0:128], vw[:, h, :])]),
                          (bk_kv1[:, 0:257], [(ktok[:, 128:256], vw[:, h, :])])],
                         reads=[b_ktok, b_vw[h]], writes=[b_kv0, b_kv1])
                    S.op(V, lambda e: e.scalar_tensor_tensor(out=Cm[:, h, 0, :], in0=Cm[:, h, 0, :], scalar=bc[:, 4 + h:5 + h],
                                                             in1=bk_kv0[:, 0:257], op0=ALU.mult, op1=ALU.add),
                         reads=[b_Cm[h], b_bc, b_kv0], writes=[b_Cm[h]])
                    S.op(V, lambda e: e.scalar_tensor_tensor(out=Cm[:, h, 1, :], in0=Cm[:, h, 1, :], scalar=bc[:, 4 + h:5 + h],
                                                             in1=bk_kv1[:, 0:257], op0=ALU.mult, op1=ALU.add),
                         reads=[b_Cm[h], b_bc, b_kv1], writes=[b_Cm[h]])
                    if main:
                        S.op(V, lambda e: e.tensor_scalar(out=hs[:, 8 + h:9 + h], in0=bk_num[:, 256:257], scalar1=-1.0,
                                                          scalar2=None, op0=ALU.mult), reads=[b_num], writes=[b_hs])
                        S.op(V, lambda e: e.tensor_tensor(out=hs[:, 8 + h:9 + h], in0=hs[:, 8 + h:9 + h],
                                                          in1=bk_num[:, 256:257], op=ALU.max), reads=[b_num, b_hs], writes=[b_hs])
                        S.op(V, lambda e: e.tensor_tensor(out=hs[:, 8 + h:9 + h], in0=hs[:, 8 + h:9 + h],
                                                          in1=eac[:, 4 + h:5 + h], op=ALU.max),
                             reads=[b_hs, b_eac], writes=[b_hs])
                        S.op(V, lambda e: e.reciprocal(out=hs[:, 12 + h:13 + h], in_=hs[:, 8 + h:9 + h]),
                             reads=[b_hs], writes=[b_hs])
                        S.op(A, lambda e: e.activation(out=hbuf[:, h * 256:(h + 1) * 256], in_=bk_num[:, 0:256],
                                                       func=AF.Copy, scale=hs[:, 12 + h:13 + h]),
                             reads=[b_num, b_hs], writes=[b_hbuf])
                        S.op(A, lambda e: e.activation(out=junk[:, 0:256], in_=hbuf[:, h * 256:(h + 1) * 256],
                                                       func=AF.Square, accum_out=hs[:, h:h + 1]),
                             reads=[b_hbuf], writes=[b_junk, b_hs])

            def P3(it):
                c = it - NPRE
                r2 = it % 2
                xt_t, b_xt = xt[it % R3]
                xT_t, b_xTt = xT[it % R3]
                ya, b_ya = ya_r[r2]
                hs, b_hs = hs_r[r2]

                def proj_s(g):
                    wt_, b_wt = stream_next(g)
                    return proj_w(xT_t, b_xTt, wt_, b_wt, 0, 512)

                for hgp in range(2):
                    bk, b_bk = proj_s(2 + hgp)
                    S.op(A, lambda e: e.activation(out=so[:, hgp * 512:(hgp + 1) * 512], in_=bk[:], func=AF.Sigmoid),
                         reads=[b_bk], writes=[b_so])
                for hgp in range(2):
                    bk, b_bk = proj_s(4 + hgp)
                    gelu_group(bk[:], b_bk, ub[:, hgp * 512:(hgp + 1) * 512], b_ub)
                for hgp in range(2):
                    bk, b_bk = proj_s(6 + hgp)
                    gelu_group(bk[:], b_bk, svb[:, hgp * 512:(hgp + 1) * 512], b_svb)
                for hgp in range(4):
                    bk, b_bk = proj_s(8 + hgp)
                    S.op(V, lambda e: e.tensor_tensor(out=gt[:, hgp * 512:(hgp + 1) * 512], in0=bk[:],
                                                      in1=bgate[:, hgp * 512:(hgp + 1) * 512], op=ALU.add),
                         reads=[b_bk, b_bgate], writes=[b_gt])
                S.op(A, lambda e: e.activation(out=gt[:], in_=gt[:], func=AF.Sigmoid), reads=[b_gt], writes=[b_gt])
                rms_rstd(A, svb[:], b_svb, junk3[:], b_junk3, st3, b_st3, float(D))
                S.op(V, lambda e: e.scalar_tensor_tensor(out=svn[:], in0=svb[:], scalar=st3[:, 2:3], in1=sgug[:],
                                                         op0=ALU.mult, op1=ALU.mult),
                     reads=[b_svb, b_st3, b_sgug], writes=[b_svn])
                for half in range(2):
                    bk, b_bk = pp.next()
                    S.mm([(bk[:, j * 128:(j + 1) * 128], [(wsT[:, half * 4 + j, :], svn[:, (half * 4 + j) * 128:(half * 4 + j + 1) * 128])])
                          for j in range(4)], reads=[b_wsT, b_svn], writes=[b_bk])
                    sl = slice(half * 512, (half + 1) * 512)
                    S.op(V, lambda e: e.tensor_tensor(out=yb[:, sl], in0=bk[:], in1=bst[:, sl], op=ALU.add),
                         reads=[b_bk, b_bst], writes=[b_yb])
                S.op(G, lambda e: e.tensor_tensor(out=yb[:], in0=yb[:], in1=ub[:], op=ALU.mult), reads=[b_yb, b_ub], writes=[b_yb])
                S.op(G, lambda e: e.tensor_tensor(out=yb[:], in0=yb[:], in1=gt[:, D:2 * D], op=ALU.mult), reads=[b_yb, b_gt], writes=[b_yb])
                S.op(V, lambda e: e.tensor_scalar(out=hs[:, 4:8], in0=hs[:, 0:4], scalar1=1.0 / 256, scalar2=EPS,
                                                  op0=ALU.mult, op1=ALU.add), reads=[b_hs], writes=[b_hs])
                S.op(A, lambda e: e.activation(out=hs[:, 4:8], in_=hs[:, 4:8], func=AF.Sqrt), reads=[b_hs], writes=[b_hs])
                S.op(V, lambda e: e.reciprocal(out=hs[:, 4:8], in_=hs[:, 4:8]), reads=[b_hs], writes=[b_hs])
                for h in range(4):
                    sl = slice(h * 256, (h + 1) * 256)
                    S.op(V, lambda e: e.scalar_tensor_tensor(out=ya[:, sl], in0=ya[:, sl], scalar=hs[:, 4 + h:5 + h],
                                                             in1=mhg[:, sl], op0=ALU.mult, op1=ALU.mult),
                         reads=[b_ya, b_hs, b_mhg], writes=[b_ya])
                S.op(G, lambda e: e.tensor_tensor(out=ya[:], in0=ya[:], in1=so[:], op=ALU.mult), reads=[b_ya, b_so], writes=[b_ya])
                S.op(G, lambda e: e.tensor_tensor(out=ya[:], in0=ya[:], in1=gt[:, 0:D], op=ALU.mult), reads=[b_ya, b_gt], writes=[b_ya])
                S.op(V, lambda e: e.tensor_tensor(out=zb[:], in0=ya[:], in1=yb[:], op=ALU.add), reads=[b_ya, b_yb], writes=[b_zb])
                trp.next()
                S.tr([(trv[:, k * 128:(k + 1) * 128], zb[:, k * 128:(k + 1) * 128]) for k in range(8)], identb[:],
                     reads=[b_zb, b_identb], writes=[b_tr])
                S.op(A, lambda e: e.copy(out=zT[:].rearrange("p k t -> p (k t)"), in_=trv), reads=[b_tr], writes=[b_zT])
                trp.release_mine()
                for half in range(2):
                    bk, b_bk = pp.next()
                    sl = slice(half * 512, (half + 1) * 512)
                    wo_, b_wo = stream_next(12 + half)
                    S.mm([(bk[:], [(zT[:, kc, :], wo_[:, kc, :]) for kc in range(8)])], reads=[b_zT, b_wo], writes=[b_bk])
                    S.op(V, lambda e: e.tensor_tensor(out=xt_t[:, sl], in0=bk[:], in1=xt_t[:, sl], op=ALU.add),
                         reads=[b_bk, b_xt], writes=[b_xt])
                if stage == 1:
                    S.dma(G, y_d[c * 128:(c + 1) * 128, :], xt_t[:], reads=[b_xt], writes=[b_x1s[c]])
                else:
                    S.dma(G, x1s[c * 128:(c + 1) * 128, :], xt_t[:], reads=[b_xt], writes=[b_x1s[c]])

            for rnd in range(NIT + 3):
                if rnd >= 2 and rnd % 2 == 0 and (rnd - 2) // 2 < NSTG:
                    stage_group((rnd - 2) // 2)
                fns = []
                if rnd < NIT:
                    fns.append((lambda i=rnd: P1a(i), 1))
                if 0 <= rnd - 1 < NIT:
                    fns.append((lambda i=rnd - 1: P1b(i), 1))
                if 0 <= rnd - 2 < NIT:
                    fns.append((lambda i=rnd - 2: P2(i), 1))
                if NPRE <= rnd - 3 < NIT:
                    fns.append((lambda i=rnd - 3: P3(i), 2))
                S.weave(fns)

        if stage == 1:
            S.wait_all(G, b_x1s)
            return nc
        S.barrier()

        pbc = ExitStack()
        with pbc:
            acc = pbc.enter_context(nc.sbuf_tensor("s_acc", [128, NCH, D], F32))
            b_acc = [Buf() for _ in range(NCH)]
            xn3B = pbc.enter_context(nc.sbuf_tensor("s_xn3B", [128, NCH, D], BF16))
            b_xn3B = [Buf() for _ in range(NCH)]
            mskA, b_mskA = sbt(pbc, "mskA", [128, NCH, NE], F32)
            posA, b_posA = sbt(pbc, "posA", [128, NCH, NE], F32)
            runcnt, b_runcnt = sbt(pbc, "runcnt", [128, NE], F32)
            desti, b_desti = sbt(pbc, "desti", [128, NCH, 2], I32)
            gate2, b_gate2 = sbt(pbc, "gate2", [128, NCH, 2], F32)
            idxWi, b_idxWi = sbt(pbc, "idxWi", [128, NBLK, 2], I32)
            trisb, b_trisb = sbt(pbc, "trisb", [128, 128], BF16)
            onesb, b_onesb = sbt(pbc, "onesb", [128, 128], BF16)
            S.op(V, lambda e: e.tensor_tensor(out=trisb[:], in0=trif[:], in1=identf[:], op=ALU.subtract),
                 reads=[b_trif, b_identf], writes=[b_trisb])
            S.op(V, lambda e: e.memset(onesb[:], 1.0), writes=[b_onesb])
            S.op(V, lambda e: e.memset(runcnt[:], 0.0), writes=[b_runcnt])
            Gt = pbc.enter_context(nc.sbuf_tensor("s_Gt", [128, NCH, NE], F32))
            b_Gt = [Buf() for _ in range(NCH)]
            gf, b_gf = sbt(pbc, "gf", [128, D], F32)
            S.dma(SP, gf[:], gf_d, writes=[b_gf])
            junk, b_junk = sbt(pbc, "junk2", [128, D], BF16)
            b_y = [Buf() for _ in range(NCH)]

            pb = ExitStack()
            with pb:
                Wxq, b_wxq = sbt(pb, "Wxq", [128, 8, D], BF16)
                for hh in range(2):
                    S.dma(SP, Wxq[:, :, hh * 512:(hh + 1) * 512], wst[14 + hh], reads=[b_wst[14 + hh]], writes=[b_wxq])
                Wxo, b_wxo = sbt(pb, "Wxo", [128, 8, D], BF16)
                for hh in range(2):
                    S.dma(SP, Wxo[:, :, hh * 512:(hh + 1) * 512], wst[16 + hh], reads=[b_wst[16 + hh]], writes=[b_wxo])
                Wkv, b_wkv = sbt(pb, "Wkv", [128, 8, 512], BF16)
                w_xkv_v = w_xkv_d.rearrange("(k p) n -> p k n", p=128)
                Wr, b_wr = sbt(pb, "Wr", [128, 8, 36], F32)
                S.dma(SP, Wr[:], w_r_d.rearrange("(k p) n -> p k n", p=128), writes=[b_wr])

                def cload2(name, shape, src):
                    t, b = sbt(pb, name, shape, F32)
                    S.dma(SP, t[:], src, writes=[b])
                    return t, b

                gx, b_gx = cload2("gx", [128, D], gx_d)
                gmoe, b_gmoe = cload2("gmoe", [128, D], gmem_d)
                gmem, b_gmem = gmoe, b_gmoe
                brt, b_brt = cload2("brt", [128, 36], br_d)

                xt = [sbt(pb, "bxt0", [128, D], F32)] * 2
                st = [sbt(pb, "bst%d" % i, [128, 4], F32) for i in range(2)]
                xn, b_xn = sbt(pb, "bxn", [128, D], BF16)
                xT, b_xT = sbt(pb, "bxT", [128, 8, 128], BF16)
                mnT, b_mnT = sbt(pb, "mnT", [128, 8, 256], BF16)
                KT, b_KT = sbt(pb, "KT", [128, 8, 256], BF16)
                Vm, b_Vm = sbt(pb, "Vm", [128, 2, D], BF16)
                pexp, b_pexp = sbt(pb, "pexp", [128, 4, 256], BF16)
                pT, b_pT = sbt(pb, "pT", [128, 8, 128], BF16)
                ss, b_ss = sbt(pb, "ss", [128, 16], F32)
                attn, b_attn = sbt(pb, "attn", [128, D], BF16)
                xn3f, b_xn3f = sbt(pb, "xn3f", [128, D], F32)
                xTf, b_xTf = sbt(pb, "xTf", [128, 8, 128], F32)
                lg, b_lg = sbt(pb, "lg", [128, 36], F32)
                rt, b_rt = sbt(pb, "rt", [128, 16], F32)
                oh, b_oh = sbt(pb, "oh", [128, 4], F32)
                ml, b_ml = sbt(pb, "ml", [128, NE], F32)
                ml2, b_ml2 = sbt(pb, "ml2", [128, NE], F32)
                msk, b_msk = sbt(pb, "msk", [128, NE], F32)
                mskb, b_mskb = sbt(pb, "mskb", [128, NE], BF16)

                pp = Pool(banks[0:2])
                ppX1 = Pool(banks[2:3])
                ppX2 = Pool(banks[3:5])
                bk_tr, b_tr = banks[5]
                bk_tr2, b_tr2 = banks[6]
                bk_tr3, b_tr3 = banks[7]
                trv = bk_tr[:].bitcast(BF16)
                trv2 = bk_tr2[:].bitcast(BF16)
                trv3 = bk_tr3[:].bitcast(BF16)

                for mc in range(2):
                    xt_t, b_xt = xt[mc]
                    st_t, b_stt = st[mc]
                    S.dma(SP, xt_t[:], mem_d[mc * 128:(mc + 1) * 128, :], writes=[b_xt])
                    rms_rstd(A, xt_t[:], b_xt, junk[:], b_junk, st_t, b_stt, float(D))
                    S.op(V, lambda e: e.scalar_tensor_tensor(out=xn[:], in0=xt_t[:], scalar=st_t[:, 2:3], in1=gmem[:],
                                                             op0=ALU.mult, op1=ALU.mult),
                         reads=[b_xt, b_stt, b_gmem], writes=[b_xn])
                    S.tr([(trv[:, k * 128:(k + 1) * 128], xn[:, k * 128:(k + 1) * 128]) for k in range(8)], identb[:],
                         reads=[b_xn, b_identb], writes=[b_tr])
                    S.op(A, lambda e: e.copy(out=mnT[:, :, mc * 128:(mc + 1) * 128],
                                             in_=trv.rearrange("p (k t) -> p k t", k=8)), reads=[b_tr], writes=[b_mnT])
                S.dma(SP, gmoe[:], gmoe_d, reads=[], writes=[b_gmoe])
                for grp4 in range(4):
                    S.dma(SP, Wkv[:], wst[18 + grp4], reads=[b_wst[18 + grp4]], writes=[b_wkv])
                    if grp4 < 2:
                        for jj in range(2):
                            i0 = grp4 * 4 + jj * 2
                            bk, b_bk = pp.next()
                            S.mm([(bk[:, j * 256:(j + 1) * 256],
                                   [(Wkv[:, kc, (jj * 2 + j) * 128:(jj * 2 + j + 1) * 128], mnT[:, kc, :]) for kc in range(8)])
                                  for j in range(2)], reads=[b_wkv, b_mnT], writes=[b_bk])
                            S.op(A, lambda e: e.copy(out=KT[:, i0:i0 + 2, :].rearrange("p a b -> p (a b)"), in_=bk[:]),
                                 reads=[b_bk], writes=[b_KT])
                    else:
                        half = grp4 - 2
                        for mc in range(2):
                            bk, b_bk = pp.next()
                            S.mm([(bk[:], [(mnT[:, kc, mc * 128:(mc + 1) * 128], Wkv[:, kc, :]) for kc in range(8)])],
                                 reads=[b_wkv, b_mnT], writes=[b_bk])
                            S.op(A, lambda e: e.copy(out=Vm[:, mc, half * 512:(half + 1) * 512], in_=bk[:]),
                                 reads=[b_bk], writes=[b_Vm])

                q2T_r = [sbt(pb, "q2Tr%d" % i, [128, 8, 128], BF16) for i in range(2)]
                aT_r = [sbt(pb, "aTr%d" % i, [128, 8, 128], BF16) for i in range(2)]
                stY = [sbt(pb, "stY%d" % i, [128, 4], F32) for i in range(2)]
                junkY, b_junkY = sbt(pb, "junkY", [128, D], BF16)

                def BX1(c):
                    st_t, b_stt = st[c % 2]
                    q2T, b_q2T = q2T_r[c % 2]
                    S.dma(SP, acc[:, c, :], x1s[c * 128:(c + 1) * 128, :], reads=[b_x1s[c]], writes=[b_acc[c]])
                    rms_rstd(A, acc[:, c, :], b_acc[c], junk[:], b_junk, st_t, b_stt, float(D))
                    S.op(V, lambda e: e.scalar_tensor_tensor(out=xn[:], in0=acc[:, c, :], scalar=st_t[:, 2:3], in1=gx[:],
                                                             op0=ALU.mult, op1=ALU.mult),
                         reads=[b_acc[c], b_stt, b_gx], writes=[b_xn])
                    S.tr([(trv[:, k * 128:(k + 1) * 128], xn[:, k * 128:(k + 1) * 128]) for k in range(8)], identb[:],
                         reads=[b_xn, b_identb], writes=[b_tr])
                    S.op(A, lambda e: e.copy(out=xT[:].rearrange("p k t -> p (k t)"), in_=trv), reads=[b_tr], writes=[b_xT])
                    for i0 in range(0, 8, 4):
                        bk, b_bk = ppX1.next()
                        S.mm([(bk[:, j * 128:(j + 1) * 128],
                               [(Wxq[:, kc, (i0 + j) * 128:(i0 + j + 1) * 128], xT[:, kc, :]) for kc in range(8)])
                              for j in range(4)], reads=[b_wxq, b_xT], writes=[b_bk])
                        S.op(A, lambda e: e.copy(out=q2T[:, i0:i0 + 4, :].rearrange("p a b -> p (a b)"), in_=bk[:]),
                             reads=[b_bk], writes=[b_q2T])

                def BX2(c):
                    q2T, b_q2T = q2T_r[c % 2]
                    aT, b_aT = aT_r[c % 2]
                    for hp in range(2):
                        bk, b_bk = ppX2.next()
                        S.mm([(bk[:, j * 256:(j + 1) * 256],
                               [(q2T[:, 2 * (2 * hp + j) + dc, :], KT[:, 2 * (2 * hp + j) + dc, :]) for dc in range(2)])
                              for j in range(2)], reads=[b_q2T, b_KT], writes=[b_bk])
                        S.op(V, lambda e: e.tensor_reduce(out=ss[:, 2 * hp:2 * hp + 2], in_=bk[:].rearrange("p (a b) -> p a b", a=2),
                                                          axis=AX.X, op=ALU.max), reads=[b_bk], writes=[b_ss])
                        S.op(V, lambda e: e.tensor_scalar(out=ss[:, 4 + 2 * hp:6 + 2 * hp], in0=ss[:, 2 * hp:2 * hp + 2],
                                                          scalar1=-0.0625, scalar2=None, op0=ALU.mult), reads=[b_ss], writes=[b_ss])
                        for j in range(2):
                            h = 2 * hp + j
                            S.op(A, lambda e: e.activation(out=pexp[:, h, :], in_=bk[:, j * 256:(j + 1) * 256], func=AF.Exp,
                                                           scale=0.0625, bias=ss[:, 4 + h:5 + h], accum_out=ss[:, 8 + h:9 + h]),
                                 reads=[b_bk, b_ss], writes=[b_pexp, b_ss])
                    S.op(V, lambda e: e.reciprocal(out=ss[:, 12:16], in_=ss[:, 8:12]), reads=[b_ss], writes=[b_ss])
                    S.tr([(trv2[:, (2 * h + mc) * 128:(2 * h + mc + 1) * 128], pexp[:, h, mc * 128:(mc + 1) * 128])
                          for h in range(4) for mc in range(2)], identb[:], reads=[b_pexp, b_identb], writes=[b_tr2])
                    S.op(A, lambda e: e.copy(out=pT[:].rearrange("p k t -> p (k t)"), in_=trv2), reads=[b_tr2], writes=[b_pT])
                    for hp in range(2):
                        bk, b_bk = ppX2.next()
                        S.mm([(bk[:, j * 256:(j + 1) * 256],
                               [(pT[:, 2 * (2 * hp + j) + mc, :], Vm[:, mc, (2 * hp + j) * 256:(2 * hp + j + 1) * 256]) for mc in range(2)])
                              for j in range(2)], reads=[b_pT, b_Vm], writes=[b_bk])
                        for j in range(2):
                            h = 2 * hp + j
                            S.op(A, lambda e: e.activation(out=attn[:, h * 256:(h + 1) * 256], in_=bk[:, j * 256:(j + 1) * 256],
                                                           func=AF.Copy, scale=ss[:, 12 + h:13 + h]),
                                 reads=[b_bk, b_ss], writes=[b_attn])
                    S.tr([(trv2[:, k * 128:(k + 1) * 128], attn[:, k * 128:(k + 1) * 128]) for k in range(8)], identb[:],
                         reads=[b_attn, b_identb], writes=[b_tr2])
                    S.op(A, lambda e: e.copy(out=aT[:].rearrange("p k t -> p (k t)"), in_=trv2), reads=[b_tr2], writes=[b_aT])

                def BY(c):
                    st_t, b_stt = stY[c % 2]
                    aT, b_aT = aT_r[c % 2]
                    for half in range(2):
                        bk, b_bk = pp.next()
                        sl = slice(half * 512, (half + 1) * 512)
                        S.mm([(bk[:], [(aT[:, kc, :], Wxo[:, kc, sl]) for kc in range(8)])], reads=[b_aT, b_wxo], writes=[b_bk])
                        S.op(V, lambda e: e.tensor_tensor(out=acc[:, c, sl], in0=bk[:], in1=acc[:, c, sl], op=ALU.add),
                             reads=[b_bk, b_acc[c]], writes=[b_acc[c]])
                    if stage == 2:
                        S.dma(G, y_d[c * 128:(c + 1) * 128, :], acc[:, c, :], reads=[b_acc[c]], writes=[b_y[c]])
                        return
                    rms_rstd(A, acc[:, c, :], b_acc[c], junkY[:], b_junkY, st_t, b_stt, float(D))
                    S.op(V, lambda e: e.scalar_tensor_tensor(out=xn3f[:], in0=acc[:, c, :], scalar=st_t[:, 2:3], in1=gmoe[:],
                                                             op0=ALU.mult, op1=ALU.mult),
                         reads=[b_acc[c], b_stt, b_gmoe], writes=[b_xn3f])
                    S.op(G, lambda e: e.tensor_copy(out=xn3B[:, c, :], in_=xn3f[:]), reads=[b_xn3f], writes=[b_xn3B[c]])
                    for half in range(2):
                        bk, b_bk = pp.next()
                        S.tr([(bk[:, k * 128:(k + 1) * 128], xn3f[:, (half * 4 + k) * 128:(half * 4 + k + 1) * 128]) for k in range(4)],
                             identf[:], reads=[b_xn3f, b_identf], writes=[b_bk])
                        S.op(V, lambda e: e.tensor_copy(out=xTf[:, half * 4:half * 4 + 4, :].rearrange("p a b -> p (a b)"), in_=bk[:]),
                             reads=[b_bk], writes=[b_xTf])
                    bk, b_bk = pp.next()
                    S.mm([(bk[:, 0:36], [(xTf[:, kc, :], Wr[:, kc, :]) for kc in range(8)])], reads=[b_xTf, b_wr], writes=[b_bk])
                    S.op(V, lambda e: e.tensor_tensor(out=lg[:], in0=bk[:, 0:36], in1=brt[:], op=ALU.add),
                         reads=[b_bk, b_brt], writes=[b_lg])
                    S.op(V, lambda e: e.tensor_reduce(out=rt[:, 0:1], in_=lg[:, 0:4], axis=AX.X, op=ALU.max), reads=[b_lg], writes=[b_rt])
                    S.op(V, lambda e: e.tensor_scalar(out=oh[:], in0=lg[:, 0:4], scalar1=rt[:, 0:1], scalar2=None, op0=ALU.is_ge),
                         reads=[b_lg, b_rt], writes=[b_oh])
                    S.op(V, lambda e: e.tensor_scalar(out=rt[:, 1:2], in0=rt[:, 0:1], scalar1=-1.0, scalar2=None, op0=ALU.mult),
                         reads=[b_rt], writes=[b_rt])
                    S.op(A, lambda e: e.activation(out=junk[:, 0:4], in_=lg[:, 0:4], func=AF.Exp, bias=rt[:, 1:2], accum_out=rt[:, 2:3]),
                         reads=[b_lg, b_rt], writes=[b_junk, b_rt])
                    S.op(V, lambda e: e.tensor_scalar(out=oh[:], in0=oh[:], scalar1=1e30, scalar2=-1e30, op0=ALU.mult, op1=ALU.add),
                         reads=[b_oh], writes=[b_oh])
                    S.op(V, lambda e: e.tensor_tensor(out=ml[:].rearrange("p (g j) -> p g j", g=4),
                                                      in0=lg[:, 4:36].rearrange("p (g j) -> p g j", g=4),
                                                      in1=oh[:].unsqueeze(2).to_broadcast([128, 4, 8]), op=ALU.add),
                         reads=[b_lg, b_oh], writes=[b_ml])
                    S.op(V, lambda e: e.tensor_reduce(out=rt[:, 3:4], in_=ml[:], axis=AX.X, op=ALU.max), reads=[b_ml], writes=[b_rt])
                    S.op(V, lambda e: e.tensor_scalar(out=ml2[:], in0=ml[:], scalar1=rt[:, 3:4], scalar2=-1e30, op0=ALU.is_ge, op1=ALU.mult),
                         reads=[b_ml, b_rt], writes=[b_ml2])
                    S.op(V, lambda e: e.tensor_tensor(out=ml2[:], in0=ml2[:], in1=ml[:], op=ALU.add), reads=[b_ml2, b_ml], writes=[b_ml2])
                    S.op(V, lambda e: e.tensor_reduce(out=rt[:, 4:5], in_=ml2[:], axis=AX.X, op=ALU.max), reads=[b_ml2], writes=[b_rt])
                    S.op(V, lambda e: e.tensor_scalar(out=msk[:], in0=ml[:], scalar1=rt[:, 4:5], scalar2=None, op0=ALU.is_ge),
                         reads=[b_ml, b_rt], writes=[b_msk])
                    S.op(V, lambda e: e.tensor_scalar(out=rt[:, 5:6], in0=rt[:, 3:4], scalar1=-1.0, scalar2=None, op0=ALU.mult),
                         reads=[b_rt], writes=[b_rt])
                    S.op(V, lambda e: e.tensor_scalar(out=ml2[:], in0=ml[:], scalar1=rt[:, 5:6], scalar2=-80.0, op0=ALU.add, op1=ALU.max),
                         reads=[b_ml, b_rt], writes=[b_ml2])
                    S.op(A, lambda e: e.activation(out=ml2[:], in_=ml2[:], func=AF.Exp), reads=[b_ml2], writes=[b_ml2])
                    S.op(V, lambda e: e.tensor_tensor(out=ml2[:], in0=ml2[:], in1=msk[:], op=ALU.mult), reads=[b_ml2, b_msk], writes=[b_ml2])
                    S.op(V, lambda e: e.tensor_reduce(out=rt[:, 6:7], in_=ml2[:], axis=AX.X, op=ALU.add), reads=[b_ml2], writes=[b_rt])
                    S.op(V, lambda e: e.tensor_tensor(out=rt[:, 7:8], in0=rt[:, 6:7], in1=rt[:, 2:3], op=ALU.mult), reads=[b_rt], writes=[b_rt])
                    S.op(V, lambda e: e.reciprocal(out=rt[:, 8:9], in_=rt[:, 7:8]), reads=[b_rt], writes=[b_rt])
                    S.op(V, lambda e: e.tensor_scalar(out=Gt[:, c, :], in0=ml2[:], scalar1=rt[:, 8:9], scalar2=None, op0=ALU.mult),
                         reads=[b_ml2, b_rt], writes=[b_Gt[c]])
                    S.op(V, lambda e: e.tensor_copy(out=mskA[:, c, :], in_=msk[:]), reads=[b_msk], writes=[b_mskA])
                    S.op(V, lambda e: e.tensor_copy(out=mskb[:], in_=msk[:]), reads=[b_msk], writes=[b_mskb])
                    bk, b_bk = pp.next()
                    S.mm([(bk[:, 0:32], [(trisb[:], mskb[:])]), (bk[:, 32:64], [(onesb[:], mskb[:])])],
                         reads=[b_trisb, b_onesb, b_mskb], writes=[b_bk])
                    S.op(V, lambda e: e.tensor_tensor(out=posA[:, c, :], in0=bk[:, 0:32], in1=runcnt[:], op=ALU.add),
                         reads=[b_bk, b_runcnt], writes=[b_posA])
                    S.op(V, lambda e: e.tensor_tensor(out=runcnt[:], in0=bk[:, 32:64], in1=runcnt[:], op=ALU.add),
                         reads=[b_bk, b_runcnt], writes=[b_runcnt])


                for rnd in range(NCH + 2):
                    fns = []
                    if rnd < NCH:
                        fns.append((lambda i=rnd: BX1(i), 1))
                    if 0 <= rnd - 1 < NCH:
                        fns.append((lambda i=rnd - 1: BX2(i), 1))
                    if 0 <= rnd - 2 < NCH:
                        fns.append((lambda i=rnd - 2: BY(i), 2))
                    S.weave(fns)

            if stage == 2:
                S.wait_all(G, b_y)
                return nc
            S.barrier()

            pd = ExitStack()
            with pd:
                def ld(name, shape, src):
                    t, bb = sbt(pd, name, shape, F32)
                    S.dma(SP, t[:], src, writes=[bb])
                    return t, bb
                thr, b_thr = ld("thr", [128, 16], thr_d)
                biota, b_biota = ld("biota", [128, NBLK], biota_d[:, 0:NBLK])
                pidx, b_pidx = ld("pidx", [128, 1], pidx_d)
                cmp1, b_cmp1 = sbt(pd, "cmp1", [128, NE, 16], F32)
                cmp2, b_cmp2 = sbt(pd, "cmp2", [128, NBLK, NE], F32)
                blocks, b_blocks = sbt(pd, "blocks", [128, NE], F32)
                pendb, b_pendb = sbt(pd, "pendb", [128, NE], F32)
                pstart, b_pstart = sbt(pd, "pstart", [128, NE], F32)
                ones32, b_ones32 = sbt(pd, "ones32", [128, NE], F32)
                ebf, b_ebf = sbt(pd, "ebf", [128, NBLK], F32)
                inv, b_inv = sbt(pd, "inv", [128, NBLK], F32)
                idxWf, b_idxWf = sbt(pd, "idxWf", [128, NBLK, 2], F32)
                incl, b_incl = sbt(pd, "incl", [128, NCH, NE], F32)
                m0, b_m0 = sbt(pd, "m0", [128, NCH, NE], F32)
                m1, b_m1 = sbt(pd, "m1", [128, NCH, NE], F32)
                dfull, b_dfull = sbt(pd, "dfull", [128, NCH, NE], F32)
                tmpd, b_tmpd = sbt(pd, "tmpd", [128, NCH, NE], F32)
                destf, b_destf = sbt(pd, "destf", [128, NCH, 2], F32)
                S.op(V, lambda e: e.tensor_tensor(out=cmp1[:], in0=runcnt[:].unsqueeze(2).to_broadcast([128, NE, 16]),
                                                  in1=thr[:].unsqueeze(1).to_broadcast([128, NE, 16]), op=ALU.is_gt),
                     reads=[b_runcnt, b_thr], writes=[b_cmp1])
                S.op(V, lambda e: e.tensor_reduce(out=blocks[:], in_=cmp1[:], axis=AX.X, op=ALU.add), reads=[b_cmp1], writes=[b_blocks])
                S.op(V, lambda e: e.memset(ones32[:], 1.0), writes=[b_ones32])
                S.op(V, lambda e: e.tensor_tensor_scan(out=pendb[:], data0=ones32[:], data1=blocks[:], initial=0.0,
                                                       op0=ALU.mult, op1=ALU.add),
                     reads=[b_ones32, b_blocks], writes=[b_pendb])
                S.op(V, lambda e: e.tensor_tensor(out=pstart[:], in0=pendb[:], in1=blocks[:], op=ALU.subtract),
                     reads=[b_pendb, b_blocks], writes=[b_pstart])
                S.op(V, lambda e: e.tensor_scalar(out=pstart[:], in0=pstart[:], scalar1=float(BS), scalar2=None, op0=ALU.mult),
                     reads=[b_pstart], writes=[b_pstart])
                S.op(V, lambda e: e.tensor_tensor(out=cmp2[:], in0=pendb[:].unsqueeze(1).to_broadcast([128, NBLK, NE]),
                                                  in1=biota[:].unsqueeze(2).to_broadcast([128, NBLK, NE]), op=ALU.is_le),
                     reads=[b_pendb, b_biota], writes=[b_cmp2])
                S.op(V, lambda e: e.tensor_reduce(out=ebf[:], in_=cmp2[:], axis=AX.X, op=ALU.add), reads=[b_cmp2], writes=[b_ebf])
                S.op(V, lambda e: e.tensor_scalar(out=ebf[:], in0=ebf[:], scalar1=float(NE - 1), scalar2=256.0, op0=ALU.min, op1=ALU.mult),
                     reads=[b_ebf], writes=[b_ebf])
                S.op(V, lambda e: e.tensor_scalar(out=inv[:], in0=biota[:], scalar1=pendb[:, NE - 1:NE], scalar2=1.0e6,
                                                  op0=ALU.is_ge, op1=ALU.mult), reads=[b_biota, b_pendb], writes=[b_inv])
                S.op(V, lambda e: e.tensor_tensor(out=ebf[:], in0=ebf[:], in1=inv[:], op=ALU.add), reads=[b_ebf, b_inv], writes=[b_ebf])
                S.op(V, lambda e: e.tensor_scalar(out=idxWf[:, :, 0], in0=ebf[:], scalar1=pidx[:, 0:1], scalar2=None, op0=ALU.add),
                     reads=[b_ebf, b_pidx], writes=[b_idxWf])
                S.op(V, lambda e: e.tensor_scalar(out=idxWf[:, :, 1], in0=idxWf[:, :, 0], scalar1=128.0, scalar2=None, op0=ALU.add),
                     reads=[b_idxWf], writes=[b_idxWf])
                S.op(V, lambda e: e.tensor_copy(out=idxWi[:].rearrange("p a b -> p (a b)"), in_=idxWf[:].rearrange("p a b -> p (a b)")),
                     reads=[b_idxWf], writes=[b_idxWi])
                for c in range(NCH):
                    S.op(V, lambda e: e.tensor_tensor_scan(out=incl[:, c, :], data0=ones32[:], data1=mskA[:, c, :], initial=0.0,
                                                           op0=ALU.mult, op1=ALU.add),
                         reads=[b_ones32, b_mskA], writes=[b_incl])
                fl = lambda t: t[:].rearrange("p a b -> p (a b)")
                S.op(V, lambda e: e.tensor_scalar(out=fl(m0), in0=fl(incl), scalar1=1.0, scalar2=None, op0=ALU.is_equal),
                     reads=[b_incl], writes=[b_m0])
                S.op(V, lambda e: e.tensor_tensor(out=fl(m0), in0=fl(m0), in1=fl(mskA), op=ALU.mult), reads=[b_m0, b_mskA], writes=[b_m0])
                S.op(V, lambda e: e.tensor_scalar(out=fl(m1), in0=fl(incl), scalar1=2.0, scalar2=None, op0=ALU.is_equal),
                     reads=[b_incl], writes=[b_m1])
                S.op(V, lambda e: e.tensor_tensor(out=fl(m1), in0=fl(m1), in1=fl(mskA), op=ALU.mult), reads=[b_m1, b_mskA], writes=[b_m1])
                S.op(V, lambda e: e.tensor_tensor(out=dfull[:], in0=posA[:], in1=pstart[:].unsqueeze(1).to_broadcast([128, NCH, NE]), op=ALU.add),
                     reads=[b_posA, b_pstart], writes=[b_dfull])
                for k, mk, b_mk in ((0, m0, b_m0), (1, m1, b_m1)):
                    S.op(V, lambda e: e.tensor_tensor(out=fl(tmpd), in0=fl(mk), in1=fl(dfull), op=ALU.mult), reads=[b_mk, b_dfull], writes=[b_tmpd])
                    S.op(V, lambda e: e.tensor_reduce(out=destf[:, :, k], in_=tmpd[:], axis=AX.X, op=ALU.add), reads=[b_tmpd], writes=[b_destf])
                    S.op(V, lambda e: e.tensor_tensor(out=fl(tmpd), in0=fl(mk), in1=Gt[:].rearrange("p a b -> p (a b)"), op=ALU.mult),
                         reads=[b_mk] + b_Gt, writes=[b_tmpd])
                    S.op(V, lambda e: e.tensor_reduce(out=gate2[:, :, k], in_=tmpd[:], axis=AX.X, op=ALU.add), reads=[b_tmpd], writes=[b_gate2])
                S.op(V, lambda e: e.tensor_copy(out=desti[:].rearrange("p a b -> p (a b)"), in_=destf[:].rearrange("p a b -> p (a b)")),
                     reads=[b_destf], writes=[b_desti])
                b_scat = []
                for c in range(NCH):
                    for k in range(2):
                        bb = Buf()
                        S.idma(G, reads=[b_xn3B[c], b_desti], writes=[bb], out=xbuf[:, :],
                               out_offset=bass.IndirectOffsetOnAxis(ap=desti[:, c, k:k + 1], axis=0),
                               in_=xn3B[:, c, :], in_offset=None)
                        b_scat.append(bb)
            S.barrier()

            pc = ExitStack()
            with pc:
                w1b = [sbt(pc, "w1b%d" % i, [128, 8, 512], BF16) for i in range(2)]
                w3b = [sbt(pc, "w3b%d" % i, [128, 8, 512], BF16) for i in range(2)]
                w2b = [sbt(pc, "w2b%d" % i, [128, 4, D], BF16) for i in range(2)]
                xb_r = [sbt(pc, "xb%d" % i, [128, NT, D], BF16) for i in range(2)]
                xbT_r = [sbt(pc, "xbT%d" % i, [128, 8, BS], BF16) for i in range(2)]
                hgT = [sbt(pc, "hgT%d" % i, [128, 4, BS], BF16) for i in range(2)]
                sl_t = [sbt(pc, "silu%d" % i, [128, 512], F32) for i in range(2)]
                ob_r = [sbt(pc, "ob%d" % i, [128, NT, D], F32) for i in range(1)]
                rg = [[sbt(pc, "rg%d_%d" % (i, k), [128, D], F32) for k in range(2)] for i in range(1)]
                yo = [sbt(pc, "yo%d" % i, [128, D], F32) for i in range(2)]
                st3, b_st3 = sbt(pc, "st3c", [128, 4], F32)
                pp = Pool(banks[0:7])
                bk_tr, b_tr = banks[7]
                trv = bk_tr[:].bitcast(BF16)
                b_ost = [Buf() for _ in range(NBLK)]

                for p_ in range(2):
                    for wt__ in (w1b[p_], w3b[p_], w2b[p_]):
                        S.op(V, lambda e_: e_.memset(wt__[0][:].rearrange("p a b -> p (a b)"), 0.0), writes=[wt__[1]])
                bc_reg = G.eng.to_reg(NE * 256 - 1)

                def load_w(bi):
                    p = bi % 2
                    for j in range(2):
                        io = bass.IndirectOffsetOnAxis(ap=idxWi[:, bi, j:j + 1], axis=0)
                        S.idma(G, reads=[b_idxWi], writes=[w1b[p][1]], out=w1b[p][0][:, 4 * j:4 * j + 4, :].rearrange("p a b -> p (a b)"),
                               out_offset=None, in_=w1_d[:, :], in_offset=io, bounds_check=bc_reg, oob_is_err=False)
                        S.idma(G, reads=[b_idxWi], writes=[w3b[p][1]], out=w3b[p][0][:, 4 * j:4 * j + 4, :].rearrange("p a b -> p (a b)"),
                               out_offset=None, in_=w3_d[:, :], in_offset=io, bounds_check=bc_reg, oob_is_err=False)
                        S.idma(G, reads=[b_idxWi], writes=[w2b[p][1]], out=w2b[p][0][:, 2 * j:2 * j + 2, :].rearrange("p a b -> p (a b)"),
                               out_offset=None, in_=w2_d[:, :], in_offset=io, bounds_check=bc_reg, oob_is_err=False)

                def load_x(bi):
                    t, bb = xb_r[bi % 2]
                    S.dma(SP, t[:], xbuf[bi * BS:(bi + 1) * BS, :].rearrange("(t p) d -> p t d", p=128), reads=b_scat, writes=[bb])

                load_w(0)
                load_x(0)
                for bi in range(NBLK):
                    p = bi % 2
                    if bi + 1 < NBLK:
                        load_w(bi + 1)
                        load_x(bi + 1)
                    w1t, b_w1 = w1b[p]
                    w3t, b_w3 = w3b[p]
                    w2t, b_w2 = w2b[p]
                    xb_t, b_xb = xb_r[p]
                    xbT, b_xbT = xbT_r[p]
                    hg_t, b_hg = hgT[p]
                    ob, b_ob = ob_r[0]
                    for t in range(NT):
                        S.tr([(trv[:, k * 128:(k + 1) * 128], xb_t[:, t, k * 128:(k + 1) * 128]) for k in range(8)], identb[:],
                             reads=[b_xb, b_identb], writes=[b_tr])
                        S.op(A, lambda e_: e_.copy(out=xbT[:, :, t * 128:(t + 1) * 128], in_=trv.rearrange("p (k s) -> p k s", k=8)),
                             reads=[b_tr], writes=[b_xbT])
                    for fp_ in range(2):
                        bk1, b_bk1 = pp.next()
                        bk3, b_bk3 = pp.next()
                        S.mm([(bk1[:, q_ * BS:(q_ + 1) * BS],
                               [(w1t[:, kc, (2 * fp_ + q_) * 128:(2 * fp_ + q_ + 1) * 128], xbT[:, kc, :]) for kc in range(8)])
                              for q_ in range(2)], reads=[b_w1, b_xbT], writes=[b_bk1])
                        S.mm([(bk3[:, q_ * BS:(q_ + 1) * BS],
                               [(w3t[:, kc, (2 * fp_ + q_) * 128:(2 * fp_ + q_ + 1) * 128], xbT[:, kc, :]) for kc in range(8)])
                              for q_ in range(2)], reads=[b_w3, b_xbT], writes=[b_bk3])
                        s_t, b_sl = sl_t[fp_]
                        S.op(A, lambda e_: e_.activation(out=s_t[:], in_=bk1[:], func=AF.Silu), reads=[b_bk1], writes=[b_sl])
                        S.op(V, lambda e_: e_.tensor_tensor(out=hg_t[:, 2 * fp_:2 * fp_ + 2, :].rearrange("p a b -> p (a b)"), in0=bk3[:], in1=s_t[:],
                                                            op=ALU.mult), reads=[b_bk3, b_sl], writes=[b_hg])
                    for t in range(NT):
                        for half in range(2):
                            bk, b_bk = pp.next()
                            sl = slice(half * 512, (half + 1) * 512)
                            S.mm([(bk[:], [(hg_t[:, fc, t * 128:(t + 1) * 128], w2t[:, fc, sl]) for fc in range(4)])],
                                 reads=[b_hg, b_w2], writes=[b_bk])
                            if half == 0:
                                S.op(A, lambda e_: e_.copy(out=ob[:, t, sl], in_=bk[:]), reads=[b_bk], writes=[b_ob])
                            else:
                                S.op(V, lambda e_: e_.tensor_copy(out=ob[:, t, sl], in_=bk[:]), reads=[b_bk], writes=[b_ob])
                    S.dma(SP, obuf[bi * BS:(bi + 1) * BS, :].rearrange("(t p) d -> p t d", p=128), ob[:], reads=[b_ob], writes=[b_ost[bi]])
                for c in range(NCH):
                    for k in range(2):
                        r_t, b_r = rg[0][k]
                        S.idma(G, reads=b_ost + [b_desti], writes=[b_r], out=r_t[:, :], out_offset=None, in_=obuf[:, :],
                               in_offset=bass.IndirectOffsetOnAxis(ap=desti[:, c, k:k + 1], axis=0))
                        S.op(V, lambda e_: e_.scalar_tensor_tensor(out=acc[:, c, :], in0=r_t[:], scalar=gate2[:, c, k:k + 1],
                                                                   in1=acc[:, c, :], op0=ALU.mult, op1=ALU.add),
                             reads=[b_r, b_gate2, b_acc[c]], writes=[b_acc[c]])
                    yo_t, b_yo = yo[c % 2]
                    rms_rstd(A, acc[:, c, :], b_acc[c], junk[:], b_junk, st3, b_st3, float(D))
                    S.op(V, lambda e_: e_.scalar_tensor_tensor(out=yo_t[:], in0=acc[:, c, :], scalar=st3[:, 2:3], in1=gf[:],
                                                               op0=ALU.mult, op1=ALU.mult),
                         reads=[b_acc[c], b_st3, b_gf], writes=[b_yo])
                    S.dma(SP, y_d[c * 128:(c + 1) * 128, :], yo_t[:], reads=[b_yo], writes=[b_y[c]])
                S.wait_all(SP, b_y)
    return nc


def make_in_maps(inputs):
    f = lambda a: np.ascontiguousarray(a, dtype=np.float32)
    x = f(inputs["x"])
    mem = f(inputs["mem"])

    def bt(v, n=128):
        v = f(v).reshape(1, -1)
        return np.ascontiguousarray(np.broadcast_to(v, (n, v.shape[1])))

    b_if = f(inputs["b_if"][0])
    conv_w = f(inputs["conv_w"][0])
    conv_b = f(inputs["conv_b"][0])
    w_s = f(inputs["w_s"][0])
    b_s = f(inputs["b_s"][0])
    shared = {
        "w_in": f(inputs["w_in"][0]),
        "w_out": f(inputs["w_out"][0]),
        "w_xq": f(inputs["w_xq"][0]),
        "w_xkv": f(inputs["w_xkv"][0]),
        "w_xo": f(inputs["w_xo"][0]),
        "w_r": f(np.concatenate([inputs["w_rg"][0], inputs["w_re"][0]], axis=1)),
        "w1r": f(f(inputs["w1"][0]).reshape(NE, 2, 4, 128, 512).transpose(0, 1, 3, 2, 4).reshape(NE * 256, 2048)),
        "w3r": f(f(inputs["w3"][0]).reshape(NE, 2, 4, 128, 512).transpose(0, 1, 3, 2, 4).reshape(NE * 256, 2048)),
        "w2r": f(f(inputs["w2"][0]).reshape(NE, 2, 2, 128, 1024).transpose(0, 1, 3, 2, 4).reshape(NE * 256, 2048)),
        "thr16": bt(np.arange(16, dtype=np.float32) * 256.0),
        "biota": bt(np.arange(64, dtype=np.float32)),
        "pidx": np.arange(128, dtype=np.float32).reshape(128, 1),
        "ident": np.eye(128, dtype=np.float32),
        "tri": np.triu(np.ones((128, 128), dtype=np.float32)),
        "gmix_t": bt(inputs["norm_mix_g"][0]),
        "gx_t": bt(inputs["norm_x_g"][0]),
        "gmem_t": bt(inputs["norm_mem_g"][0]),
        "gmoe_t": bt(inputs["norm_moe_g"][0]),
        "gf_t": bt(inputs["norm_f_g"]),
        "mhg_t": bt(inputs["mh_norm_g"][0]),
        "sgug_t": bt(inputs["sgu_norm_g"][0]),
        "bs_t": f(np.repeat(b_s.T[:, :, None], 128, axis=2).reshape(128, 1024)),
        "bgate_t": bt(inputs["b_gate"][0]),
        "bif_t": bt(b_if),
        "br_t": bt(np.concatenate([inputs["b_rg"][0], inputs["b_re"][0]])),
        "convw_t": f(conv_w.reshape(4, 16, 128).transpose(2, 1, 0)),
        "convb_t": f(conv_b.reshape(16, 128).T),
        "wsT": f(w_s.transpose(2, 0, 1)),
    }
    maps = []
    for c in range(NCORES):
        b, j = divmod(c, 4)
        npad = NPRE - NCH * j
        xs = np.zeros((NIT * 128, D), dtype=np.float32)
        xs[npad * 128:] = x[b, 0:(j + 1) * NCH * 128]
        pm = np.zeros((128, NIT), dtype=np.float32)
        pm[:, npad:] = 1.0
        m = dict(shared)
        m["xseq"] = xs
        m["pmask"] = pm
        m["mem_b"] = mem[b]
        maps.append(m)
    return maps


def assemble(res):
    out = np.zeros((2, 8192, D), dtype=np.float32)
    for c in range(NCORES):
        b, j = divmod(c, 4)
        out[b, j * NCH * 128:(j + 1) * NCH * 128] = res.results[c]["y"]
    return out


def build2(stage=3):
    rec = []
    build(stage, None, rec)
    return build(stage, rec)


def kernel(**inputs):
    nc = build2(3)
    maps = make_in_maps(inputs)
    res = run_bass_kernel_spmd(nc, maps, core_ids=list(range(NCORES)))
    return assemble(res)
```

```python
import numpy as np
import concourse.bass as bass
import concourse.mybir as mybir
from concourse.bass_utils import run_bass_kernel_spmd
from contextlib import ExitStack

F32 = mybir.dt.float32
BF16 = mybir.dt.bfloat16
ALU = mybir.AluOpType
AF = mybir.ActivationFunctionType
AX = mybir.AxisListType

NDS = 24
NCORES = 8
D = 1024
NCH = 16
NPRE = 48
NIT = NCH + NPRE
IN_DIM = 8200
NE = 32
EPS = 1e-6
RELAXED_SAME_ENGINE = False


class Buf:
    __slots__ = ("w", "r")

    def __init__(self):
        self.w = None
        self.r = {}


class Q:
    def __init__(self, S, name, eng):
        self.eng = eng
        self.name = name
        self.sem = S.new_sem("q_" + name)
        self.count = 0
        self.waited = {}


class Sched:
    def __init__(self, nc, es):
        self.nc = nc
        self.es = es
        self.T = Q(self, "pe", nc.tensor)
        self.V = Q(self, "dve", nc.vector)
        self.A = Q(self, "act", nc.scalar)
        self.G = Q(self, "pool", nc.gpsimd)
        self.SP = Q(self, "sp", nc.sync)
        self.dma_sems = [self.new_sem("dma%d" % i) for i in range(NDS)]
        self.dma_uses = [0] * NDS
        self.dma_next = 0
        self.dma_next_g = 0
        self._wv = None
        self._force = None
        self.pools = []
        self.noswitch = False

    def weave(self, fns):
        import threading
        if len(fns) == 1:
            fns[0][0]()
            return
        n = len(fns)
        evs = [threading.Event() for _ in range(n)]
        alive = [True] * n
        errs = []
        done = threading.Event()
        st = {"cur": 0, "cnt": 0}
        quanta = [q for _, q in fns]

        def next_alive(k):
            for d in range(1, n + 1):
                j = (k + d) % n
                if alive[j] and j != k:
                    return j
            return None

        def runner(k, fn):
            evs[k].wait()
            try:
                fn()
            except BaseException as e:
                errs.append(e)
            for pl in self.pools:
                pl.release_mine()
            alive[k] = False
            j = next_alive(k)
            if j is None:
                done.set()
            else:
                st["cur"] = j
                st["cnt"] = 0
                evs[j].set()

        def switch():
            k = st["cur"]
            st["cnt"] += 1
            if st["cnt"] < quanta[k]:
                return
            j = next_alive(k)
            if j is None:
                st["cnt"] = 0
                return
            st["cur"] = j
            st["cnt"] = 0
            evs[k].clear()
            evs[j].set()
            evs[k].wait()

        def force():
            k = st["cur"]
            j = next_alive(k)
            if j is None:
                raise RuntimeError("weave: stream blocked on a PSUM bank with no other stream alive")
            st["cur"] = j
            st["cnt"] = 0
            evs[k].clear()
            evs[j].set()
            evs[k].wait()

        self._wv = switch
        self._force = force
        ths = [threading.Thread(target=runner, args=(k, fn)) for k, (fn, _) in enumerate(fns)]
        for t in ths:
            t.start()
        evs[0].set()
        done.wait()
        for t in ths:
            t.join()
        self._wv = None
        self._force = None
        for pl in self.pools:
            pl.free = list(range(len(pl.items)))
            pl.owned = {}
        if errs:
            raise errs[0]

    def yield_now(self):
        self._force()

    def _sw(self):
        if self._wv is not None and not self.noswitch:
            self._wv()

    def new_sem(self, name):
        return self.es.enter_context(self.nc.semaphore(name))

    def _wait(self, q, tok):
        sem, val = tok
        k = id(sem)
        if q.waited.get(k, 0) >= val:
            return
        q.eng.wait_ge(sem, val)
        q.waited[k] = val

    def _deps(self, q, reads, writes):
        best = {}

        def add(tok):
            k = id(tok[0])
            if k not in best or best[k][1] < tok[1]:
                best[k] = tok

        for b in reads:
            if b.w is not None:
                add(b.w)
        relaxed = RELAXED_SAME_ENGINE and q in (self.T, self.V, self.A)
        for b in writes:
            if b.w is not None and not (relaxed and b.w[0] is q.sem):
                add(b.w)
            for t in b.r.values():
                if not (relaxed and t[0] is q.sem):
                    add(t)
        for tok in best.values():
            if RELAXED_SAME_ENGINE and q is self.T and tok[0] is q.sem:
                continue
            self._wait(q, tok)

    def _commit(self, tok, reads, writes):
        for b in writes:
            b.w = tok
            b.r = {}
        k = id(tok[0])
        for b in reads:
            if b not in writes:
                b.r[k] = tok

    def op(self, q, fn, reads=(), writes=()):
        self._deps(q, reads, writes)
        ins = fn(q.eng)
        q.count += 1
        ins.then_inc(q.sem, 1)
        self._commit((q.sem, q.count), reads, writes)
        self._sw()

    def mm(self, groups, reads=(), writes=()):
        q = self.T
        self._deps(q, reads, writes)
        ins = None
        for out, pairs in groups:
            n = len(pairs)
            for i, (l, r) in enumerate(pairs):
                ins = q.eng.matmul(out, lhsT=l, rhs=r, start=(i == 0), stop=(i == n - 1))
        q.count += 1
        ins.then_inc(q.sem, 1)
        self._commit((q.sem, q.count), reads, writes)
        self._sw()

    def tr(self, items, ident, reads=(), writes=()):
        q = self.T
        self._deps(q, reads, writes)
        ins = None
        for out, in_ in items:
            ins = q.eng.transpose(out=out, in_=in_, identity=ident)
        q.count += 1
        ins.then_inc(q.sem, 1)
        self._commit((q.sem, q.count), reads, writes)
        self._sw()

    def dma(self, q, out, in_, reads=(), writes=()):
        self._deps(q, reads, writes)
        half = NDS // 2
        if q is self.G:
            i = half + self.dma_next_g
            self.dma_next_g = (self.dma_next_g + 1) % half
        else:
            i = self.dma_next
            self.dma_next = (self.dma_next + 1) % half
        sem = self.dma_sems[i]
        if self.dma_uses[i] > 0:
            self._wait(q, (sem, 16 * self.dma_uses[i]))
        q.eng.dma_start(out=out, in_=in_).then_inc(sem, 16)
        self.dma_uses[i] += 1
        tok = (sem, 16 * self.dma_uses[i])
        self._commit(tok, reads, writes)
        self._sw()

    def idma(self, q, reads=(), writes=(), **kw):
        self._deps(q, reads, writes)
        half = NDS // 2
        i = half + self.dma_next_g
        self.dma_next_g = (self.dma_next_g + 1) % half
        sem = self.dma_sems[i]
        if self.dma_uses[i] > 0:
            self._wait(q, (sem, 16 * self.dma_uses[i]))
        q.eng.indirect_dma_start(**kw).then_inc(sem, 16)
        self.dma_uses[i] += 1
        self._commit((sem, 16 * self.dma_uses[i]), reads, writes)
        self._sw()

    def barrier(self):
        qs = [self.T, self.V, self.A, self.G, self.SP]
        toks = [(q.sem, q.count) for q in qs if q.count > 0]
        toks += [(self.dma_sems[i], 16 * self.dma_uses[i]) for i in range(NDS) if self.dma_uses[i] > 0]
        for q in qs:
            for tok in toks:
                if tok[0] is q.sem:
                    continue
                self._wait(q, tok)

    def wait_all(self, q, bufs):
        for b in bufs:
            if b.w is not None:
                self._wait(q, b.w)


class Pool:
    def __init__(self, items, S=None, hold=1):
        self.items = items
        self.i = 0
        self.S = S
        self.hold = hold
        self.free = list(range(len(items)))
        self.owned = {}
        if S is not None:
            S.pools.append(self)

    def next(self):
        S = self.S
        if S is None or S._wv is None:
            it = self.items[self.i]
            self.i = (self.i + 1) % len(self.items)
            return it
        import threading
        tid = threading.get_ident()
        mine = self.owned.setdefault(tid, [])
        while len(mine) >= self.hold:
            self.free.append(mine.pop(0))
        while not self.free:
            S.yield_now()
        k = self.free.pop(0)
        mine.append(k)
        return self.items[k]

    def release_mine(self):
        import threading
        mine = self.owned.get(threading.get_ident(), [])
        while mine:
            self.free.append(mine.pop(0))


def build(stage=3, sseq_in=None, srec=None):
    if srec is None:
        srec = []
    nc = bass.Bass("TRN2", target_bir_lowering=False)

    def din(name, shape):
        return nc.dram_tensor(name, list(shape), F32, kind="ExternalInput").ap()

    xseq = din("xseq", [NIT * 128, D])
    pmask_d = din("pmask", [128, NIT])
    mem_d = din("mem_b", [256, D])
    w_in_d = din("w_in", [D, IN_DIM])
    w_out_d = din("w_out", [D, D])
    w_xq_d = din("w_xq", [D, D])
    w_xkv_d = din("w_xkv", [D, 2 * D])
    w_xo_d = din("w_xo", [D, D])
    w_r_d = din("w_r", [D, 36])
    w1_d = din("w1r", [NE * 2 * 128, 2048])
    w3_d = din("w3r", [NE * 2 * 128, 2048])
    w2_d = din("w2r", [NE * 2 * 128, 2048])
    thr_d = din("thr16", [128, 16])
    biota_d = din("biota", [128, 64])
    pidx_d = din("pidx", [128, 1])
    ident_d = din("ident", [128, 128])
    tri_d = din("tri", [128, 128])
    gmix_d = din("gmix_t", [128, D])
    gx_d = din("gx_t", [128, D])
    gmem_d = din("gmem_t", [128, D])
    gmoe_d = din("gmoe_t", [128, D])
    gf_d = din("gf_t", [128, D])
    mhg_d = din("mhg_t", [128, D])
    sgug_d = din("sgug_t", [128, D])
    bs_d = din("bs_t", [128, D])
    bgate_d = din("bgate_t", [128, 2 * D])
    bif_d = din("bif_t", [128, 8])
    br_d = din("br_t", [128, 36])
    convw_d = din("convw_t", [128, 16, 4])
    convb_d = din("convb_t", [128, 16])
    wsT_d = din("wsT", [128, 8, 128])
    y_d = nc.dram_tensor("y", [NCH * 128, D], F32, kind="ExternalOutput").ap()
    x1s = nc.dram_tensor("x1s", [NCH * 128, D], F32, kind="Internal").ap()
    wst = nc.dram_tensor("wst", [22, 128, 8, 512], BF16, kind="Internal").ap()
    BS = 256
    NT = BS // 128
    NBLK = (NCH * 128 * 2) // BS + NE
    xbuf = nc.dram_tensor("xbuf", [NBLK * BS, D], BF16, kind="Internal").ap()
    obuf = nc.dram_tensor("obuf", [NBLK * BS, D], F32, kind="Internal").ap()
    I32 = mybir.dt.int32

    es = ExitStack()
    with es:
        S = Sched(nc, es)
        T, V, A, G, SP = S.T, S.V, S.A, S.G, S.SP

        def sbt(stack, name, shape, dt):
            return stack.enter_context(nc.sbuf_tensor("s_" + name, list(shape), dt)), Buf()

        def pst(stack, name, shape, dt):
            return stack.enter_context(nc.psum_tensor(name, list(shape), dt)), Buf()

        identf, b_identf = sbt(es, "identf", [128, 128], F32)
        identb, b_identb = sbt(es, "identb", [128, 128], BF16)
        trif, b_trif = sbt(es, "trif", [128, 128], F32)
        trib, b_trib = sbt(es, "trib", [128, 128], BF16)
        onesf, b_onesf = sbt(es, "onesf", [128, 128], F32)
        S.dma(SP, identf[:], ident_d, writes=[b_identf])
        S.dma(SP, trif[:], tri_d, writes=[b_trif])
        S.op(V, lambda e: e.tensor_copy(out=identb[:], in_=identf[:]), reads=[b_identf], writes=[b_identb])
        S.op(V, lambda e: e.tensor_copy(out=trib[:], in_=trif[:]), reads=[b_trif], writes=[b_trib])
        S.op(V, lambda e: e.memset(onesf[:], 1.0), writes=[b_onesf])
        epsc, b_epsc = sbt(es, "epsc", [128, 1], F32)
        S.op(V, lambda e: e.memset(epsc[:], EPS), writes=[b_epsc])

        banks = []
        for i in range(8):
            t, b = pst(es, "bank%d" % i, [128, 512], F32)
            banks.append((t, b))

        def rms_rstd(q_sq, xin_ap, b_xin, junk, b_junk, st, b_st, n):
            S.op(A, lambda e: e.activation(out=junk, in_=xin_ap, func=AF.Square, accum_out=st[:, 0:1]),
                 reads=[b_xin], writes=[b_junk, b_st])
            S.op(A, lambda e: e.activation(out=st[:, 3:4], in_=st[:, 0:1], func=AF.Sqrt, scale=1.0 / n, bias=epsc[:, 0:1]),
                 reads=[b_st, b_epsc], writes=[b_st])
            S.op(V, lambda e: e.reciprocal(out=st[:, 2:3], in_=st[:, 3:4]), reads=[b_st], writes=[b_st])

        pa = ExitStack()
        with pa:
            w_in_v = w_in_d.rearrange("(k p) n -> p k n", p=128)
            Wk, b_wk = sbt(pa, "Wk", [128, 8, 1024], BF16)
            Wv, b_wv = sbt(pa, "Wv", [128, 8, 1024], BF16)
            Wif, b_wif = sbt(pa, "Wif", [128, 8, 8], BF16)
            S.dma(G, Wk[:], w_in_v[:, :, 1024:2048], writes=[b_wk])
            S.dma(G, Wv[:], w_in_v[:, :, 2048:3072], writes=[b_wv])
            S.dma(G, Wif[:], w_in_v[:, :, 4096:4104], writes=[b_wif])
            GCOLS = [0, 512, 3072, 3584, 4104, 4616, 5128, 5640, 6152, 6664, 7176, 7688]
            NSG = 14
            w_out_v = w_out_d.rearrange("(k p) n -> p k n", p=128)
            NSB = 3
            wsb = [sbt(pa, "wsb%d" % i, [128, 8, 512], BF16) for i in range(NSB)]
            NSTG = NSG + 8
            b_wst = [Buf() for _ in range(NSTG)]
            w_xq_v = w_xq_d.rearrange("(k p) n -> p k n", p=128)
            w_xo_v = w_xo_d.rearrange("(k p) n -> p k n", p=128)
            w_xkv_v0 = w_xkv_d.rearrange("(k p) n -> p k n", p=128)
            S.op(V, lambda e: e.memset(wsb[0][0][:].rearrange("p a b -> p (a b)"), 0.0), writes=[wsb[0][1]])
            xbuf_z = xbuf.rearrange("(g p r) d -> g p (r d)", p=128, r=4)
            NZG = xbuf_z.shape[0]

            def zero_group(zg):
                S.dma(G, xbuf_z[zg], wsb[0][0][:].rearrange("p a b -> p (a b)"), reads=[wsb[0][1]], writes=[Buf()])

            def stage_group(g):
                sg_t, b_sg = wsb[1 + g % 2] if g < 13 else wsb[g % NSB]
                if g < 12:
                    src = w_in_v[:, :, GCOLS[g]:GCOLS[g] + 512]
                elif g < 14:
                    src = w_out_v[:, :, (g - 12) * 512:(g - 11) * 512]
                elif g < 16:
                    src = w_xq_v[:, :, (g - 14) * 512:(g - 13) * 512]
                elif g < 18:
                    src = w_xo_v[:, :, (g - 16) * 512:(g - 15) * 512]
                else:
                    src = w_xkv_v0[:, :, (g - 18) * 512:(g - 17) * 512]
                S.dma(G, sg_t[:], src, writes=[b_sg])
                S.dma(SP, wst[g], sg_t[:], reads=[b_sg], writes=[b_wst[g]])
            sstate = {"issued": 0, "used": 0}
            sseq = list(sseq_in) if sseq_in is not None else None

            def stream_issue(upto):
                while sstate["issued"] < min(upto, len(sseq)):
                    n = sstate["issued"]
                    t, b = wsb[n % NSB]
                    S.dma(SP, t[:], wst[sseq[n]], reads=[b_wst[sseq[n]]], writes=[b])
                    sstate["issued"] += 1

            def stream_next(g):
                n = sstate["used"]
                S.noswitch = True
                if sseq is None:
                    srec.append(g)
                    t, b = wsb[n % NSB]
                    S.dma(SP, t[:], wst[g], reads=[b_wst[g]], writes=[b])
                else:
                    assert sseq[n] == g, (n, g, sseq[n])
                    stream_issue(n + NSB)
                S.noswitch = False
                sstate["used"] += 1
                return wsb[n % NSB]

            def cload(name, shape, src, stack=pa):
                t, b = sbt(stack, name, shape, F32)
                S.dma(SP, t[:], src, writes=[b])
                return t, b

            gmix, b_gmix = cload("gmix", [128, D], gmix_d)
            mhg, b_mhg = cload("mhg", [128, D], mhg_d)
            sgug, b_sgug = cload("sgug", [128, D], sgug_d)
            bst, b_bst = cload("bst", [128, D], bs_d)
            bgate, b_bgate = cload("bgate", [128, 2 * D], bgate_d)
            bif, b_bif = cload("bif", [128, 8], bif_d)
            convw, b_convw = cload("convw", [128, 16, 4], convw_d)
            convb, b_convb = cload("convb", [128, 16], convb_d)
            pmask, b_pmask = cload("pmask", [128, NIT], pmask_d)
            wsTf, b_wsTf = cload("wsTf", [128, 8, 128], wsT_d)
            wsT, b_wsT = sbt(pa, "wsT", [128, 8, 128], BF16)
            for g in range(8):
                S.op(V, lambda e: e.tensor_tensor(out=wsT[:, g, :], in0=wsTf[:, g, :], in1=trif[:], op=ALU.mult),
                     reads=[b_wsTf, b_trif], writes=[b_wsT])

            R3 = 4
            RG = 3
            xt = [sbt(pa, "xt%d" % i, [128, D], F32) for i in range(R3)]
            xT = [sbt(pa, "xT%d" % i, [128, 8, 128], BF16) for i in range(R3)]
            st = [sbt(pa, "st%d" % i, [128, 4], F32) for i in range(2)]
            junk, b_junk = sbt(pa, "junk", [128, D], BF16)
            junk3, b_junk3 = sbt(pa, "junk3", [128, D], BF16)
            xn, b_xn = sbt(pa, "xn", [128, D], BF16)
            pre_t, b_pre = sbt(pa, "pre", [128, 16, 131], F32)
            halo, b_halo = sbt(pa, "halo", [128, 16, 3], F32)
            cacc, _ = sbt(pa, "cacc", [128, 16, 128], F32)
            b_cacc = [Buf() for _ in range(16)]
            qkT_r = [sbt(pa, "qkT%d" % i, [128, 16, 128], BF16) for i in range(2)]
            vaug_r = [sbt(pa, "vaug%d" % i, [128, 4, 257], F32) for i in range(2)]
            gif_r = [sbt(pa, "gif%d" % i, [128, 8], F32) for i in range(RG)]
            lsp_r = [sbt(pa, "lsp%d" % i, [128, 8], F32) for i in range(RG)]
            anb_r = [sbt(pa, "anb%d" % i, [128, 8], F32) for i in range(RG)]
            sm_r = [sbt(pa, "sm%d" % i, [4, 16], F32) for i in range(RG)]
            vw, _ = sbt(pa, "vw", [128, 4, 257], BF16)
            b_vw = [Buf() for _ in range(4)]
            eac, b_eac = sbt(pa, "eac", [128, 8], F32)
            bc, b_bc = sbt(pa, "bc", [128, 8], F32)
            mst, b_mst = sbt(pa, "mst", [4, 1], F32)
            rhs8, b_rhs8 = sbt(pa, "rhs8", [4, 8], F32)
            Cm, b_Cm0 = sbt(pa, "Cm", [128, 4, 2, 257], F32)
            b_Cm = [Buf() for _ in range(4)]
            Cb, b_Cb = sbt(pa, "Cb", [128, 2, 257], BF16)
            ktok, b_ktok = sbt(pa, "ktok", [128, 256], BF16)
            PT, b_PT = sbt(pa, "PT", [128, 128], BF16)
            ya_r = [sbt(pa, "ya%d" % i, [128, D], F32) for i in range(2)]
            hs_r = [sbt(pa, "hs%d" % i, [128, 16], F32) for i in range(2)]
            st3, b_st3 = sbt(pa, "st3", [128, 4], F32)
            so, b_so = sbt(pa, "so", [128, D], F32)
            ub, b_ub = sbt(pa, "ub", [128, D], F32)
            svb, b_svb = sbt(pa, "svb", [128, D], F32)
            svn, b_svn = sbt(pa, "svn", [128, D], BF16)
            gt, b_gt = sbt(pa, "gt", [128, 2 * D], F32)
            t1, b_t1 = sbt(pa, "t1", [128, 512], F32)
            t2, b_t2 = sbt(pa, "t2", [128, 512], F32)
            t3, b_t3 = sbt(pa, "t3", [128, 512], F32)
            yb, b_yb = sbt(pa, "yb", [128, D], F32)
            zb, b_zb = sbt(pa, "zb", [128, D], BF16)
            zT, b_zT = sbt(pa, "zT", [128, 8, 128], BF16)

            S.op(V, lambda e: e.memset(Cm[:].rearrange("p a b c -> p (a b c)"), 0.0), writes=b_Cm)
            S.op(V, lambda e: e.memset(mst[:], 0.0), writes=[b_mst])
            for i in range(2):
                S.op(V, lambda e: e.memset(vaug_r[i][0][:].rearrange("p a b -> p (a b)"), 1.0), writes=[vaug_r[i][1]])
            S.op(V, lambda e: e.memset(halo[:].rearrange("p a b -> p (a b)"), 0.0), writes=[b_halo])

            pp = Pool(banks[0:3], S, hold=1)
            trp = Pool(banks[3:4], S, hold=1)
            bk_tr, b_tr = banks[3]
            bk_s, b_s = banks[4]
            bk_num, b_num = banks[5]
            bk_kv0, b_kv0 = banks[6]
            bk_kv1, b_kv1 = banks[7]
            trv = bk_tr[:].bitcast(BF16)
            kv1_bf = bk_kv1[:].bitcast(BF16)

            b_x1s = [Buf() for _ in range(NCH)]

            def gelu_group(src_ps, b_src, dst, b_dst):
                S.op(A, lambda e: e.copy(out=t1[:], in_=src_ps), reads=[b_src], writes=[b_t1])
                S.op(A, lambda e: e.activation(out=t2[:], in_=t1[:], func=AF.Square), reads=[b_t1], writes=[b_t2])
                S.op(V, lambda e: e.tensor_scalar(out=t2[:], in0=t2[:], scalar1=0.044715, scalar2=1.0,
                                                  op0=ALU.mult, op1=ALU.add), reads=[b_t2], writes=[b_t2])
                S.op(V, lambda e: e.tensor_tensor(out=t3[:], in0=t2[:], in1=t1[:], op=ALU.mult),
                     reads=[b_t2, b_t1], writes=[b_t3])
                S.op(A, lambda e: e.activation(out=t3[:], in_=t3[:], func=AF.Sigmoid, scale=1.5957691216057308),
                     reads=[b_t3], writes=[b_t3])
                S.op(V, lambda e: e.tensor_tensor(out=dst, in0=t3[:], in1=t1[:], op=ALU.mult),
                     reads=[b_t3, b_t1], writes=[b_dst])

            def proj_w(xT_t, b_xTt, wt_, b_wt, c0, ncols):
                bk, b_bk = pp.next()
                S.mm([(bk[:, 0:ncols], [(xT_t[:, kc, :], wt_[:, kc, c0:c0 + ncols]) for kc in range(8)])],
                     reads=[b_xTt, b_wt], writes=[b_bk])
                return bk, b_bk

            def P1a(it):
                r2 = it % 2
                xt_t, b_xt = xt[it % R3]
                xT_t, b_xTt = xT[it % R3]
                st_t, b_stt = st[r2]
                gif, b_gif = gif_r[it % RG]
                lsp, b_lsp = lsp_r[it % RG]
                anb, b_anb = anb_r[it % RG]
                sm, b_sm = sm_r[it % RG]
                rms_rstd(A, xt_t[:], b_xt, junk[:], b_junk, st_t, b_stt, float(D))
                S.op(V, lambda e: e.scalar_tensor_tensor(out=xn[:], in0=xt_t[:], scalar=st_t[:, 2:3], in1=gmix[:],
                                                         op0=ALU.mult, op1=ALU.mult),
                     reads=[b_xt, b_stt, b_gmix], writes=[b_xn])
                trp.next()
                S.tr([(trv[:, k * 128:(k + 1) * 128], xn[:, k * 128:(k + 1) * 128]) for k in range(8)], identb[:],
                     reads=[b_xn, b_identb], writes=[b_tr])
                S.op(A, lambda e: e.copy(out=xT_t[:].rearrange("p k t -> p (k t)"), in_=trv), reads=[b_tr], writes=[b_xTt])
                trp.release_mine()
                bk, b_bk = proj_w(xT_t, b_xTt, Wif, b_wif, 0, 8)
                S.op(V, lambda e: e.tensor_tensor(out=gif[:], in0=bk[:, 0:8], in1=bif[:], op=ALU.add),
                     reads=[b_bk, b_bif], writes=[b_gif])
                S.op(A, lambda e: e.activation(out=lsp[:, 0:4], in_=gif[:, 4:8], func=AF.Exp, scale=-1.0),
                     reads=[b_gif], writes=[b_lsp])
                S.op(A, lambda e: e.activation(out=lsp[:, 4:8], in_=lsp[:, 0:4], func=AF.Ln, bias=1.0),
                     reads=[b_lsp], writes=[b_lsp])
                bkg, b_bkg = pp.next()
                S.mm([(bkg[:, 128:132], [(trif[:], lsp[:, 4:8])]),
                      (bkg[0:4, 136:137], [(lsp[:, 4:8], onesf[:, 0:1])])],
                     reads=[b_trif, b_lsp, b_onesf], writes=[b_bkg])
                S.op(V, lambda e: e.tensor_copy(out=anb[:, 4:8], in_=bkg[:, 128:132]), reads=[b_bkg], writes=[b_anb])
                S.op(V, lambda e: e.tensor_tensor(out=anb[:, 0:4], in0=bkg[:, 128:132], in1=gif[:, 0:4], op=ALU.add),
                     reads=[b_bkg, b_gif], writes=[b_anb])
                S.op(V, lambda e: e.tensor_copy(out=sm[:, 0:1], in_=bkg[0:4, 136:137]), reads=[b_bkg], writes=[b_sm])
                S.tr([(bkg[0:4, 256:384], anb[:, 0:4])], identf[:], reads=[b_anb, b_identf], writes=[b_bkg])
                S.op(V, lambda e: e.tensor_reduce(out=sm[:, 1:2], in_=bkg[0:4, 256:384], axis=AX.X, op=ALU.max),
                     reads=[b_bkg], writes=[b_sm])
                pp.release_mine()

            def P1b(it):
                main = it >= NPRE
                r2 = it % 2
                xT_t, b_xTt = xT[it % R3]
                qkT, b_qkT = qkT_r[r2]
                vaug, b_vaug = vaug_r[r2]
                nlist = list(range(16)) if (main or it == NPRE - 1) else list(range(8, 16))
                n0 = nlist[0]
                S.op(G, lambda e: e.tensor_copy(out=pre_t[:, n0:16, 0:3], in_=halo[:, n0:16, :]),
                     reads=[b_halo], writes=[b_pre])
                for g0 in range(0, len(nlist), 4):
                    grp = nlist[g0:g0 + 4]
                    bk, b_bk = pp.next()
                    if grp[0] >= 8:
                        wt_, b_wt = Wk, b_wk
                        off = (grp[0] - 8) * 128
                    else:
                        wt_, b_wt = stream_next(grp[0] // 4)
                        off = 0
                    S.mm([(bk[:, j * 128:(j + 1) * 128],
                           [(wt_[:, kc, off + j * 128:off + (j + 1) * 128], xT_t[:, kc, :]) for kc in range(8)])
                          for j in range(4)],
                         reads=[b_xTt, b_wt], writes=[b_bk])
                    S.op(A, lambda e: e.copy(out=pre_t[:, grp[0]:grp[0] + 4, 3:131],
                                             in_=bk[:].rearrange("p (a b) -> p a b", a=4)),
                         reads=[b_bk], writes=[b_pre])
                S.op(G, lambda e: e.tensor_copy(out=halo[:, n0:16, :], in_=pre_t[:, n0:16, 128:131]),
                     reads=[b_pre], writes=[b_halo])
                for i in nlist:
                    S.op(A, lambda e: e.activation(out=cacc[:, i, :], in_=pre_t[:, i, 3:131], func=AF.Identity,
                                                   scale=convw[:, i, 3:4], bias=convb[:, i:i + 1]),
                         reads=[b_pre, b_convw, b_convb], writes=[b_cacc[i]])
                for j in range(3):
                    for i in nlist:
                        S.op(V, lambda e: e.scalar_tensor_tensor(out=cacc[:, i, :], in0=pre_t[:, i, j:j + 128],
                                                                 scalar=convw[:, i, j:j + 1], in1=cacc[:, i, :],
                                                                 op0=ALU.mult, op1=ALU.add),
                             reads=[b_pre, b_convw, b_cacc[i]], writes=[b_cacc[i]])
                S.op(A, lambda e: e.activation(out=qkT[:, n0:16, :].rearrange("p a b -> p (a b)"),
                                               in_=cacc[:, n0:16, :].rearrange("p a b -> p (a b)"), func=AF.Silu),
                     reads=[b_cacc[i] for i in nlist], writes=[b_qkT])
                for hgp in range(2):
                    bk, b_bk = proj_w(xT_t, b_xTt, Wv, b_wv, hgp * 512, 512)
                    S.op(A, lambda e: e.copy(out=vaug[:, 2 * hgp:2 * hgp + 2, 0:256],
                                             in_=bk[:].rearrange("p (a b) -> p a b", a=2)),
                         reads=[b_bk], writes=[b_vaug])
                pp.release_mine()

            def P2(it):
                main = it >= NPRE
                r2 = it % 2
                qkT, b_qkT = qkT_r[r2]
                vaug, b_vaug = vaug_r[r2]
                gif, b_gif = gif_r[it % RG]
                anb, b_anb = anb_r[it % RG]
                sm, b_sm = sm_r[it % RG]
                hbuf, b_hbuf = ya_r[r2]
                hs, b_hs = hs_r[r2]
                S.op(V, lambda e: e.tensor_tensor(out=sm[:, 2:3], in0=sm[:, 1:2], in1=mst[:], op=ALU.max),
                     reads=[b_sm, b_mst], writes=[b_sm])
                S.op(V, lambda e: e.tensor_scalar(out=sm[:, 3:4], in0=sm[:, 2:3], scalar1=-1.0, scalar2=None, op0=ALU.mult),
                     reads=[b_sm], writes=[b_sm])
                S.op(A, lambda e: e.activation(out=sm[:, 4:5], in_=mst[:], func=AF.Exp, bias=sm[:, 3:4]),
                     reads=[b_sm, b_mst], writes=[b_sm])
                S.op(V, lambda e: e.tensor_tensor(out=mst[:], in0=sm[:, 2:3], in1=sm[:, 0:1], op=ALU.subtract),
                     reads=[b_sm], writes=[b_mst])
                S.op(V, lambda e: e.tensor_scalar(out=rhs8[:, 0:4], in0=identf[0:4, 0:4], scalar1=sm[:, 3:4], scalar2=None,
                                                  op0=ALU.mult), reads=[b_sm, b_identf], writes=[b_rhs8])
                S.op(V, lambda e: e.tensor_scalar(out=rhs8[:, 4:8], in0=identf[0:4, 0:4], scalar1=sm[:, 4:5], scalar2=None,
                                                  op0=ALU.mult), reads=[b_sm, b_identf], writes=[b_rhs8])
                S.mm([(bk_s[:, 144:152], [(onesf[0:4, :], rhs8[:])])], reads=[b_onesf, b_rhs8], writes=[b_s])
                S.op(V, lambda e: e.tensor_copy(out=bc[:], in_=bk_s[:, 144:152]), reads=[b_s], writes=[b_bc])
                S.op(V, lambda e: e.tensor_tensor(out=eac[:, 0:4], in0=anb[:, 0:4], in1=bc[:, 0:4], op=ALU.add),
                     reads=[b_anb, b_bc], writes=[b_eac])
                S.op(V, lambda e: e.tensor_tensor(out=eac[:, 4:8], in0=anb[:, 4:8], in1=bc[:, 0:4], op=ALU.add),
                     reads=[b_anb, b_bc], writes=[b_eac])
                S.op(A, lambda e: e.activation(out=eac[:], in_=eac[:], func=AF.Exp), reads=[b_eac], writes=[b_eac])
                if not main:
                    S.op(V, lambda e: e.tensor_scalar(out=eac[:, 0:4], in0=eac[:, 0:4], scalar1=pmask[:, it:it + 1],
                                                      scalar2=None, op0=ALU.mult),
                         reads=[b_eac, b_pmask], writes=[b_eac])
                for h in range(4):
                    S.op(V, lambda e: e.tensor_scalar(out=vw[:, h, :], in0=vaug[:, h, :], scalar1=eac[:, h:h + 1],
                                                      scalar2=None, op0=ALU.mult),
                         reads=[b_vaug, b_eac], writes=[b_vw[h]])
                for h in range(4):
                    S.tr([(kv1_bf[:, 640 + dc * 128:640 + (dc + 1) * 128], qkT[:, 8 + 2 * h + dc, :]) for dc in range(2)],
                         identb[:], reads=[b_qkT, b_identb], writes=[b_kv1])
                    S.op(A, lambda e: e.mul(out=ktok[:], in_=kv1_bf[:, 640:896], mul=0.0625), reads=[b_kv1], writes=[b_ktok])
                    if main:
                        S.mm([(bk_s[:, 0:128], [(qkT[:, 8 + 2 * h + dc, :], qkT[:, 2 * h + dc, :]) for dc in range(2)])],
                             reads=[b_qkT], writes=[b_s])
                        S.op(V, lambda e: e.scalar_tensor_tensor(out=PT[:], in0=bk_s[:, 0:128], scalar=0.0625, in1=trif[:],
                                                                 op0=ALU.mult, op1=ALU.mult),
                             reads=[b_s, b_trif], writes=[b_PT])
                        S.op(V, lambda e: e.tensor_scalar(out=Cb[:].rearrange("p a b -> p (a b)"),
                                                          in0=Cm[:, h, :, :].rearrange("p a b -> p (a b)"),
                                                          scalar1=bc[:, 4 + h:5 + h], scalar2=None, op0=ALU.mult),
                             reads=[b_Cm[h], b_bc], writes=[b_Cb])
                        S.mm([(bk_num[:, 0:257], [(PT[:], vw[:, h, :]),
                                                  (qkT[:, 2 * h, :], Cb[:, 0, :]),
                                                  (qkT[:, 2 * h + 1, :], Cb[:, 1, :])])],
                             reads=[b_PT, b_vw[h], b_qkT, b_Cb], writes=[b_num])
                    S.mm([(bk_kv0[:, 0:257], [(ktok[:, 0:128], vw[:, h, :])]),
                          (bk_kv1[:, 0:257], [(ktok[:, 128:256], vw[:, h, :])])],
                         reads=[b_ktok, b_vw[h]], writes=[b_kv0, b_kv1])
                    S.op(V, lambda e: e.scalar_tensor_tensor(out=Cm[:, h, 0, :], in0=Cm[:, h, 0, :], scalar=bc[:, 4 + h:5 + h],
                                                             in1=bk_kv0[:, 0:257], op0=ALU.mult, op1=ALU.add),
                         reads=[b_Cm[h], b_bc, b_kv0], writes=[b_Cm[h]])
                    S.op(V, lambda e: e.scalar_tensor_tensor(out=Cm[:, h, 1, :], in0=Cm[:, h, 1, :], scalar=bc[:, 4 + h:5 + h],
                                                             in1=bk_kv1[:, 0:257], op0=ALU.mult, op1=ALU.add),
                         reads=[b_Cm[h], b_bc, b_kv1], writes=[b_Cm[h]])
                    if main:
                        S.op(V, lambda e: e.tensor_scalar(out=hs[:, 8 + h:9 + h], in0=bk_num[:, 256:257], scalar1=-1.0,
                                                          scalar2=None, op0=ALU.mult), reads=[b_num], writes=[b_hs])
                        S.op(V, lambda e: e.tensor_tensor(out=hs[:, 8 + h:9 + h], in0=hs[:, 8 + h:9 + h],
                                                          in1=bk_num[:, 256:257], op=ALU.max), reads=[b_num, b_hs], writes=[b_hs])
                        S.op(V, lambda e: e.tensor_tensor(out=hs[:, 8 + h:9 + h], in0=hs[:, 8 + h:9 + h],
                                                          in1=eac[:, 4 + h:5 + h], op=ALU.max),
                             reads=[b_hs, b_eac], writes=[b_hs])
                        S.op(V, lambda e: e.reciprocal(out=hs[:, 12 + h:13 + h], in_=hs[:, 8 + h:9 + h]),
                             reads=[b_hs], writes=[b_hs])
                        S.op(A, lambda e: e.activation(out=hbuf[:, h * 256:(h + 1) * 256], in_=bk_num[:, 0:256],
                                                       func=AF.Copy, scale=hs[:, 12 + h:13 + h]),
                             reads=[b_num, b_hs], writes=[b_hbuf])
                        S.op(A, lambda e: e.activation(out=junk[:, 0:256], in_=hbuf[:, h * 256:(h + 1) * 256],
                                                       func=AF.Square, accum_out=hs[:, h:h + 1]),
                             reads=[b_hbuf], writes=[b_junk, b_hs])

            def P3(it):
                c = it - NPRE
                r2 = it % 2
                xt_t, b_xt = xt[it % R3]
                xT_t, b_xTt = xT[it % R3]
                ya, b_ya = ya_r[r2]
                hs, b_hs = hs_r[r2]

                def proj_s(g):
                    wt_, b_wt = stream_next(g)
                    return proj_w(xT_t, b_xTt, wt_, b_wt, 0, 512)

                for hgp in range(2):
                    bk, b_bk = proj_s(2 + hgp)
                    S.op(A, lambda e: e.activation(out=so[:, hgp * 512:(hgp + 1) * 512], in_=bk[:], func=AF.Sigmoid),
                         reads=[b_bk], writes=[b_so])
                for hgp in range(2):
                    bk, b_bk = proj_s(4 + hgp)
                    gelu_group(bk[:], b_bk, ub[:, hgp * 512:(hgp + 1) * 512], b_ub)
                for hgp in range(2):
                    bk, b_bk = proj_s(6 + hgp)
                    gelu_group(bk[:], b_bk, svb[:, hgp * 512:(hgp + 1) * 512], b_svb)
                for hgp in range(4):
                    bk, b_bk = proj_s(8 + hgp)
                    S.op(V, lambda e: e.tensor_tensor(out=gt[:, hgp * 512:(hgp + 1) * 512], in0=bk[:],
                                                      in1=bgate[:, hgp * 512:(hgp + 1) * 512], op=ALU.add),
                         reads=[b_bk, b_bgate], writes=[b_gt])
                S.op(A, lambda e: e.activation(out=gt[:], in_=gt[:], func=AF.Sigmoid), reads=[b_gt], writes=[b_gt])
                rms_rstd(A, svb[:], b_svb, junk3[:], b_junk3, st3, b_st3, float(D))
                S.op(V, lambda e: e.scalar_tensor_tensor(out=svn[:], in0=svb[:], scalar=st3[:, 2:3], in1=sgug[:],
                                                         op0=ALU.mult, op1=ALU.mult),
                     reads=[b_svb, b_st3, b_sgug], writes=[b_svn])
                for half in range(2):
                    bk, b_bk = pp.next()
                    S.mm([(bk[:, j * 128:(j + 1) * 128], [(wsT[:, half * 4 + j, :], svn[:, (half * 4 + j) * 128:(half * 4 + j + 1) * 128])])
                          for j in range(4)], reads=[b_wsT, b_svn], writes=[b_bk])
                    sl = slice(half * 512, (half + 1) * 512)
                    S.op(V, lambda e: e.tensor_tensor(out=yb[:, sl], in0=bk[:], in1=bst[:, sl], op=ALU.add),
                         reads=[b_bk, b_bst], writes=[b_yb])
                S.op(G, lambda e: e.tensor_tensor(out=yb[:], in0=yb[:], in1=ub[:], op=ALU.mult), reads=[b_yb, b_ub], writes=[b_yb])
                S.op(G, lambda e: e.tensor_tensor(out=yb[:], in0=yb[:], in1=gt[:, D:2 * D], op=ALU.mult), reads=[b_yb, b_gt], writes=[b_yb])
                S.op(V, lambda e: e.tensor_scalar(out=hs[:, 4:8], in0=hs[:, 0:4], scalar1=1.0 / 256, scalar2=EPS,
                                                  op0=ALU.mult, op1=ALU.add), reads=[b_hs], writes=[b_hs])
                S.op(A, lambda e: e.activation(out=hs[:, 4:8], in_=hs[:, 4:8], func=AF.Sqrt), reads=[b_hs], writes=[b_hs])
                S.op(V, lambda e: e.reciprocal(out=hs[:, 4:8], in_=hs[:, 4:8]), reads=[b_hs], writes=[b_hs])
                for h in range(4):
                    sl = slice(h * 256, (h + 1) * 256)
                    S.op(V, lambda e: e.scalar_tensor_tensor(out=ya[:, sl], in0=ya[:, sl], scalar=hs[:, 4 + h:5 + h],
                                                             in1=mhg[:, sl], op0=ALU.mult, op1=ALU.mult),
                         reads=[b_ya, b_hs, b_mhg], writes=[b_ya])
                S.op(G, lambda e: e.tensor_tensor(out=ya[:], in0=ya[:], in1=so[:], op=ALU.mult), reads=[b_ya, b_so], writes=[b_ya])
                S.op(G, lambda e: e.tensor_tensor(out=ya[:], in0=ya[:], in1=gt[:, 0:D], op=ALU.mult), reads=[b_ya, b_gt], writes=[b_ya])
                S.op(V, lambda e: e.tensor_tensor(out=zb[:], in0=ya[:], in1=yb[:], op=ALU.add), reads=[b_ya, b_yb], writes=[b_zb])
                trp.next()
                S.tr([(trv[:, k * 128:(k + 1) * 128], zb[:, k * 128:(k + 1) * 128]) for k in range(8)], identb[:],
                     reads=[b_zb, b_identb], writes=[b_tr])
                S.op(A, lambda e: e.copy(out=zT[:].rearrange("p k t -> p (k t)"), in_=trv), reads=[b_tr], writes=[b_zT])
                trp.release_mine()
                for half in range(2):
                    bk, b_bk = pp.next()
                    sl = slice(half * 512, (half + 1) * 512)
                    wo_, b_wo = stream_next(12 + half)
                    S.mm([(bk[:], [(zT[:, kc, :], wo_[:, kc, :]) for kc in range(8)])], reads=[b_zT, b_wo], writes=[b_bk])
                    S.op(V, lambda e: e.tensor_tensor(out=xt_t[:, sl], in0=bk[:], in1=xt_t[:, sl], op=ALU.add),
                         reads=[b_bk, b_xt], writes=[b_xt])
                if stage == 1:
                    S.dma(G, y_d[c * 128:(c + 1) * 128, :], xt_t[:], reads=[b_xt], writes=[b_x1s[c]])
                else:
                    S.dma(G, x1s[c * 128:(c + 1) * 128, :], xt_t[:], reads=[b_xt], writes=[b_x1s[c]])

            def load_xchunk(it):
                xt_t, b_xt = xt[it % R3]
                S.dma(SP, xt_t[:], xseq[it * 128:(it + 1) * 128, :], writes=[b_xt])

            XPF = 2
            xsched = {}
            for it0 in range(NIT):
                r0 = max(0, it0 - (XPF if it0 < NPRE + R3 else 0))
                xsched.setdefault(r0, []).append(it0)
            for rnd in range(NIT + 3):
                for it0 in xsched.get(rnd, []):
                    load_xchunk(it0)
                if 1 <= rnd <= NZG:
                    zero_group(rnd - 1)
                if rnd >= 2 and rnd % 2 == 0 and (rnd - 2) // 2 < NSTG:
                    stage_group((rnd - 2) // 2)
                fns = []
                if rnd < NIT:
                    fns.append((lambda i=rnd: P1a(i), 1))
                if 0 <= rnd - 1 < NIT:
                    fns.append((lambda i=rnd - 1: P1b(i), 1))
                if 0 <= rnd - 2 < NIT:
                    fns.append((lambda i=rnd - 2: P2(i), 1))
                if NPRE <= rnd - 3 < NIT:
                    fns.append((lambda i=rnd - 3: P3(i), 2))
                S.weave(fns)

        if stage == 1:
            S.wait_all(G, b_x1s)
            return nc
        S.barrier()

        pbc = ExitStack()
        with pbc:
            acc = pbc.enter_context(nc.sbuf_tensor("s_acc", [128, NCH, D], F32))
            b_acc = [Buf() for _ in range(NCH)]
            xn3B = pbc.enter_context(nc.sbuf_tensor("s_xn3B", [128, NCH, D], BF16))
            b_xn3B = [Buf() for _ in range(NCH)]
            mskA, b_mskA = sbt(pbc, "mskA", [128, NCH, NE], F32)
            posA, b_posA = sbt(pbc, "posA", [128, NCH, NE], F32)
            runcnt, b_runcnt = sbt(pbc, "runcnt", [128, NE], F32)
            desti, b_desti = sbt(pbc, "desti", [128, NCH, 2], I32)
            gate2, b_gate2 = sbt(pbc, "gate2", [128, NCH, 2], F32)
            idxWi, b_idxWi = sbt(pbc, "idxWi", [128, NBLK, 2], I32)
            trisb, b_trisb = sbt(pbc, "trisb", [128, 128], BF16)
            onesb, b_onesb = sbt(pbc, "onesb", [128, 128], BF16)
            S.op(V, lambda e: e.tensor_tensor(out=trisb[:], in0=trif[:], in1=identf[:], op=ALU.subtract),
                 reads=[b_trif, b_identf], writes=[b_trisb])
            S.op(V, lambda e: e.memset(onesb[:], 1.0), writes=[b_onesb])
            S.op(V, lambda e: e.memset(runcnt[:], 0.0), writes=[b_runcnt])
            Gt = pbc.enter_context(nc.sbuf_tensor("s_Gt", [128, NCH, NE], F32))
            b_Gt = [Buf() for _ in range(NCH)]
            gf, b_gf = sbt(pbc, "gf", [128, D], F32)
            S.dma(SP, gf[:], gf_d, writes=[b_gf])
            junk, b_junk = sbt(pbc, "junk2", [128, D], BF16)
            b_y = [Buf() for _ in range(NCH)]

            pb = ExitStack()
            with pb:
                Wxq, b_wxq = sbt(pb, "Wxq", [128, 8, D], BF16)
                for hh in range(2):
                    S.dma(SP, Wxq[:, :, hh * 512:(hh + 1) * 512], wst[14 + hh], reads=[b_wst[14 + hh]], writes=[b_wxq])
                Wxo, b_wxo = sbt(pb, "Wxo", [128, 8, D], BF16)
                for hh in range(2):
                    S.dma(SP, Wxo[:, :, hh * 512:(hh + 1) * 512], wst[16 + hh], reads=[b_wst[16 + hh]], writes=[b_wxo])
                Wkv, b_wkv = sbt(pb, "Wkv", [128, 8, 512], BF16)
                w_xkv_v = w_xkv_d.rearrange("(k p) n -> p k n", p=128)
                Wr, b_wr = sbt(pb, "Wr", [128, 8, 36], F32)
                S.dma(SP, Wr[:], w_r_d.rearrange("(k p) n -> p k n", p=128), writes=[b_wr])

                def cload2(name, shape, src):
                    t, b = sbt(pb, name, shape, F32)
                    S.dma(SP, t[:], src, writes=[b])
                    return t, b

                gx, b_gx = cload2("gx", [128, D], gx_d)
                gmoe, b_gmoe = cload2("gmoe", [128, D], gmem_d)
                gmem, b_gmem = gmoe, b_gmoe
                brt, b_brt = cload2("brt", [128, 36], br_d)

                xt = [sbt(pb, "bxt0", [128, D], F32)] * 2
                st = [sbt(pb, "bst%d" % i, [128, 4], F32) for i in range(2)]
                xn, b_xn = sbt(pb, "bxn", [128, D], BF16)
                xT, b_xT = sbt(pb, "bxT", [128, 8, 128], BF16)
                mnT, b_mnT = sbt(pb, "mnT", [128, 8, 256], BF16)
                KT, b_KT = sbt(pb, "KT", [128, 8, 256], BF16)
                Vm, b_Vm = sbt(pb, "Vm", [128, 2, D], BF16)
                pexp, b_pexp = sbt(pb, "pexp", [128, 4, 256], BF16)
                pT, b_pT = sbt(pb, "pT", [128, 8, 128], BF16)
                ss, b_ss = sbt(pb, "ss", [128, 16], F32)
                attn, b_attn = sbt(pb, "attn", [128, D], BF16)
                xn3f, b_xn3f = sbt(pb, "xn3f", [128, D], F32)
                xTf, b_xTf = sbt(pb, "xTf", [128, 8, 128], F32)
                lg, b_lg = sbt(pb, "lg", [128, 36], F32)
                rt, b_rt = sbt(pb, "rt", [128, 16], F32)
                oh, b_oh = sbt(pb, "oh", [128, 4], F32)
                ml, b_ml = sbt(pb, "ml", [128, NE], F32)
                ml2, b_ml2 = sbt(pb, "ml2", [128, NE], F32)
                msk, b_msk = sbt(pb, "msk", [128, NE], F32)
                mskb, b_mskb = sbt(pb, "mskb", [128, NE], BF16)

                pp = Pool(banks[0:2])
                ppX1 = Pool(banks[2:3])
                ppX2 = Pool(banks[3:5])
                bk_tr, b_tr = banks[5]
                bk_tr2, b_tr2 = banks[6]
                bk_tr3, b_tr3 = banks[7]
                trv = bk_tr[:].bitcast(BF16)
                trv2 = bk_tr2[:].bitcast(BF16)
                trv3 = bk_tr3[:].bitcast(BF16)

                for mc in range(2):
                    xt_t, b_xt = xt[mc]
                    st_t, b_stt = st[mc]
                    S.dma(SP, xt_t[:], mem_d[mc * 128:(mc + 1) * 128, :], writes=[b_xt])
                    rms_rstd(A, xt_t[:], b_xt, junk[:], b_junk, st_t, b_stt, float(D))
                    S.op(V, lambda e: e.scalar_tensor_tensor(out=xn[:], in0=xt_t[:], scalar=st_t[:, 2:3], in1=gmem[:],
                                                             op0=ALU.mult, op1=ALU.mult),
                         reads=[b_xt, b_stt, b_gmem], writes=[b_xn])
                    S.tr([(trv[:, k * 128:(k + 1) * 128], xn[:, k * 128:(k + 1) * 128]) for k in range(8)], identb[:],
                         reads=[b_xn, b_identb], writes=[b_tr])
                    S.op(A, lambda e: e.copy(out=mnT[:, :, mc * 128:(mc + 1) * 128],
                                             in_=trv.rearrange("p (k t) -> p k t", k=8)), reads=[b_tr], writes=[b_mnT])
                S.dma(SP, gmoe[:], gmoe_d, reads=[], writes=[b_gmoe])
                for grp4 in range(4):
                    S.dma(SP, Wkv[:], wst[18 + grp4], reads=[b_wst[18 + grp4]], writes=[b_wkv])
                    if grp4 < 2:
                        for jj in range(2):
                            i0 = grp4 * 4 + jj * 2
                            bk, b_bk = pp.next()
                            S.mm([(bk[:, j * 256:(j + 1) * 256],
                                   [(Wkv[:, kc, (jj * 2 + j) * 128:(jj * 2 + j + 1) * 128], mnT[:, kc, :]) for kc in range(8)])
                                  for j in range(2)], reads=[b_wkv, b_mnT], writes=[b_bk])
                            S.op(A, lambda e: e.copy(out=KT[:, i0:i0 + 2, :].rearrange("p a b -> p (a b)"), in_=bk[:]),
                                 reads=[b_bk], writes=[b_KT])
                    else:
                        half = grp4 - 2
                        for mc in range(2):
                            bk, b_bk = pp.next()
                            S.mm([(bk[:], [(mnT[:, kc, mc * 128:(mc + 1) * 128], Wkv[:, kc, :]) for kc in range(8)])],
                                 reads=[b_wkv, b_mnT], writes=[b_bk])
                            S.op(A, lambda e: e.copy(out=Vm[:, mc, half * 512:(half + 1) * 512], in_=bk[:]),
                                 reads=[b_bk], writes=[b_Vm])

                q2T_r = [sbt(pb, "q2Tr%d" % i, [128, 8, 128], BF16) for i in range(2)]
                aT_r = [sbt(pb, "aTr%d" % i, [128, 8, 128], BF16) for i in range(2)]
                stY = [sbt(pb, "stY%d" % i, [128, 4], F32) for i in range(2)]
                junkY, b_junkY = sbt(pb, "junkY", [128, D], BF16)

                def BX1(c):
                    st_t, b_stt = st[c % 2]
                    q2T, b_q2T = q2T_r[c % 2]
                    S.dma(SP, acc[:, c, :], x1s[c * 128:(c + 1) * 128, :], reads=[b_x1s[c]], writes=[b_acc[c]])
                    rms_rstd(A, acc[:, c, :], b_acc[c], junk[:], b_junk, st_t, b_stt, float(D))
                    S.op(V, lambda e: e.scalar_tensor_tensor(out=xn[:], in0=acc[:, c, :], scalar=st_t[:, 2:3], in1=gx[:],
                                                             op0=ALU.mult, op1=ALU.mult),
                         reads=[b_acc[c], b_stt, b_gx], writes=[b_xn])
                    S.tr([(trv[:, k * 128:(k + 1) * 128], xn[:, k * 128:(k + 1) * 128]) for k in range(8)], identb[:],
                         reads=[b_xn, b_identb], writes=[b_tr])
                    S.op(A, lambda e: e.copy(out=xT[:].rearrange("p k t -> p (k t)"), in_=trv), reads=[b_tr], writes=[b_xT])
                    for i0 in range(0, 8, 4):
                        bk, b_bk = ppX1.next()
                        S.mm([(bk[:, j * 128:(j + 1) * 128],
                               [(Wxq[:, kc, (i0 + j) * 128:(i0 + j + 1) * 128], xT[:, kc, :]) for kc in range(8)])
                              for j in range(4)], reads=[b_wxq, b_xT], writes=[b_bk])
                        S.op(A, lambda e: e.copy(out=q2T[:, i0:i0 + 4, :].rearrange("p a b -> p (a b)"), in_=bk[:]),
                             reads=[b_bk], writes=[b_q2T])

                def BX2(c):
                    q2T, b_q2T = q2T_r[c % 2]
                    aT, b_aT = aT_r[c % 2]
                    for hp in range(2):
                        bk, b_bk = ppX2.next()
                        S.mm([(bk[:, j * 256:(j + 1) * 256],
                               [(q2T[:, 2 * (2 * hp + j) + dc, :], KT[:, 2 * (2 * hp + j) + dc, :]) for dc in range(2)])
                              for j in range(2)], reads=[b_q2T, b_KT], writes=[b_bk])
                        S.op(V, lambda e: e.tensor_reduce(out=ss[:, 2 * hp:2 * hp + 2], in_=bk[:].rearrange("p (a b) -> p a b", a=2),
                                                          axis=AX.X, op=ALU.max), reads=[b_bk], writes=[b_ss])
                        S.op(V, lambda e: e.tensor_scalar(out=ss[:, 4 + 2 * hp:6 + 2 * hp], in0=ss[:, 2 * hp:2 * hp + 2],
                                                          scalar1=-0.0625, scalar2=None, op0=ALU.mult), reads=[b_ss], writes=[b_ss])
                        for j in range(2):
                            h = 2 * hp + j
                            S.op(A, lambda e: e.activation(out=pexp[:, h, :], in_=bk[:, j * 256:(j + 1) * 256], func=AF.Exp,
                                                           scale=0.0625, bias=ss[:, 4 + h:5 + h], accum_out=ss[:, 8 + h:9 + h]),
                                 reads=[b_bk, b_ss], writes=[b_pexp, b_ss])
                    S.op(V, lambda e: e.reciprocal(out=ss[:, 12:16], in_=ss[:, 8:12]), reads=[b_ss], writes=[b_ss])
                    S.tr([(trv2[:, (2 * h + mc) * 128:(2 * h + mc + 1) * 128], pexp[:, h, mc * 128:(mc + 1) * 128])
                          for h in range(4) for mc in range(2)], identb[:], reads=[b_pexp, b_identb], writes=[b_tr2])
                    S.op(A, lambda e: e.copy(out=pT[:].rearrange("p k t -> p (k t)"), in_=trv2), reads=[b_tr2], writes=[b_pT])
                    for hp in range(2):
                        bk, b_bk = ppX2.next()
                        S.mm([(bk[:, j * 256:(j + 1) * 256],
                               [(pT[:, 2 * (2 * hp + j) + mc, :], Vm[:, mc, (2 * hp + j) * 256:(2 * hp + j + 1) * 256]) for mc in range(2)])
                              for j in range(2)], reads=[b_pT, b_Vm], writes=[b_bk])
                        for j in range(2):
                            h = 2 * hp + j
                            S.op(A, lambda e: e.activation(out=attn[:, h * 256:(h + 1) * 256], in_=bk[:, j * 256:(j + 1) * 256],
                                                           func=AF.Copy, scale=ss[:, 12 + h:13 + h]),
                                 reads=[b_bk, b_ss], writes=[b_attn])
                    S.tr([(trv2[:, k * 128:(k + 1) * 128], attn[:, k * 128:(k + 1) * 128]) for k in range(8)], identb[:],
                         reads=[b_attn, b_identb], writes=[b_tr2])
                    S.op(A, lambda e: e.copy(out=aT[:].rearrange("p k t -> p (k t)"), in_=trv2), reads=[b_tr2], writes=[b_aT])

                def BY(c):
                    st_t, b_stt = stY[c % 2]
                    aT, b_aT = aT_r[c % 2]
                    for half in range(2):
                        bk, b_bk = pp.next()
                        sl = slice(half * 512, (half + 1) * 512)
                        S.mm([(bk[:], [(aT[:, kc, :], Wxo[:, kc, sl]) for kc in range(8)])], reads=[b_aT, b_wxo], writes=[b_bk])
                        S.op(V, lambda e: e.tensor_tensor(out=acc[:, c, sl], in0=bk[:], in1=acc[:, c, sl], op=ALU.add),
                             reads=[b_bk, b_acc[c]], writes=[b_acc[c]])
                    if stage == 2:
                        S.dma(G, y_d[c * 128:(c + 1) * 128, :], acc[:, c, :], reads=[b_acc[c]], writes=[b_y[c]])
                        return
                    rms_rstd(A, acc[:, c, :], b_acc[c], junkY[:], b_junkY, st_t, b_stt, float(D))
                    S.op(V, lambda e: e.scalar_tensor_tensor(out=xn3f[:], in0=acc[:, c, :], scalar=st_t[:, 2:3], in1=gmoe[:],
                                                             op0=ALU.mult, op1=ALU.mult),
                         reads=[b_acc[c], b_stt, b_gmoe], writes=[b_xn3f])
                    S.op(G, lambda e: e.tensor_copy(out=xn3B[:, c, :], in_=xn3f[:]), reads=[b_xn3f], writes=[b_xn3B[c]])
                    for half in range(2):
                        bk, b_bk = pp.next()
                        S.tr([(bk[:, k * 128:(k + 1) * 128], xn3f[:, (half * 4 + k) * 128:(half * 4 + k + 1) * 128]) for k in range(4)],
                             identf[:], reads=[b_xn3f, b_identf], writes=[b_bk])
                        S.op(V, lambda e: e.tensor_copy(out=xTf[:, half * 4:half * 4 + 4, :].rearrange("p a b -> p (a b)"), in_=bk[:]),
                             reads=[b_bk], writes=[b_xTf])
                    bk, b_bk = pp.next()
                    S.mm([(bk[:, 0:36], [(xTf[:, kc, :], Wr[:, kc, :]) for kc in range(8)])], reads=[b_xTf, b_wr], writes=[b_bk])
                    S.op(V, lambda e: e.tensor_tensor(out=lg[:], in0=bk[:, 0:36], in1=brt[:], op=ALU.add),
                         reads=[b_bk, b_brt], writes=[b_lg])
                    S.op(V, lambda e: e.tensor_reduce(out=rt[:, 0:1], in_=lg[:, 0:4], axis=AX.X, op=ALU.max), reads=[b_lg], writes=[b_rt])
                    S.op(V, lambda e: e.tensor_scalar(out=oh[:], in0=lg[:, 0:4], scalar1=rt[:, 0:1], scalar2=None, op0=ALU.is_ge),
                         reads=[b_lg, b_rt], writes=[b_oh])
                    S.op(V, lambda e: e.tensor_scalar(out=rt[:, 1:2], in0=rt[:, 0:1], scalar1=-1.0, scalar2=None, op0=ALU.mult),
                         reads=[b_rt], writes=[b_rt])
                    S.op(A, lambda e: e.activation(out=junk[:, 0:4], in_=lg[:, 0:4], func=AF.Exp, bias=rt[:, 1:2], accum_out=rt[:, 2:3]),
                         reads=[b_lg, b_rt], writes=[b_junk, b_rt])
                    S.op(V, lambda e: e.tensor_scalar(out=oh[:], in0=oh[:], scalar1=1e30, scalar2=-1e30, op0=ALU.mult, op1=ALU.add),
                         reads=[b_oh], writes=[b_oh])
                    S.op(V, lambda e: e.tensor_tensor(out=ml[:].rearrange("p (g j) -> p g j", g=4),
                                                      in0=lg[:, 4:36].rearrange("p (g j) -> p g j", g=4),
                                                      in1=oh[:].unsqueeze(2).to_broadcast([128, 4, 8]), op=ALU.add),
                         reads=[b_lg, b_oh], writes=[b_ml])
                    S.op(V, lambda e: e.tensor_reduce(out=rt[:, 3:4], in_=ml[:], axis=AX.X, op=ALU.max), reads=[b_ml], writes=[b_rt])
                    S.op(V, lambda e: e.tensor_scalar(out=ml2[:], in0=ml[:], scalar1=rt[:, 3:4], scalar2=-1e30, op0=ALU.is_ge, op1=ALU.mult),
                         reads=[b_ml, b_rt], writes=[b_ml2])
                    S.op(V, lambda e: e.tensor_tensor(out=ml2[:], in0=ml2[:], in1=ml[:], op=ALU.add), reads=[b_ml2, b_ml], writes=[b_ml2])
                    S.op(V, lambda e: e.tensor_reduce(out=rt[:, 4:5], in_=ml2[:], axis=AX.X, op=ALU.max), reads=[b_ml2], writes=[b_rt])
                    S.op(V, lambda e: e.tensor_scalar(out=msk[:], in0=ml[:], scalar1=rt[:, 4:5], scalar2=None, op0=ALU.is_ge),
                         reads=[b_ml, b_rt], writes=[b_msk])
                    S.op(V, lambda e: e.tensor_scalar(out=rt[:, 5:6], in0=rt[:, 3:4], scalar1=-1.0, scalar2=None, op0=ALU.mult),
                         reads=[b_rt], writes=[b_rt])
                    S.op(V, lambda e: e.tensor_scalar(out=ml2[:], in0=ml[:], scalar1=rt[:, 5:6], scalar2=-80.0, op0=ALU.add, op1=ALU.max),
                         reads=[b_ml, b_rt], writes=[b_ml2])
                    S.op(A, lambda e: e.activation(out=ml2[:], in_=ml2[:], func=AF.Exp), reads=[b_ml2], writes=[b_ml2])
                    S.op(V, lambda e: e.tensor_tensor(out=ml2[:], in0=ml2[:], in1=msk[:], op=ALU.mult), reads=[b_ml2, b_msk], writes=[b_ml2])
                    S.op(V, lambda e: e.tensor_reduce(out=rt[:, 6:7], in_=ml2[:], axis=AX.X, op=ALU.add), reads=[b_ml2], writes=[b_rt])
                    S.op(V, lambda e: e.tensor_tensor(out=rt[:, 7:8], in0=rt[:, 6:7], in1=rt[:, 2:3], op=ALU.mult), reads=[b_rt], writes=[b_rt])
                    S.op(V, lambda e: e.reciprocal(out=rt[:, 8:9], in_=rt[:, 7:8]), reads=[b_rt], writes=[b_rt])
                    S.op(V, lambda e: e.tensor_scalar(out=Gt[:, c, :], in0=ml2[:], scalar1=rt[:, 8:9], scalar2=None, op0=ALU.mult),
                         reads=[b_ml2, b_rt], writes=[b_Gt[c]])
                    S.op(V, lambda e: e.tensor_copy(out=mskA[:, c, :], in_=msk[:]), reads=[b_msk], writes=[b_mskA])
                    S.op(V, lambda e: e.tensor_copy(out=mskb[:], in_=msk[:]), reads=[b_msk], writes=[b_mskb])
                    bk, b_bk = pp.next()
                    S.mm([(bk[:, 0:32], [(trisb[:], mskb[:])]), (bk[:, 32:64], [(onesb[:], mskb[:])])],
                         reads=[b_trisb, b_onesb, b_mskb], writes=[b_bk])
                    S.op(V, lambda e: e.tensor_tensor(out=posA[:, c, :], in0=bk[:, 0:32], in1=runcnt[:], op=ALU.add),
                         reads=[b_bk, b_runcnt], writes=[b_posA])
                    S.op(V, lambda e: e.tensor_tensor(out=runcnt[:], in0=bk[:, 32:64], in1=runcnt[:], op=ALU.add),
                         reads=[b_bk, b_runcnt], writes=[b_runcnt])


                for rnd in range(NCH + 2):
                    fns = []
                    if rnd < NCH:
                        fns.append((lambda i=rnd: BX1(i), 1))
                    if 0 <= rnd - 1 < NCH:
                        fns.append((lambda i=rnd - 1: BX2(i), 1))
                    if 0 <= rnd - 2 < NCH:
                        fns.append((lambda i=rnd - 2: BY(i), 2))
                    S.weave(fns)

            if stage == 2:
                S.wait_all(G, b_y)
                return nc
            S.barrier()

            pd = ExitStack()
            with pd:
                def ld(name, shape, src):
                    t, bb = sbt(pd, name, shape, F32)
                    S.dma(SP, t[:], src, writes=[bb])
                    return t, bb
                thr, b_thr = ld("thr", [128, 16], thr_d)
                biota, b_biota = ld("biota", [128, NBLK], biota_d[:, 0:NBLK])
                pidx, b_pidx = ld("pidx", [128, 1], pidx_d)
                cmp1, b_cmp1 = sbt(pd, "cmp1", [128, NE, 16], F32)
                cmp2, b_cmp2 = sbt(pd, "cmp2", [128, NBLK, NE], F32)
                blocks, b_blocks = sbt(pd, "blocks", [128, NE], F32)
                pendb, b_pendb = sbt(pd, "pendb", [128, NE], F32)
                pstart, b_pstart = sbt(pd, "pstart", [128, NE], F32)
                ones32, b_ones32 = sbt(pd, "ones32", [128, NE], F32)
                ebf, b_ebf = sbt(pd, "ebf", [128, NBLK], F32)
                inv, b_inv = sbt(pd, "inv", [128, NBLK], F32)
                idxWf, b_idxWf = sbt(pd, "idxWf", [128, NBLK, 2], F32)
                incl, b_incl = sbt(pd, "incl", [128, NCH, NE], F32)
                m0, b_m0 = sbt(pd, "m0", [128, NCH, NE], F32)
                m1, b_m1 = sbt(pd, "m1", [128, NCH, NE], F32)
                dfull, b_dfull = sbt(pd, "dfull", [128, NCH, NE], F32)
                tmpd, b_tmpd = sbt(pd, "tmpd", [128, NCH, NE], F32)
                destf, b_destf = sbt(pd, "destf", [128, NCH, 2], F32)
                S.op(V, lambda e: e.tensor_tensor(out=cmp1[:], in0=runcnt[:].unsqueeze(2).to_broadcast([128, NE, 16]),
                                                  in1=thr[:].unsqueeze(1).to_broadcast([128, NE, 16]), op=ALU.is_gt),
                     reads=[b_runcnt, b_thr], writes=[b_cmp1])
                S.op(V, lambda e: e.tensor_reduce(out=blocks[:], in_=cmp1[:], axis=AX.X, op=ALU.add), reads=[b_cmp1], writes=[b_blocks])
                S.op(V, lambda e: e.memset(ones32[:], 1.0), writes=[b_ones32])
                S.op(V, lambda e: e.tensor_tensor_scan(out=pendb[:], data0=ones32[:], data1=blocks[:], initial=0.0,
                                                       op0=ALU.mult, op1=ALU.add),
                     reads=[b_ones32, b_blocks], writes=[b_pendb])
                S.op(V, lambda e: e.tensor_tensor(out=pstart[:], in0=pendb[:], in1=blocks[:], op=ALU.subtract),
                     reads=[b_pendb, b_blocks], writes=[b_pstart])
                S.op(V, lambda e: e.tensor_scalar(out=pstart[:], in0=pstart[:], scalar1=float(BS), scalar2=None, op0=ALU.mult),
                     reads=[b_pstart], writes=[b_pstart])
                S.op(V, lambda e: e.tensor_tensor(out=cmp2[:], in0=pendb[:].unsqueeze(1).to_broadcast([128, NBLK, NE]),
                                                  in1=biota[:].unsqueeze(2).to_broadcast([128, NBLK, NE]), op=ALU.is_le),
                     reads=[b_pendb, b_biota], writes=[b_cmp2])
                S.op(V, lambda e: e.tensor_reduce(out=ebf[:], in_=cmp2[:], axis=AX.X, op=ALU.add), reads=[b_cmp2], writes=[b_ebf])
                S.op(V, lambda e: e.tensor_scalar(out=ebf[:], in0=ebf[:], scalar1=float(NE - 1), scalar2=256.0, op0=ALU.min, op1=ALU.mult),
                     reads=[b_ebf], writes=[b_ebf])
                S.op(V, lambda e: e.tensor_scalar(out=inv[:], in0=biota[:], scalar1=pendb[:, NE - 1:NE], scalar2=1.0e6,
                                                  op0=ALU.is_ge, op1=ALU.mult), reads=[b_biota, b_pendb], writes=[b_inv])
                S.op(V, lambda e: e.tensor_tensor(out=ebf[:], in0=ebf[:], in1=inv[:], op=ALU.add), reads=[b_ebf, b_inv], writes=[b_ebf])
                S.op(V, lambda e: e.tensor_scalar(out=idxWf[:, :, 0], in0=ebf[:], scalar1=pidx[:, 0:1], scalar2=None, op0=ALU.add),
                     reads=[b_ebf, b_pidx], writes=[b_idxWf])
                S.op(V, lambda e: e.tensor_scalar(out=idxWf[:, :, 1], in0=idxWf[:, :, 0], scalar1=128.0, scalar2=None, op0=ALU.add),
                     reads=[b_idxWf], writes=[b_idxWf])
                S.op(V, lambda e: e.tensor_copy(out=idxWi[:].rearrange("p a b -> p (a b)"), in_=idxWf[:].rearrange("p a b -> p (a b)")),
                     reads=[b_idxWf], writes=[b_idxWi])
                for c in range(NCH):
                    S.op(V, lambda e: e.tensor_tensor_scan(out=incl[:, c, :], data0=ones32[:], data1=mskA[:, c, :], initial=0.0,
                                                           op0=ALU.mult, op1=ALU.add),
                         reads=[b_ones32, b_mskA], writes=[b_incl])
                fl = lambda t: t[:].rearrange("p a b -> p (a b)")
                S.op(V, lambda e: e.tensor_scalar(out=fl(m0), in0=fl(incl), scalar1=1.0, scalar2=None, op0=ALU.is_equal),
                     reads=[b_incl], writes=[b_m0])
                S.op(V, lambda e: e.tensor_tensor(out=fl(m0), in0=fl(m0), in1=fl(mskA), op=ALU.mult), reads=[b_m0, b_mskA], writes=[b_m0])
                S.op(V, lambda e: e.tensor_scalar(out=fl(m1), in0=fl(incl), scalar1=2.0, scalar2=None, op0=ALU.is_equal),
                     reads=[b_incl], writes=[b_m1])
                S.op(V, lambda e: e.tensor_tensor(out=fl(m1), in0=fl(m1), in1=fl(mskA), op=ALU.mult), reads=[b_m1, b_mskA], writes=[b_m1])
                S.op(V, lambda e: e.tensor_tensor(out=dfull[:], in0=posA[:], in1=pstart[:].unsqueeze(1).to_broadcast([128, NCH, NE]), op=ALU.add),
                     reads=[b_posA, b_pstart], writes=[b_dfull])
                for k, mk, b_mk in ((0, m0, b_m0), (1, m1, b_m1)):
                    S.op(V, lambda e: e.tensor_tensor(out=fl(tmpd), in0=fl(mk), in1=fl(dfull), op=ALU.mult), reads=[b_mk, b_dfull], writes=[b_tmpd])
                    S.op(V, lambda e: e.tensor_reduce(out=destf[:, :, k], in_=tmpd[:], axis=AX.X, op=ALU.add), reads=[b_tmpd], writes=[b_destf])
                    S.op(V, lambda e: e.tensor_tensor(out=fl(tmpd), in0=fl(mk), in1=Gt[:].rearrange("p a b -> p (a b)"), op=ALU.mult),
                         reads=[b_mk] + b_Gt, writes=[b_tmpd])
                    S.op(V, lambda e: e.tensor_reduce(out=gate2[:, :, k], in_=tmpd[:], axis=AX.X, op=ALU.add), reads=[b_tmpd], writes=[b_gate2])
                S.op(V, lambda e: e.tensor_copy(out=desti[:].rearrange("p a b -> p (a b)"), in_=destf[:].rearrange("p a b -> p (a b)")),
                     reads=[b_destf], writes=[b_desti])
                b_scat = []
                for c in range(NCH):
                    for k in range(2):
                        bb = Buf()
                        S.idma(G, reads=[b_xn3B[c], b_desti], writes=[bb], out=xbuf[:, :],
                               out_offset=bass.IndirectOffsetOnAxis(ap=desti[:, c, k:k + 1], axis=0),
                               in_=xn3B[:, c, :], in_offset=None)
                        b_scat.append(bb)
            S.barrier()

            pc = ExitStack()
            with pc:
                w1b = [sbt(pc, "w1b%d" % i, [128, 8, 512], BF16) for i in range(2)]
                w3b = [sbt(pc, "w3b%d" % i, [128, 8, 512], BF16) for i in range(2)]
                w2b = [sbt(pc, "w2b%d" % i, [128, 4, D], BF16) for i in range(2)]
                xb_r = [sbt(pc, "xb%d" % i, [128, NT, D], BF16) for i in range(2)]
                xbT_r = [sbt(pc, "xbT%d" % i, [128, 8, BS], BF16) for i in range(2)]
                hgT = [sbt(pc, "hgT%d" % i, [128, 4, BS], BF16) for i in range(2)]
                sl_t = [sbt(pc, "silu%d" % i, [128, 512], F32) for i in range(2)]
                ob_r = [sbt(pc, "ob%d" % i, [128, NT, D], F32) for i in range(1)]
                rg = [[sbt(pc, "rg%d_%d" % (i, k), [128, D], F32) for k in range(2)] for i in range(1)]
                yo = [sbt(pc, "yo%d" % i, [128, D], F32) for i in range(2)]
                st3, b_st3 = sbt(pc, "st3c", [128, 4], F32)
                pp = Pool(banks[0:7])
                bk_tr, b_tr = banks[7]
                trv = bk_tr[:].bitcast(BF16)
                b_ost = [Buf() for _ in range(NBLK)]

                for p_ in range(2):
                    for wt__ in (w1b[p_], w3b[p_], w2b[p_]):
                        S.op(V, lambda e_: e_.memset(wt__[0][:].rearrange("p a b -> p (a b)"), 0.0), writes=[wt__[1]])
                bc_reg = G.eng.to_reg(NE * 256 - 1)

                def load_w(bi):
                    p = bi % 2
                    for j in range(2):
                        io = bass.IndirectOffsetOnAxis(ap=idxWi[:, bi, j:j + 1], axis=0)
                        S.idma(G, reads=[b_idxWi], writes=[w1b[p][1]], out=w1b[p][0][:, 4 * j:4 * j + 4, :].rearrange("p a b -> p (a b)"),
                               out_offset=None, in_=w1_d[:, :], in_offset=io, bounds_check=bc_reg, oob_is_err=False)
                        S.idma(G, reads=[b_idxWi], writes=[w3b[p][1]], out=w3b[p][0][:, 4 * j:4 * j + 4, :].rearrange("p a b -> p (a b)"),
                               out_offset=None, in_=w3_d[:, :], in_offset=io, bounds_check=bc_reg, oob_is_err=False)
                        S.idma(G, reads=[b_idxWi], writes=[w2b[p][1]], out=w2b[p][0][:, 2 * j:2 * j + 2, :].rearrange("p a b -> p (a b)"),
                               out_offset=None, in_=w2_d[:, :], in_offset=io, bounds_check=bc_reg, oob_is_err=False)

                def load_x(bi):
                    t, bb = xb_r[bi % 2]
                    S.dma(SP, t[:], xbuf[bi * BS:(bi + 1) * BS, :].rearrange("(t p) d -> p t d", p=128), reads=b_scat, writes=[bb])

                load_w(0)
                load_x(0)
                for bi in range(NBLK):
                    p = bi % 2
                    if bi + 1 < NBLK:
                        load_w(bi + 1)
                        load_x(bi + 1)
                    w1t, b_w1 = w1b[p]
                    w3t, b_w3 = w3b[p]
                    w2t, b_w2 = w2b[p]
                    xb_t, b_xb = xb_r[p]
                    xbT, b_xbT = xbT_r[p]
                    hg_t, b_hg = hgT[p]
                    ob, b_ob = ob_r[0]
                    for t in range(NT):
                        S.tr([(trv[:, k * 128:(k + 1) * 128], xb_t[:, t, k * 128:(k + 1) * 128]) for k in range(8)], identb[:],
                             reads=[b_xb, b_identb], writes=[b_tr])
                        S.op(A, lambda e_: e_.copy(out=xbT[:, :, t * 128:(t + 1) * 128], in_=trv.rearrange("p (k s) -> p k s", k=8)),
                             reads=[b_tr], writes=[b_xbT])
                    for fp_ in range(2):
                        bk1, b_bk1 = pp.next()
                        bk3, b_bk3 = pp.next()
                        S.mm([(bk1[:, q_ * BS:(q_ + 1) * BS],
                               [(w1t[:, kc, (2 * fp_ + q_) * 128:(2 * fp_ + q_ + 1) * 128], xbT[:, kc, :]) for kc in range(8)])
                              for q_ in range(2)], reads=[b_w1, b_xbT], writes=[b_bk1])
                        S.mm([(bk3[:, q_ * BS:(q_ + 1) * BS],
                               [(w3t[:, kc, (2 * fp_ + q_) * 128:(2 * fp_ + q_ + 1) * 128], xbT[:, kc, :]) for kc in range(8)])
                              for q_ in range(2)], reads=[b_w3, b_xbT], writes=[b_bk3])
                        s_t, b_sl = sl_t[fp_]
                        S.op(A, lambda e_: e_.activation(out=s_t[:], in_=bk1[:], func=AF.Silu), reads=[b_bk1], writes=[b_sl])
                        S.op(V, lambda e_: e_.tensor_tensor(out=hg_t[:, 2 * fp_:2 * fp_ + 2, :].rearrange("p a b -> p (a b)"), in0=bk3[:], in1=s_t[:],
                                                            op=ALU.mult), reads=[b_bk3, b_sl], writes=[b_hg])
                    for t in range(NT):
                        for half in range(2):
                            bk, b_bk = pp.next()
                            sl = slice(half * 512, (half + 1) * 512)
                            S.mm([(bk[:], [(hg_t[:, fc, t * 128:(t + 1) * 128], w2t[:, fc, sl]) for fc in range(4)])],
                                 reads=[b_hg, b_w2], writes=[b_bk])
                            if half == 0:
                                S.op(A, lambda e_: e_.copy(out=ob[:, t, sl], in_=bk[:]), reads=[b_bk], writes=[b_ob])
                            else:
                                S.op(V, lambda e_: e_.tensor_copy(out=ob[:, t, sl], in_=bk[:]), reads=[b_bk], writes=[b_ob])
                    S.dma(SP, obuf[bi * BS:(bi + 1) * BS, :].rearrange("(t p) d -> p t d", p=128), ob[:], reads=[b_ob], writes=[b_ost[bi]])
                for c in range(NCH):
                    for k in range(2):
                        r_t, b_r = rg[0][k]
                        S.idma(G, reads=b_ost + [b_desti], writes=[b_r], out=r_t[:, :], out_offset=None, in_=obuf[:, :],
                               in_offset=bass.IndirectOffsetOnAxis(ap=desti[:, c, k:k + 1], axis=0))
                        S.op(V, lambda e_: e_.scalar_tensor_tensor(out=acc[:, c, :], in0=r_t[:], scalar=gate2[:, c, k:k + 1],
                                                                   in1=acc[:, c, :], op0=ALU.mult, op1=ALU.add),
                             reads=[b_r, b_gate2, b_acc[c]], writes=[b_acc[c]])
                    yo_t, b_yo = yo[c % 2]
                    rms_rstd(A, acc[:, c, :], b_acc[c], junk[:], b_junk, st3, b_st3, float(D))
                    S.op(V, lambda e_: e_.scalar_tensor_tensor(out=yo_t[:], in0=acc[:, c, :], scalar=st3[:, 2:3], in1=gf[:],
                                                               op0=ALU.mult, op1=ALU.mult),
                         reads=[b_acc[c], b_st3, b_gf], writes=[b_yo])
                    S.dma(SP, y_d[c * 128:(c + 1) * 128, :], yo_t[:], reads=[b_yo], writes=[b_y[c]])
                S.wait_all(SP, b_y)
    return nc


def make_in_maps(inputs):
    f = lambda a: np.ascontiguousarray(a, dtype=np.float32)
    x = f(inputs["x"])
    mem = f(inputs["mem"])

    def bt(v, n=128):
        v = f(v).reshape(1, -1)
        return np.ascontiguousarray(np.broadcast_to(v, (n, v.shape[1])))

    b_if = f(inputs["b_if"][0])
    conv_w = f(inputs["conv_w"][0])
    conv_b = f(inputs["conv_b"][0])
    w_s = f(inputs["w_s"][0])
    b_s = f(inputs["b_s"][0])
    shared = {
        "w_in": f(inputs["w_in"][0]),
        "w_out": f(inputs["w_out"][0]),
        "w_xq": f(inputs["w_xq"][0]),
        "w_xkv": f(inputs["w_xkv"][0]),
        "w_xo": f(inputs["w_xo"][0]),
        "w_r": f(np.concatenate([inputs["w_rg"][0], inputs["w_re"][0]], axis=1)),
        "w1r": f(f(inputs["w1"][0]).reshape(NE, 2, 4, 128, 512).transpose(0, 1, 3, 2, 4).reshape(NE * 256, 2048)),
        "w3r": f(f(inputs["w3"][0]).reshape(NE, 2, 4, 128, 512).transpose(0, 1, 3, 2, 4).reshape(NE * 256, 2048)),
        "w2r": f(f(inputs["w2"][0]).reshape(NE, 2, 2, 128, 1024).transpose(0, 1, 3, 2, 4).reshape(NE * 256, 2048)),
        "thr16": bt(np.arange(16, dtype=np.float32) * 256.0),
        "biota": bt(np.arange(64, dtype=np.float32)),
        "pidx": np.arange(128, dtype=np.float32).reshape(128, 1),
        "ident": np.eye(128, dtype=np.float32),
        "tri": np.triu(np.ones((128, 128), dtype=np.float32)),
        "gmix_t": bt(inputs["norm_mix_g"][0]),
        "gx_t": bt(inputs["norm_x_g"][0]),
        "gmem_t": bt(inputs["norm_mem_g"][0]),
        "gmoe_t": bt(inputs["norm_moe_g"][0]),
        "gf_t": bt(inputs["norm_f_g"]),
        "mhg_t": bt(inputs["mh_norm_g"][0]),
        "sgug_t": bt(inputs["sgu_norm_g"][0]),
        "bs_t": f(np.repeat(b_s.T[:, :, None], 128, axis=2).reshape(128, 1024)),
        "bgate_t": bt(inputs["b_gate"][0]),
        "bif_t": bt(b_if),
        "br_t": bt(np.concatenate([inputs["b_rg"][0], inputs["b_re"][0]])),
        "convw_t": f(conv_w.reshape(4, 16, 128).transpose(2, 1, 0)),
        "convb_t": f(conv_b.reshape(16, 128).T),
        "wsT": f(w_s.transpose(2, 0, 1)),
    }
    maps = []
    for c in range(NCORES):
        b, j = divmod(c, 4)
        npad = NPRE - NCH * j
        xs = np.zeros((NIT * 128, D), dtype=np.float32)
        xs[npad * 128:] = x[b, 0:(j + 1) * NCH * 128]
        pm = np.zeros((128, NIT), dtype=np.float32)
        pm[:, npad:] = 1.0
        m = dict(shared)
        m["xseq"] = xs
        m["pmask"] = pm
        m["mem_b"] = mem[b]
        maps.append(m)
    return maps


def assemble(res):
    out = np.zeros((2, 8192, D), dtype=np.float32)
    for c in range(NCORES):
        b, j = divmod(c, 4)
        out[b, j * NCH * 128:(j + 1) * NCH * 128] = res.results[c]["y"]
    return out


def build2(stage=3):
    rec = []
    build(stage, None, rec)
    return build(stage, rec)


def kernel(**inputs):
    nc = build2(3)
    maps = make_in_maps(inputs)
    res = run_bass_kernel_spmd(nc, maps, core_ids=list(range(NCORES)))
    return assemble(res)
```

```python
import numpy as np
import concourse.bass as bass
import concourse.mybir as mybir
from concourse.bass_utils import run_bass_kernel_spmd
from contextlib import ExitStack

F32 = mybir.dt.float32
BF16 = mybir.dt.bfloat16
ALU = mybir.AluOpType
AF = mybir.ActivationFunctionType
AX = mybir.AxisListType

NDS = 24
NCORES = 8
D = 1024
NCH = 16
NPRE = 48
NIT = NCH + NPRE
IN_DIM = 8200
NE = 32
EPS = 1e-6
RELAXED_SAME_ENGINE = True


class Buf:
    __slots__ = ("w", "r")

    def __init__(self):
        self.w = None
        self.r = {}


class Q:
    def __init__(self, S, name, eng):
        self.eng = eng
        self.name = name
        self.sem = S.new_sem("q_" + name)
        self.count = 0
        self.waited = {}


class Sched:
    def __init__(self, nc, es):
        self.nc = nc
        self.es = es
        self.T = Q(self, "pe", nc.tensor)
        self.V = Q(self, "dve", nc.vector)
        self.A = Q(self, "act", nc.scalar)
        self.G = Q(self, "pool", nc.gpsimd)
        self.SP = Q(self, "sp", nc.sync)
        self.dma_sems = [self.new_sem("dma%d" % i) for i in range(NDS)]
        self.dma_uses = [0] * NDS
        self.dma_next = 0
        self.dma_next_g = 0
        self._wv = None
        self._force = None
        self.pools = []
        self.noswitch = False

    def weave(self, fns):
        import threading
        if len(fns) == 1:
            fns[0][0]()
            return
        n = len(fns)
        evs = [threading.Event() for _ in range(n)]
        alive = [True] * n
        errs = []
        done = threading.Event()
        st = {"cur": 0, "cnt": 0}
        quanta = [q for _, q in fns]

        def next_alive(k):
            for d in range(1, n + 1):
                j = (k + d) % n
                if alive[j] and j != k:
                    return j
            return None

        def runner(k, fn):
            evs[k].wait()
            try:
                fn()
            except BaseException as e:
                errs.append(e)
            for pl in self.pools:
                pl.release_mine()
            alive[k] = False
            j = next_alive(k)
            if j is None:
                done.set()
            else:
                st["cur"] = j
                st["cnt"] = 0
                evs[j].set()

        def switch():
            k = st["cur"]
            st["cnt"] += 1
            if st["cnt"] < quanta[k]:
                return
            j = next_alive(k)
            if j is None:
                st["cnt"] = 0
                return
            st["cur"] = j
            st["cnt"] = 0
            evs[k].clear()
            evs[j].set()
            evs[k].wait()

        def force():
            k = st["cur"]
            j = next_alive(k)
            if j is None:
                raise RuntimeError("weave: stream blocked on a PSUM bank with no other stream alive")
            st["cur"] = j
            st["cnt"] = 0
            evs[k].clear()
            evs[j].set()
            evs[k].wait()

        self._wv = switch
        self._force = force
        ths = [threading.Thread(target=runner, args=(k, fn)) for k, (fn, _) in enumerate(fns)]
        for t in ths:
            t.start()
        evs[0].set()
        done.wait()
        for t in ths:
            t.join()
        self._wv = None
        self._force = None
        for pl in self.pools:
            pl.free = list(range(len(pl.items)))
            pl.owned = {}
        if errs:
            raise errs[0]

    def yield_now(self):
        self._force()

    def _sw(self):
        if self._wv is not None and not self.noswitch:
            self._wv()

    def new_sem(self, name):
        return self.es.enter_context(self.nc.semaphore(name))

    def _wait(self, q, tok):
        sem, val = tok
        k = id(sem)
        if q.waited.get(k, 0) >= val:
            return
        q.eng.wait_ge(sem, val)
        q.waited[k] = val

    def _deps(self, q, reads, writes):
        best = {}

        def add(tok):
            k = id(tok[0])
            if k not in best or best[k][1] < tok[1]:
                best[k] = tok

        for b in reads:
            if b.w is not None:
                add(b.w)
        relaxed = RELAXED_SAME_ENGINE and q in (self.T, self.V, self.A)
        for b in writes:
            if b.w is not None and not (relaxed and b.w[0] is q.sem):
                add(b.w)
            for t in b.r.values():
                if not (relaxed and t[0] is q.sem):
                    add(t)
        for tok in best.values():
            if RELAXED_SAME_ENGINE and q is self.T and tok[0] is q.sem:
                continue
            self._wait(q, tok)

    def _commit(self, tok, reads, writes):
        for b in writes:
            b.w = tok
            b.r = {}
        k = id(tok[0])
        for b in reads:
            if b not in writes:
                b.r[k] = tok

    def op(self, q, fn, reads=(), writes=()):
        self._deps(q, reads, writes)
        ins = fn(q.eng)
        q.count += 1
        ins.then_inc(q.sem, 1)
        self._commit((q.sem, q.count), reads, writes)
        self._sw()

    def mm(self, groups, reads=(), writes=()):
        q = self.T
        self._deps(q, reads, writes)
        ins = None
        for out, pairs in groups:
            n = len(pairs)
            for i, (l, r) in enumerate(pairs):
                ins = q.eng.matmul(out, lhsT=l, rhs=r, start=(i == 0), stop=(i == n - 1))
        q.count += 1
        ins.then_inc(q.sem, 1)
        self._commit((q.sem, q.count), reads, writes)
        self._sw()

    def tr(self, items, ident, reads=(), writes=()):
        q = self.T
        self._deps(q, reads, writes)
        ins = None
        for out, in_ in items:
            ins = q.eng.transpose(out=out, in_=in_, identity=ident)
        q.count += 1
        ins.then_inc(q.sem, 1)
        self._commit((q.sem, q.count), reads, writes)
        self._sw()

    def dma(self, q, out, in_, reads=(), writes=()):
        self._deps(q, reads, writes)
        half = NDS // 2
        if q is self.G:
            i = half + self.dma_next_g
            self.dma_next_g = (self.dma_next_g + 1) % half
        else:
            i = self.dma_next
            self.dma_next = (self.dma_next + 1) % half
        sem = self.dma_sems[i]
        if self.dma_uses[i] > 0:
            self._wait(q, (sem, 16 * self.dma_uses[i]))
        q.eng.dma_start(out=out, in_=in_).then_inc(sem, 16)
        self.dma_uses[i] += 1
        tok = (sem, 16 * self.dma_uses[i])
        self._commit(tok, reads, writes)
        self._sw()

    def idma(self, q, reads=(), writes=(), **kw):
        self._deps(q, reads, writes)
        half = NDS // 2
        i = half + self.dma_next_g
        self.dma_next_g = (self.dma_next_g + 1) % half
        sem = self.dma_sems[i]
        if self.dma_uses[i] > 0:
            self._wait(q, (sem, 16 * self.dma_uses[i]))
        q.eng.indirect_dma_start(**kw).then_inc(sem, 16)
        self.dma_uses[i] += 1
        self._commit((sem, 16 * self.dma_uses[i]), reads, writes)
        self._sw()

    def barrier(self):
        qs = [self.T, self.V, self.A, self.G, self.SP]
        toks = [(q.sem, q.count) for q in qs if q.count > 0]
        toks += [(self.dma_sems[i], 16 * self.dma_uses[i]) for i in range(NDS) if self.dma_uses[i] > 0]
        for q in qs:
            for tok in toks:
                if tok[0] is q.sem:
                    continue
                self._wait(q, tok)

    def wait_all(self, q, bufs):
        for b in bufs:
            if b.w is not None:
                self._wait(q, b.w)


class Pool:
    def __init__(self, items, S=None, hold=1):
        self.items = items
        self.i = 0
        self.S = S
        self.hold = hold
        self.free = list(range(len(items)))
        self.owned = {}
        if S is not None:
            S.pools.append(self)

    def next(self):
        S = self.S
        if S is None or S._wv is None:
            it = self.items[self.i]
            self.i = (self.i + 1) % len(self.items)
            return it
        import threading
        tid = threading.get_ident()
        mine = self.owned.setdefault(tid, [])
        while len(mine) >= self.hold:
            self.free.append(mine.pop(0))
        while not self.free:
            S.yield_now()
        k = self.free.pop(0)
        mine.append(k)
        return self.items[k]

    def release_mine(self):
        import threading
        mine = self.owned.get(threading.get_ident(), [])
        while mine:
            self.free.append(mine.pop(0))


def build(stage=3, sseq_in=None, srec=None):
    if srec is None:
        srec = []
    nc = bass.Bass("TRN2", target_bir_lowering=False)

    def din(name, shape):
        return nc.dram_tensor(name, list(shape), F32, kind="ExternalInput").ap()

    xseq = din("xseq", [NIT * 128, D])
    pmask_d = din("pmask", [128, NIT])
    mem_d = din("mem_b", [256, D])
    w_in_d = din("w_in", [D, IN_DIM])
    w_out_d = din("w_out", [D, D])
    w_xq_d = din("w_xq", [D, D])
    w_xkv_d = din("w_xkv", [D, 2 * D])
    w_xo_d = din("w_xo", [D, D])
    w_r_d = din("w_r", [D, 36])
    w1_d = din("w1r", [NE * 2 * 128, 2048])
    w3_d = din("w3r", [NE * 2 * 128, 2048])
    w2_d = din("w2r", [NE * 2 * 128, 2048])
    thr_d = din("thr16", [128, 16])
    biota_d = din("biota", [128, 64])
    pidx_d = din("pidx", [128, 1])
    ident_d = din("ident", [128, 128])
    tri_d = din("tri", [128, 128])
    gmix_d = din("gmix_t", [128, D])
    gx_d = din("gx_t", [128, D])
    gmem_d = din("gmem_t", [128, D])
    gmoe_d = din("gmoe_t", [128, D])
    gf_d = din("gf_t", [128, D])
    mhg_d = din("mhg_t", [128, D])
    sgug_d = din("sgug_t", [128, D])
    bs_d = din("bs_t", [128, D])
    bgate_d = din("bgate_t", [128, 2 * D])
    bif_d = din("bif_t", [128, 8])
    br_d = din("br_t", [128, 36])
    convw_d = din("convw_t", [128, 16, 4])
    convb_d = din("convb_t", [128, 16])
    wsT_d = din("wsT", [128, 8, 128])
    y_d = nc.dram_tensor("y", [NCH * 128, D], F32, kind="ExternalOutput").ap()
    x1s = nc.dram_tensor("x1s", [NCH * 128, D], F32, kind="Internal").ap()
    wst = nc.dram_tensor("wst", [14, 128, 8, 512], BF16, kind="Internal").ap()
    BS = 256
    NT = BS // 128
    NBLK = (NCH * 128 * 2) // BS + NE
    xbuf = nc.dram_tensor("xbuf", [NBLK * BS, D], BF16, kind="Internal").ap()
    obuf = nc.dram_tensor("obuf", [NBLK * BS, D], F32, kind="Internal").ap()
    I32 = mybir.dt.int32

    es = ExitStack()
    with es:
        S = Sched(nc, es)
        T, V, A, G, SP = S.T, S.V, S.A, S.G, S.SP

        def sbt(stack, name, shape, dt):
            return stack.enter_context(nc.sbuf_tensor("s_" + name, list(shape), dt)), Buf()

        def pst(stack, name, shape, dt):
            return stack.enter_context(nc.psum_tensor(name, list(shape), dt)), Buf()

        identf, b_identf = sbt(es, "identf", [128, 128], F32)
        identb, b_identb = sbt(es, "identb", [128, 128], BF16)
        trif, b_trif = sbt(es, "trif", [128, 128], F32)
        trib, b_trib = sbt(es, "trib", [128, 128], BF16)
        onesf, b_onesf = sbt(es, "onesf", [128, 128], F32)
        S.dma(SP, identf[:], ident_d, writes=[b_identf])
        S.dma(SP, trif[:], tri_d, writes=[b_trif])
        S.op(V, lambda e: e.tensor_copy(out=identb[:], in_=identf[:]), reads=[b_identf], writes=[b_identb])
        S.op(V, lambda e: e.tensor_copy(out=trib[:], in_=trif[:]), reads=[b_trif], writes=[b_trib])
        S.op(V, lambda e: e.memset(onesf[:], 1.0), writes=[b_onesf])
        epsc, b_epsc = sbt(es, "epsc", [128, 1], F32)
        S.op(V, lambda e: e.memset(epsc[:], EPS), writes=[b_epsc])

        banks = []
        for i in range(8):
            t, b = pst(es, "bank%d" % i, [128, 512], F32)
            banks.append((t, b))

        def rms_rstd(q_sq, xin_ap, b_xin, junk, b_junk, st, b_st, n):
            S.op(A, lambda e: e.activation(out=junk, in_=xin_ap, func=AF.Square, accum_out=st[:, 0:1]),
                 reads=[b_xin], writes=[b_junk, b_st])
            S.op(A, lambda e: e.activation(out=st[:, 3:4], in_=st[:, 0:1], func=AF.Sqrt, scale=1.0 / n, bias=epsc[:, 0:1]),
                 reads=[b_st, b_epsc], writes=[b_st])
            S.op(V, lambda e: e.reciprocal(out=st[:, 2:3], in_=st[:, 3:4]), reads=[b_st], writes=[b_st])

        pa = ExitStack()
        with pa:
            w_in_v = w_in_d.rearrange("(k p) n -> p k n", p=128)
            Wk, b_wk = sbt(pa, "Wk", [128, 8, 1024], BF16)
            Wv, b_wv = sbt(pa, "Wv", [128, 8, 1024], BF16)
            Wif, b_wif = sbt(pa, "Wif", [128, 8, 8], BF16)
            S.dma(G, Wk[:], w_in_v[:, :, 1024:2048], writes=[b_wk])
            S.dma(G, Wv[:], w_in_v[:, :, 2048:3072], writes=[b_wv])
            S.dma(G, Wif[:], w_in_v[:, :, 4096:4104], writes=[b_wif])
            GCOLS = [0, 512, 3072, 3584, 4104, 4616, 5128, 5640, 6152, 6664, 7176, 7688]
            NSG = 14
            w_out_v = w_out_d.rearrange("(k p) n -> p k n", p=128)
            NSB = 3
            wsb = [sbt(pa, "wsb%d" % i, [128, 8, 512], BF16) for i in range(NSB)]
            b_wst = [Buf() for _ in range(NSG)]
            def stage_group(g):
                sg_t, b_sg = wsb[g % NSB]
                src = w_in_v[:, :, GCOLS[g]:GCOLS[g] + 512] if g < 12 else w_out_v[:, :, (g - 12) * 512:(g - 11) * 512]
                S.dma(G, sg_t[:], src, writes=[b_sg])
                S.dma(SP, wst[g], sg_t[:], reads=[b_sg], writes=[b_wst[g]])
            sstate = {"issued": 0, "used": 0}
            sseq = list(sseq_in) if sseq_in is not None else None

            def stream_issue(upto):
                while sstate["issued"] < min(upto, len(sseq)):
                    n = sstate["issued"]
                    t, b = wsb[n % NSB]
                    S.dma(SP, t[:], wst[sseq[n]], reads=[b_wst[sseq[n]]], writes=[b])
                    sstate["issued"] += 1

            def stream_next(g):
                n = sstate["used"]
                S.noswitch = True
                if sseq is None:
                    srec.append(g)
                    t, b = wsb[n % NSB]
                    S.dma(SP, t[:], wst[g], reads=[b_wst[g]], writes=[b])
                else:
                    assert sseq[n] == g, (n, g, sseq[n])
                    stream_issue(n + NSB)
                S.noswitch = False
                sstate["used"] += 1
                return wsb[n % NSB]

            def cload(name, shape, src, stack=pa):
                t, b = sbt(stack, name, shape, F32)
                S.dma(SP, t[:], src, writes=[b])
                return t, b

            gmix, b_gmix = cload("gmix", [128, D], gmix_d)
            mhg, b_mhg = cload("mhg", [128, D], mhg_d)
            sgug, b_sgug = cload("sgug", [128, D], sgug_d)
            bst, b_bst = cload("bst", [128, D], bs_d)
            bgate, b_bgate = cload("bgate", [128, 2 * D], bgate_d)
            bif, b_bif = cload("bif", [128, 8], bif_d)
            convw, b_convw = cload("convw", [128, 16, 4], convw_d)
            convb, b_convb = cload("convb", [128, 16], convb_d)
            pmask, b_pmask = cload("pmask", [128, NIT], pmask_d)
            wsTf, b_wsTf = cload("wsTf", [128, 8, 128], wsT_d)
            wsT, b_wsT = sbt(pa, "wsT", [128, 8, 128], BF16)
            for g in range(8):
                S.op(V, lambda e: e.tensor_tensor(out=wsT[:, g, :], in0=wsTf[:, g, :], in1=trif[:], op=ALU.mult),
                     reads=[b_wsTf, b_trif], writes=[b_wsT])

            R3 = 4
            RG = 3
            xt = [sbt(pa, "xt%d" % i, [128, D], F32) for i in range(R3)]
            xT = [sbt(pa, "xT%d" % i, [128, 8, 128], BF16) for i in range(R3)]
            st = [sbt(pa, "st%d" % i, [128, 4], F32) for i in range(2)]
            junk, b_junk = sbt(pa, "junk", [128, D], BF16)
            junk3, b_junk3 = sbt(pa, "junk3", [128, D], BF16)
            xn, b_xn = sbt(pa, "xn", [128, D], BF16)
            pre_t, b_pre = sbt(pa, "pre", [128, 16, 131], F32)
            halo, b_halo = sbt(pa, "halo", [128, 16, 3], F32)
            cacc, _ = sbt(pa, "cacc", [128, 16, 128], F32)
            b_cacc = [Buf() for _ in range(16)]
            qkT_r = [sbt(pa, "qkT%d" % i, [128, 16, 128], BF16) for i in range(2)]
            vaug_r = [sbt(pa, "vaug%d" % i, [128, 4, 257], F32) for i in range(2)]
            gif_r = [sbt(pa, "gif%d" % i, [128, 8], F32) for i in range(RG)]
            lsp_r = [sbt(pa, "lsp%d" % i, [128, 8], F32) for i in range(RG)]
            anb_r = [sbt(pa, "anb%d" % i, [128, 8], F32) for i in range(RG)]
            sm_r = [sbt(pa, "sm%d" % i, [4, 16], F32) for i in range(RG)]
            vw, _ = sbt(pa, "vw", [128, 4, 257], BF16)
            b_vw = [Buf() for _ in range(4)]
            eac, b_eac = sbt(pa, "eac", [128, 8], F32)
            bc, b_bc = sbt(pa, "bc", [128, 8], F32)
            mst, b_mst = sbt(pa, "mst", [4, 1], F32)
            rhs8, b_rhs8 = sbt(pa, "rhs8", [4, 8], F32)
            Cm, b_Cm0 = sbt(pa, "Cm", [128, 4, 2, 257], F32)
            b_Cm = [Buf() for _ in range(4)]
            Cb, b_Cb = sbt(pa, "Cb", [128, 2, 257], BF16)
            ktok, b_ktok = sbt(pa, "ktok", [128, 256], BF16)
            PT, b_PT = sbt(pa, "PT", [128, 128], BF16)
            ya_r = [sbt(pa, "ya%d" % i, [128, D], F32) for i in range(2)]
            hs_r = [sbt(pa, "hs%d" % i, [128, 16], F32) for i in range(2)]
            st3, b_st3 = sbt(pa, "st3", [128, 4], F32)
            so, b_so = sbt(pa, "so", [128, D], F32)
            ub, b_ub = sbt(pa, "ub", [128, D], F32)
            svb, b_svb = sbt(pa, "svb", [128, D], F32)
            svn, b_svn = sbt(pa, "svn", [128, D], BF16)
            gt, b_gt = sbt(pa, "gt", [128, 2 * D], F32)
            t1, b_t1 = sbt(pa, "t1", [128, 512], F32)
            t2, b_t2 = sbt(pa, "t2", [128, 512], F32)
            t3, b_t3 = sbt(pa, "t3", [128, 512], F32)
            yb, b_yb = sbt(pa, "yb", [128, D], F32)
            zb, b_zb = sbt(pa, "zb", [128, D], BF16)
            zT, b_zT = sbt(pa, "zT", [128, 8, 128], BF16)

            S.op(V, lambda e: e.memset(Cm[:].rearrange("p a b c -> p (a b c)"), 0.0), writes=b_Cm)
            S.op(V, lambda e: e.memset(mst[:], 0.0), writes=[b_mst])
            for i in range(2):
                S.op(V, lambda e: e.memset(vaug_r[i][0][:].rearrange("p a b -> p (a b)"), 1.0), writes=[vaug_r[i][1]])
            S.op(V, lambda e: e.memset(halo[:].rearrange("p a b -> p (a b)"), 0.0), writes=[b_halo])

            pp = Pool(banks[0:3], S, hold=1)
            trp = Pool(banks[3:4], S, hold=1)
            bk_tr, b_tr = banks[3]
            bk_s, b_s = banks[4]
            bk_num, b_num = banks[5]
            bk_kv0, b_kv0 = banks[6]
            bk_kv1, b_kv1 = banks[7]
            trv = bk_tr[:].bitcast(BF16)
            kv1_bf = bk_kv1[:].bitcast(BF16)

            b_x1s = [Buf() for _ in range(NCH)]

            def gelu_group(src_ps, b_src, dst, b_dst):
                S.op(A, lambda e: e.copy(out=t1[:], in_=src_ps), reads=[b_src], writes=[b_t1])
                S.op(A, lambda e: e.activation(out=t2[:], in_=t1[:], func=AF.Square), reads=[b_t1], writes=[b_t2])
                S.op(V, lambda e: e.tensor_scalar(out=t2[:], in0=t2[:], scalar1=0.044715, scalar2=1.0,
                                                  op0=ALU.mult, op1=ALU.add), reads=[b_t2], writes=[b_t2])
                S.op(V, lambda e: e.tensor_tensor(out=t3[:], in0=t2[:], in1=t1[:], op=ALU.mult),
                     reads=[b_t2, b_t1], writes=[b_t3])
                S.op(A, lambda e: e.activation(out=t3[:], in_=t3[:], func=AF.Sigmoid, scale=1.5957691216057308),
                     reads=[b_t3], writes=[b_t3])
                S.op(V, lambda e: e.tensor_tensor(out=dst, in0=t3[:], in1=t1[:], op=ALU.mult),
                     reads=[b_t3, b_t1], writes=[b_dst])

            def proj_w(xT_t, b_xTt, wt_, b_wt, c0, ncols):
                bk, b_bk = pp.next()
                S.mm([(bk[:, 0:ncols], [(xT_t[:, kc, :], wt_[:, kc, c0:c0 + ncols]) for kc in range(8)])],
                     reads=[b_xTt, b_wt], writes=[b_bk])
                return bk, b_bk

            def P1a(it):
                r2 = it % 2
                xt_t, b_xt = xt[it % R3]
                xT_t, b_xTt = xT[it % R3]
                st_t, b_stt = st[r2]
                gif, b_gif = gif_r[it % RG]
                lsp, b_lsp = lsp_r[it % RG]
                anb, b_anb = anb_r[it % RG]
                sm, b_sm = sm_r[it % RG]
                S.dma(SP, xt_t[:], xseq[it * 128:(it + 1) * 128, :], writes=[b_xt])
                rms_rstd(A, xt_t[:], b_xt, junk[:], b_junk, st_t, b_stt, float(D))
                S.op(V, lambda e: e.scalar_tensor_tensor(out=xn[:], in0=xt_t[:], scalar=st_t[:, 2:3], in1=gmix[:],
                                                         op0=ALU.mult, op1=ALU.mult),
                     reads=[b_xt, b_stt, b_gmix], writes=[b_xn])
                trp.next()
                S.tr([(trv[:, k * 128:(k + 1) * 128], xn[:, k * 128:(k + 1) * 128]) for k in range(8)], identb[:],
                     reads=[b_xn, b_identb], writes=[b_tr])
                S.op(A, lambda e: e.copy(out=xT_t[:].rearrange("p k t -> p (k t)"), in_=trv), reads=[b_tr], writes=[b_xTt])
                trp.release_mine()
                bk, b_bk = proj_w(xT_t, b_xTt, Wif, b_wif, 0, 8)
                S.op(V, lambda e: e.tensor_tensor(out=gif[:], in0=bk[:, 0:8], in1=bif[:], op=ALU.add),
                     reads=[b_bk, b_bif], writes=[b_gif])
                S.op(A, lambda e: e.activation(out=lsp[:, 0:4], in_=gif[:, 4:8], func=AF.Exp, scale=-1.0),
                     reads=[b_gif], writes=[b_lsp])
                S.op(A, lambda e: e.activation(out=lsp[:, 4:8], in_=lsp[:, 0:4], func=AF.Ln, bias=1.0),
                     reads=[b_lsp], writes=[b_lsp])
                bkg, b_bkg = pp.next()
                S.mm([(bkg[:, 128:132], [(trif[:], lsp[:, 4:8])]),
                      (bkg[0:4, 136:137], [(lsp[:, 4:8], onesf[:, 0:1])])],
                     reads=[b_trif, b_lsp, b_onesf], writes=[b_bkg])
                S.op(V, lambda e: e.tensor_copy(out=anb[:, 4:8], in_=bkg[:, 128:132]), reads=[b_bkg], writes=[b_anb])
                S.op(V, lambda e: e.tensor_tensor(out=anb[:, 0:4], in0=bkg[:, 128:132], in1=gif[:, 0:4], op=ALU.add),
                     reads=[b_bkg, b_gif], writes=[b_anb])
                S.op(V, lambda e: e.tensor_copy(out=sm[:, 0:1], in_=bkg[0:4, 136:137]), reads=[b_bkg], writes=[b_sm])
                S.tr([(bkg[0:4, 256:384], anb[:, 0:4])], identf[:], reads=[b_anb, b_identf], writes=[b_bkg])
                S.op(V, lambda e: e.tensor_reduce(out=sm[:, 1:2], in_=bkg[0:4, 256:384], axis=AX.X, op=ALU.max),
                     reads=[b_bkg], writes=[b_sm])
                pp.release_mine()

            def P1b(it):
                main = it >= NPRE
                r2 = it % 2
                xT_t, b_xTt = xT[it % R3]
                qkT, b_qkT = qkT_r[r2]
                vaug, b_vaug = vaug_r[r2]
                nlist = list(range(16)) if (main or it == NPRE - 1) else list(range(8, 16))
                n0 = nlist[0]
                S.op(G, lambda e: e.tensor_copy(out=pre_t[:, n0:16, 0:3], in_=halo[:, n0:16, :]),
                     reads=[b_halo], writes=[b_pre])
                for g0 in range(0, len(nlist), 4):
                    grp = nlist[g0:g0 + 4]
                    bk, b_bk = pp.next()
                    if grp[0] >= 8:
                        wt_, b_wt = Wk, b_wk
                        off = (grp[0] - 8) * 128
                    else:
                        wt_, b_wt = stream_next(grp[0] // 4)
                        off = 0
                    S.mm([(bk[:, j * 128:(j + 1) * 128],
                           [(wt_[:, kc, off + j * 128:off + (j + 1) * 128], xT_t[:, kc, :]) for kc in range(8)])
                          for j in range(4)],
                         reads=[b_xTt, b_wt], writes=[b_bk])
                    S.op(A, lambda e: e.copy(out=pre_t[:, grp[0]:grp[0] + 4, 3:131],
                                             in_=bk[:].rearrange("p (a b) -> p a b", a=4)),
                         reads=[b_bk], writes=[b_pre])
                S.op(G, lambda e: e.tensor_copy(out=halo[:, n0:16, :], in_=pre_t[:, n0:16, 128:131]),
                     reads=[b_pre], writes=[b_halo])
                for i in nlist:
                    S.op(A, lambda e: e.activation(out=cacc[:, i, :], in_=pre_t[:, i, 3:131], func=AF.Identity,
                                                   scale=convw[:, i, 3:4], bias=convb[:, i:i + 1]),
                         reads=[b_pre, b_convw, b_convb], writes=[b_cacc[i]])
                for j in range(3):
                    for i in nlist:
                        S.op(V, lambda e: e.scalar_tensor_tensor(out=cacc[:, i, :], in0=pre_t[:, i, j:j + 128],
                                                                 scalar=convw[:, i, j:j + 1], in1=cacc[:, i, :],
                                                                 op0=ALU.mult, op1=ALU.add),
                             reads=[b_pre, b_convw, b_cacc[i]], writes=[b_cacc[i]])
                S.op(A, lambda e: e.activation(out=qkT[:, n0:16, :].rearrange("p a b -> p (a b)"),
                                               in_=cacc[:, n0:16, :].rearrange("p a b -> p (a b)"), func=AF.Silu),
                     reads=[b_cacc[i] for i in nlist], writes=[b_qkT])
                for hgp in range(2):
                    bk, b_bk = proj_w(xT_t, b_xTt, Wv, b_wv, hgp * 512, 512)
                    S.op(A, lambda e: e.copy(out=vaug[:, 2 * hgp:2 * hgp + 2, 0:256],
                                             in_=bk[:].rearrange("p (a b) -> p a b", a=2)),
                         reads=[b_bk], writes=[b_vaug])
                pp.release_mine()

            def P2(it):
                main = it >= NPRE
                r2 = it % 2
                qkT, b_qkT = qkT_r[r2]
                vaug, b_vaug = vaug_r[r2]
                gif, b_gif = gif_r[it % RG]
                anb, b_anb = anb_r[it % RG]
                sm, b_sm = sm_r[it % RG]
                hbuf, b_hbuf = ya_r[r2]
                hs, b_hs = hs_r[r2]
                S.op(V, lambda e: e.tensor_tensor(out=sm[:, 2:3], in0=sm[:, 1:2], in1=mst[:], op=ALU.max),
                     reads=[b_sm, b_mst], writes=[b_sm])
                S.op(V, lambda e: e.tensor_scalar(out=sm[:, 3:4], in0=sm[:, 2:3], scalar1=-1.0, scalar2=None, op0=ALU.mult),
                     reads=[b_sm], writes=[b_sm])
                S.op(A, lambda e: e.activation(out=sm[:, 4:5], in_=mst[:], func=AF.Exp, bias=sm[:, 3:4]),
                     reads=[b_sm, b_mst], writes=[b_sm])
                S.op(V, lambda e: e.tensor_tensor(out=mst[:], in0=sm[:, 2:3], in1=sm[:, 0:1], op=ALU.subtract),
                     reads=[b_sm], writes=[b_mst])
                S.op(V, lambda e: e.tensor_scalar(out=rhs8[:, 0:4], in0=identf[0:4, 0:4], scalar1=sm[:, 3:4], scalar2=None,
                                                  op0=ALU.mult), reads=[b_sm, b_identf], writes=[b_rhs8])
                S.op(V, lambda e: e.tensor_scalar(out=rhs8[:, 4:8], in0=identf[0:4, 0:4], scalar1=sm[:, 4:5], scalar2=None,
                                                  op0=ALU.mult), reads=[b_sm, b_identf], writes=[b_rhs8])
                S.mm([(bk_s[:, 144:152], [(onesf[0:4, :], rhs8[:])])], reads=[b_onesf, b_rhs8], writes=[b_s])
                S.op(V, lambda e: e.tensor_copy(out=bc[:], in_=bk_s[:, 144:152]), reads=[b_s], writes=[b_bc])
                S.op(V, lambda e: e.tensor_tensor(out=eac[:, 0:4], in0=anb[:, 0:4], in1=bc[:, 0:4], op=ALU.add),
                     reads=[b_anb, b_bc], writes=[b_eac])
                S.op(V, lambda e: e.tensor_tensor(out=eac[:, 4:8], in0=anb[:, 4:8], in1=bc[:, 0:4], op=ALU.add),
                     reads=[b_anb, b_bc], writes=[b_eac])
                S.op(A, lambda e: e.activation(out=eac[:], in_=eac[:], func=AF.Exp), reads=[b_eac], writes=[b_eac])
                if not main:
                    S.op(V, lambda e: e.tensor_scalar(out=eac[:, 0:4], in0=eac[:, 0:4], scalar1=pmask[:, it:it + 1],
                                                      scalar2=None, op0=ALU.mult),
                         reads=[b_eac, b_pmask], writes=[b_eac])
                for h in range(4):
                    S.op(V, lambda e: e.tensor_scalar(out=vw[:, h, :], in0=vaug[:, h, :], scalar1=eac[:, h:h + 1],
                                                      scalar2=None, op0=ALU.mult),
                         reads=[b_vaug, b_eac], writes=[b_vw[h]])
                for h in range(4):
                    S.tr([(kv1_bf[:, 640 + dc * 128:640 + (dc + 1) * 128], qkT[:, 8 + 2 * h + dc, :]) for dc in range(2)],
                         identb[:], reads=[b_qkT, b_identb], writes=[b_kv1])
                    S.op(A, lambda e: e.mul(out=ktok[:], in_=kv1_bf[:, 640:896], mul=0.0625), reads=[b_kv1], writes=[b_ktok])
                    if main:
                        S.mm([(bk_s[:, 0:128], [(qkT[:, 8 + 2 * h + dc, :], qkT[:, 2 * h + dc, :]) for dc in range(2)])],
                             reads=[b_qkT], writes=[b_s])
                        S.op(V, lambda e: e.scalar_tensor_tensor(out=PT[:], in0=bk_s[:, 0:128], scalar=0.0625, in1=trif[:],
                                                                 op0=ALU.mult, op1=ALU.mult),
                             reads=[b_s, b_trif], writes=[b_PT])
                        S.op(V, lambda e: e.tensor_scalar(out=Cb[:].rearrange("p a b -> p (a b)"),
                                                          in0=Cm[:, h, :, :].rearrange("p a b -> p (a b)"),
                                                          scalar1=bc[:, 4 + h:5 + h], scalar2=None, op0=ALU.mult),
                             reads=[b_Cm[h], b_bc], writes=[b_Cb])
                        S.mm([(bk_num[:, 0:257], [(PT[:], vw[:, h, :]),
                                                  (qkT[:, 2 * h, :], Cb[:, 0, :]),
                                                  (qkT[:, 2 * h + 1, :], Cb[:, 1, :])])],
                             reads=[b_PT, b_vw[h], b_qkT, b_Cb], writes=[b_num])
                    S.mm([(bk_kv0[:, 0:257], [(ktok[:, 0:128], vw[:, h, :])]),
                          (bk_kv1[:, 0:257], [(ktok[:, 128:256], vw[:, h, :])])],
                         reads=[b_ktok, b_vw[h]], writes=[b_kv0, b_kv1])
                    S.op(V, lambda e: e.scalar_tensor_tensor(out=Cm[:, h, 0, :], in0=Cm[:, h, 0, :], scalar=bc[:, 4 + h:5 + h],
                                                             in1=bk_kv0[:, 0:257], op0=ALU.mult, op1=ALU.add),
                         reads=[b_Cm[h], b_bc, b_kv0], writes=[b_Cm[h]])
                    S.op(V, lambda e: e.scalar_tensor_tensor(out=Cm[:, h, 1, :], in0=Cm[:, h, 1, :], scalar=bc[:, 4 + h:5 + h],
                                                             in1=bk_kv1[:, 0:257], op0=ALU.mult, op1=ALU.add),
                         reads=[b_Cm[h], b_bc, b_kv1], writes=[b_Cm[h]])
                    if main:
                        S.op(V, lambda e: e.tensor_scalar(out=hs[:, 8 + h:9 + h], in0=bk_num[:, 256:257], scalar1=-1.0,
                                                          scalar2=None, op0=ALU.mult), reads=[b_num], writes=[b_hs])
                        S.op(V, lambda e: e.tensor_tensor(out=hs[:, 8 + h:9 + h], in0=hs[:, 8 + h:9 + h],
                                                          in1=bk_num[:, 256:257], op=ALU.max), reads=[b_num, b_hs], writes=[b_hs])
                        S.op(V, lambda e: e.tensor_tensor(out=hs[:, 8 + h:9 + h], in0=hs[:, 8 + h:9 + h],
                                                          in1=eac[:, 4 + h:5 + h], op=ALU.max),
                             reads=[b_hs, b_eac], writes=[b_hs])
                        S.op(V, lambda e: e.reciprocal(out=hs[:, 12 + h:13 + h], in_=hs[:, 8 + h:9 + h]),
                             reads=[b_hs], writes=[b_hs])
                        S.op(A, lambda e: e.activation(out=hbuf[:, h * 256:(h + 1) * 256], in_=bk_num[:, 0:256],
                                                       func=AF.Copy, scale=hs[:, 12 + h:13 + h]),
                             reads=[b_num, b_hs], writes=[b_hbuf])
                        S.op(A, lambda e: e.activation(out=junk[:, 0:256], in_=hbuf[:, h * 256:(h + 1) * 256],
                                                       func=AF.Square, accum_out=hs[:, h:h + 1]),
                             reads=[b_hbuf], writes=[b_junk, b_hs])

            def P3(it):
                c = it - NPRE
                r2 = it % 2
                xt_t, b_xt = xt[it % R3]
                xT_t, b_xTt = xT[it % R3]
                ya, b_ya = ya_r[r2]
                hs, b_hs = hs_r[r2]

                def proj_s(g):
                    wt_, b_wt = stream_next(g)
                    return proj_w(xT_t, b_xTt, wt_, b_wt, 0, 512)

                for hgp in range(2):
                    bk, b_bk = proj_s(2 + hgp)
                    S.op(A, lambda e: e.activation(out=so[:, hgp * 512:(hgp + 1) * 512], in_=bk[:], func=AF.Sigmoid),
                         reads=[b_bk], writes=[b_so])
                for hgp in range(2):
                    bk, b_bk = proj_s(4 + hgp)
                    gelu_group(bk[:], b_bk, ub[:, hgp * 512:(hgp + 1) * 512], b_ub)
                for hgp in range(2):
                    bk, b_bk = proj_s(6 + hgp)
                    gelu_group(bk[:], b_bk, svb[:, hgp * 512:(hgp + 1) * 512], b_svb)
                for hgp in range(4):
                    bk, b_bk = proj_s(8 + hgp)
                    S.op(V, lambda e: e.tensor_tensor(out=gt[:, hgp * 512:(hgp + 1) * 512], in0=bk[:],
                                                      in1=bgate[:, hgp * 512:(hgp + 1) * 512], op=ALU.add),
                         reads=[b_bk, b_bgate], writes=[b_gt])
                S.op(A, lambda e: e.activation(out=gt[:], in_=gt[:], func=AF.Sigmoid), reads=[b_gt], writes=[b_gt])
                rms_rstd(A, svb[:], b_svb, junk3[:], b_junk3, st3, b_st3, float(D))
                S.op(V, lambda e: e.scalar_tensor_tensor(out=svn[:], in0=svb[:], scalar=st3[:, 2:3], in1=sgug[:],
                                                         op0=ALU.mult, op1=ALU.mult),
                     reads=[b_svb, b_st3, b_sgug], writes=[b_svn])
                for half in range(2):
                    bk, b_bk = pp.next()
                    S.mm([(bk[:, j * 128:(j + 1) * 128], [(wsT[:, half * 4 + j, :], svn[:, (half * 4 + j) * 128:(half * 4 + j + 1) * 128])])
                          for j in range(4)], reads=[b_wsT, b_svn], writes=[b_bk])
                    sl = slice(half * 512, (half + 1) * 512)
                    S.op(V, lambda e: e.tensor_tensor(out=yb[:, sl], in0=bk[:], in1=bst[:, sl], op=ALU.add),
                         reads=[b_bk, b_bst], writes=[b_yb])
                S.op(G, lambda e: e.tensor_tensor(out=yb[:], in0=yb[:], in1=ub[:], op=ALU.mult), reads=[b_yb, b_ub], writes=[b_yb])
                S.op(G, lambda e: e.tensor_tensor(out=yb[:], in0=yb[:], in1=gt[:, D:2 * D], op=ALU.mult), reads=[b_yb, b_gt], writes=[b_yb])
                S.op(V, lambda e: e.tensor_scalar(out=hs[:, 4:8], in0=hs[:, 0:4], scalar1=1.0 / 256, scalar2=EPS,
                                                  op0=ALU.mult, op1=ALU.add), reads=[b_hs], writes=[b_hs])
                S.op(A, lambda e: e.activation(out=hs[:, 4:8], in_=hs[:, 4:8], func=AF.Sqrt), reads=[b_hs], writes=[b_hs])
                S.op(V, lambda e: e.reciprocal(out=hs[:, 4:8], in_=hs[:, 4:8]), reads=[b_hs], writes=[b_hs])
                for h in range(4):
                    sl = slice(h * 256, (h + 1) * 256)
                    S.op(V, lambda e: e.scalar_tensor_tensor(out=ya[:, sl], in0=ya[:, sl], scalar=hs[:, 4 + h:5 + h],
                                                             in1=mhg[:, sl], op0=ALU.mult, op1=ALU.mult),
                         reads=[b_ya, b_hs, b_mhg], writes=[b_ya])
                S.op(G, lambda e: e.tensor_tensor(out=ya[:], in0=ya[:], in1=so[:], op=ALU.mult), reads=[b_ya, b_so], writes=[b_ya])
                S.op(G, lambda e: e.tensor_tensor(out=ya[:], in0=ya[:], in1=gt[:, 0:D], op=ALU.mult), reads=[b_ya, b_gt], writes=[b_ya])
                S.op(V, lambda e: e.tensor_tensor(out=zb[:], in0=ya[:], in1=yb[:], op=ALU.add), reads=[b_ya, b_yb], writes=[b_zb])
                trp.next()
                S.tr([(trv[:, k * 128:(k + 1) * 128], zb[:, k * 128:(k + 1) * 128]) for k in range(8)], identb[:],
                     reads=[b_zb, b_identb], writes=[b_tr])
                S.op(A, lambda e: e.copy(out=zT[:].rearrange("p k t -> p (k t)"), in_=trv), reads=[b_tr], writes=[b_zT])
                trp.release_mine()
                for half in range(2):
                    bk, b_bk = pp.next()
                    sl = slice(half * 512, (half + 1) * 512)
                    wo_, b_wo = stream_next(12 + half)
                    S.mm([(bk[:], [(zT[:, kc, :], wo_[:, kc, :]) for kc in range(8)])], reads=[b_zT, b_wo], writes=[b_bk])
                    S.op(V, lambda e: e.tensor_tensor(out=xt_t[:, sl], in0=bk[:], in1=xt_t[:, sl], op=ALU.add),
                         reads=[b_bk, b_xt], writes=[b_xt])
                if stage == 1:
                    S.dma(G, y_d[c * 128:(c + 1) * 128, :], xt_t[:], reads=[b_xt], writes=[b_x1s[c]])
                else:
                    S.dma(G, x1s[c * 128:(c + 1) * 128, :], xt_t[:], reads=[b_xt], writes=[b_x1s[c]])

            for rnd in range(NIT + 3):
                if rnd >= 2 and rnd % 3 == 2 and (rnd - 2) // 3 < NSG:
                    stage_group((rnd - 2) // 3)
                fns = []
                if rnd < NIT:
                    fns.append((lambda i=rnd: P1a(i), 1))
                if 0 <= rnd - 1 < NIT:
                    fns.append((lambda i=rnd - 1: P1b(i), 1))
                if 0 <= rnd - 2 < NIT:
                    fns.append((lambda i=rnd - 2: P2(i), 1))
                if NPRE <= rnd - 3 < NIT:
                    fns.append((lambda i=rnd - 3: P3(i), 2))
                S.weave(fns)

        if stage == 1:
            S.wait_all(G, b_x1s)
            return nc
        S.barrier()

        pbc = ExitStack()
        with pbc:
            acc = pbc.enter_context(nc.sbuf_tensor("s_acc", [128, NCH, D], F32))
            b_acc = [Buf() for _ in range(NCH)]
            xn3B = pbc.enter_context(nc.sbuf_tensor("s_xn3B", [128, NCH, D], BF16))
            b_xn3B = [Buf() for _ in range(NCH)]
            mskA, b_mskA = sbt(pbc, "mskA", [128, NCH, NE], F32)
            posA, b_posA = sbt(pbc, "posA", [128, NCH, NE], F32)
            runcnt, b_runcnt = sbt(pbc, "runcnt", [128, NE], F32)
            desti, b_desti = sbt(pbc, "desti", [128, NCH, 2], I32)
            gate2, b_gate2 = sbt(pbc, "gate2", [128, NCH, 2], F32)
            idxWi, b_idxWi = sbt(pbc, "idxWi", [128, NBLK, 2], I32)
            trisb, b_trisb = sbt(pbc, "trisb", [128, 128], BF16)
            onesb, b_onesb = sbt(pbc, "onesb", [128, 128], BF16)
            S.op(V, lambda e: e.tensor_tensor(out=trisb[:], in0=trif[:], in1=identf[:], op=ALU.subtract),
                 reads=[b_trif, b_identf], writes=[b_trisb])
            S.op(V, lambda e: e.memset(onesb[:], 1.0), writes=[b_onesb])
            S.op(V, lambda e: e.memset(runcnt[:], 0.0), writes=[b_runcnt])
            Gt = pbc.enter_context(nc.sbuf_tensor("s_Gt", [128, NCH, NE], F32))
            b_Gt = [Buf() for _ in range(NCH)]
            gf, b_gf = sbt(pbc, "gf", [128, D], F32)
            S.dma(SP, gf[:], gf_d, writes=[b_gf])
            junk, b_junk = sbt(pbc, "junk2", [128, D], BF16)
            b_y = [Buf() for _ in range(NCH)]

            pb = ExitStack()
            with pb:
                Wxq, b_wxq = sbt(pb, "Wxq", [128, 8, D], BF16)
                S.dma(G, Wxq[:], w_xq_d.rearrange("(k p) n -> p k n", p=128), writes=[b_wxq])
                Wxo, b_wxo = sbt(pb, "Wxo", [128, 8, D], BF16)
                S.dma(G, Wxo[:], w_xo_d.rearrange("(k p) n -> p k n", p=128), writes=[b_wxo])
                Wkv, b_wkv = sbt(pb, "Wkv", [128, 8, 512], BF16)
                w_xkv_v = w_xkv_d.rearrange("(k p) n -> p k n", p=128)
                Wr, b_wr = sbt(pb, "Wr", [128, 8, 36], F32)
                S.dma(SP, Wr[:], w_r_d.rearrange("(k p) n -> p k n", p=128), writes=[b_wr])

                def cload2(name, shape, src):
                    t, b = sbt(pb, name, shape, F32)
                    S.dma(SP, t[:], src, writes=[b])
                    return t, b

                gx, b_gx = cload2("gx", [128, D], gx_d)
                gmoe, b_gmoe = cload2("gmoe", [128, D], gmem_d)
                gmem, b_gmem = gmoe, b_gmoe
                brt, b_brt = cload2("brt", [128, 36], br_d)

                xt = [sbt(pb, "bxt0", [128, D], F32)] * 2
                st = [sbt(pb, "bst%d" % i, [128, 4], F32) for i in range(2)]
                xn, b_xn = sbt(pb, "bxn", [128, D], BF16)
                xT, b_xT = sbt(pb, "bxT", [128, 8, 128], BF16)
                mnT, b_mnT = sbt(pb, "mnT", [128, 8, 256], BF16)
                KT, b_KT = sbt(pb, "KT", [128, 8, 256], BF16)
                Vm, b_Vm = sbt(pb, "Vm", [128, 2, D], BF16)
                pexp, b_pexp = sbt(pb, "pexp", [128, 4, 256], BF16)
                pT, b_pT = sbt(pb, "pT", [128, 8, 128], BF16)
                ss, b_ss = sbt(pb, "ss", [128, 16], F32)
                attn, b_attn = sbt(pb, "attn", [128, D], BF16)
                xn3f, b_xn3f = sbt(pb, "xn3f", [128, D], F32)
                zrow, b_zrow = sbt(pb, "zrow", [128, D], BF16)
                S.op(V, lambda e: e.memset(zrow[:], 0.0), writes=[b_zrow])
                xTf, b_xTf = sbt(pb, "xTf", [128, 8, 128], F32)
                lg, b_lg = sbt(pb, "lg", [128, 36], F32)
                rt, b_rt = sbt(pb, "rt", [128, 16], F32)
                oh, b_oh = sbt(pb, "oh", [128, 4], F32)
                ml, b_ml = sbt(pb, "ml", [128, NE], F32)
                ml2, b_ml2 = sbt(pb, "ml2", [128, NE], F32)
                msk, b_msk = sbt(pb, "msk", [128, NE], F32)
                mskb, b_mskb = sbt(pb, "mskb", [128, NE], BF16)

                pp = Pool(banks[0:2])
                ppX1 = Pool(banks[2:3])
                ppX2 = Pool(banks[3:5])
                bk_tr, b_tr = banks[5]
                bk_tr2, b_tr2 = banks[6]
                bk_tr3, b_tr3 = banks[7]
                trv = bk_tr[:].bitcast(BF16)
                trv2 = bk_tr2[:].bitcast(BF16)
                trv3 = bk_tr3[:].bitcast(BF16)

                for mc in range(2):
                    xt_t, b_xt = xt[mc]
                    st_t, b_stt = st[mc]
                    S.dma(SP, xt_t[:], mem_d[mc * 128:(mc + 1) * 128, :], writes=[b_xt])
                    rms_rstd(A, xt_t[:], b_xt, junk[:], b_junk, st_t, b_stt, float(D))
                    S.op(V, lambda e: e.scalar_tensor_tensor(out=xn[:], in0=xt_t[:], scalar=st_t[:, 2:3], in1=gmem[:],
                                                             op0=ALU.mult, op1=ALU.mult),
                         reads=[b_xt, b_stt, b_gmem], writes=[b_xn])
                    S.tr([(trv[:, k * 128:(k + 1) * 128], xn[:, k * 128:(k + 1) * 128]) for k in range(8)], identb[:],
                         reads=[b_xn, b_identb], writes=[b_tr])
                    S.op(A, lambda e: e.copy(out=mnT[:, :, mc * 128:(mc + 1) * 128],
                                             in_=trv.rearrange("p (k t) -> p k t", k=8)), reads=[b_tr], writes=[b_mnT])
                S.dma(SP, gmoe[:], gmoe_d, reads=[], writes=[b_gmoe])
                for grp4 in range(4):
                    S.dma(G, Wkv[:], w_xkv_v[:, :, grp4 * 512:(grp4 + 1) * 512], writes=[b_wkv])
                    if grp4 < 2:
                        for jj in range(2):
                            i0 = grp4 * 4 + jj * 2
                            bk, b_bk = pp.next()
                            S.mm([(bk[:, j * 256:(j + 1) * 256],
                                   [(Wkv[:, kc, (jj * 2 + j) * 128:(jj * 2 + j + 1) * 128], mnT[:, kc, :]) for kc in range(8)])
                                  for j in range(2)], reads=[b_wkv, b_mnT], writes=[b_bk])
                            S.op(A, lambda e: e.copy(out=KT[:, i0:i0 + 2, :].rearrange("p a b -> p (a b)"), in_=bk[:]),
                                 reads=[b_bk], writes=[b_KT])
                    else:
                        half = grp4 - 2
                        for mc in range(2):
                            bk, b_bk = pp.next()
                            S.mm([(bk[:], [(mnT[:, kc, mc * 128:(mc + 1) * 128], Wkv[:, kc, :]) for kc in range(8)])],
                                 reads=[b_wkv, b_mnT], writes=[b_bk])
                            S.op(A, lambda e: e.copy(out=Vm[:, mc, half * 512:(half + 1) * 512], in_=bk[:]),
                                 reads=[b_bk], writes=[b_Vm])

                for blk in range(NBLK * NT):
                    S.dma(G, xbuf[blk * 128:(blk + 1) * 128, :], zrow[:], reads=[b_zrow], writes=[Buf()])
                q2T_r = [sbt(pb, "q2Tr%d" % i, [128, 8, 128], BF16) for i in range(2)]
                aT_r = [sbt(pb, "aTr%d" % i, [128, 8, 128], BF16) for i in range(2)]
                stY = [sbt(pb, "stY%d" % i, [128, 4], F32) for i in range(2)]
                junkY, b_junkY = sbt(pb, "junkY", [128, D], BF16)

                def BX1(c):
                    st_t, b_stt = st[c % 2]
                    q2T, b_q2T = q2T_r[c % 2]
                    S.dma(SP, acc[:, c, :], x1s[c * 128:(c + 1) * 128, :], reads=[b_x1s[c]], writes=[b_acc[c]])
                    rms_rstd(A, acc[:, c, :], b_acc[c], junk[:], b_junk, st_t, b_stt, float(D))
                    S.op(V, lambda e: e.scalar_tensor_tensor(out=xn[:], in0=acc[:, c, :], scalar=st_t[:, 2:3], in1=gx[:],
                                                             op0=ALU.mult, op1=ALU.mult),
                         reads=[b_acc[c], b_stt, b_gx], writes=[b_xn])
                    S.tr([(trv[:, k * 128:(k + 1) * 128], xn[:, k * 128:(k + 1) * 128]) for k in range(8)], identb[:],
                         reads=[b_xn, b_identb], writes=[b_tr])
                    S.op(A, lambda e: e.copy(out=xT[:].rearrange("p k t -> p (k t)"), in_=trv), reads=[b_tr], writes=[b_xT])
                    for i0 in range(0, 8, 4):
                        bk, b_bk = ppX1.next()
                        S.mm([(bk[:, j * 128:(j + 1) * 128],
                               [(Wxq[:, kc, (i0 + j) * 128:(i0 + j + 1) * 128], xT[:, kc, :]) for kc in range(8)])
                              for j in range(4)], reads=[b_wxq, b_xT], writes=[b_bk])
                        S.op(A, lambda e: e.copy(out=q2T[:, i0:i0 + 4, :].rearrange("p a b -> p (a b)"), in_=bk[:]),
                             reads=[b_bk], writes=[b_q2T])

                def BX2(c):
                    q2T, b_q2T = q2T_r[c % 2]
                    aT, b_aT = aT_r[c % 2]
                    for hp in range(2):
                        bk, b_bk = ppX2.next()
                        S.mm([(bk[:, j * 256:(j + 1) * 256],
                               [(q2T[:, 2 * (2 * hp + j) + dc, :], KT[:, 2 * (2 * hp + j) + dc, :]) for dc in range(2)])
                              for j in range(2)], reads=[b_q2T, b_KT], writes=[b_bk])
                        S.op(V, lambda e: e.tensor_reduce(out=ss[:, 2 * hp:2 * hp + 2], in_=bk[:].rearrange("p (a b) -> p a b", a=2),
                                                          axis=AX.X, op=ALU.max), reads=[b_bk], writes=[b_ss])
                        S.op(V, lambda e: e.tensor_scalar(out=ss[:, 4 + 2 * hp:6 + 2 * hp], in0=ss[:, 2 * hp:2 * hp + 2],
                                                          scalar1=-0.0625, scalar2=None, op0=ALU.mult), reads=[b_ss], writes=[b_ss])
                        for j in range(2):
                            h = 2 * hp + j
                            S.op(A, lambda e: e.activation(out=pexp[:, h, :], in_=bk[:, j * 256:(j + 1) * 256], func=AF.Exp,
                                                           scale=0.0625, bias=ss[:, 4 + h:5 + h], accum_out=ss[:, 8 + h:9 + h]),
                                 reads=[b_bk, b_ss], writes=[b_pexp, b_ss])
                    S.op(V, lambda e: e.reciprocal(out=ss[:, 12:16], in_=ss[:, 8:12]), reads=[b_ss], writes=[b_ss])
                    S.tr([(trv2[:, (2 * h + mc) * 128:(2 * h + mc + 1) * 128], pexp[:, h, mc * 128:(mc + 1) * 128])
                          for h in range(4) for mc in range(2)], identb[:], reads=[b_pexp, b_identb], writes=[b_tr2])
                    S.op(A, lambda e: e.copy(out=pT[:].rearrange("p k t -> p (k t)"), in_=trv2), reads=[b_tr2], writes=[b_pT])
                    for hp in range(2):
                        bk, b_bk = ppX2.next()
                        S.mm([(bk[:, j * 256:(j + 1) * 256],
                               [(pT[:, 2 * (2 * hp + j) + mc, :], Vm[:, mc, (2 * hp + j) * 256:(2 * hp + j + 1) * 256]) for mc in range(2)])
                              for j in range(2)], reads=[b_pT, b_Vm], writes=[b_bk])
                        for j in range(2):
                            h = 2 * hp + j
                            S.op(A, lambda e: e.activation(out=attn[:, h * 256:(h + 1) * 256], in_=bk[:, j * 256:(j + 1) * 256],
                                                           func=AF.Copy, scale=ss[:, 12 + h:13 + h]),
                                 reads=[b_bk, b_ss], writes=[b_attn])
                    S.tr([(trv2[:, k * 128:(k + 1) * 128], attn[:, k * 128:(k + 1) * 128]) for k in range(8)], identb[:],
                         reads=[b_attn, b_identb], writes=[b_tr2])
                    S.op(A, lambda e: e.copy(out=aT[:].rearrange("p k t -> p (k t)"), in_=trv2), reads=[b_tr2], writes=[b_aT])

                def BY(c):
                    st_t, b_stt = stY[c % 2]
                    aT, b_aT = aT_r[c % 2]
                    for half in range(2):
                        bk, b_bk = pp.next()
                        sl = slice(half * 512, (half + 1) * 512)
                        S.mm([(bk[:], [(aT[:, kc, :], Wxo[:, kc, sl]) for kc in range(8)])], reads=[b_aT, b_wxo], writes=[b_bk])
                        S.op(V, lambda e: e.tensor_tensor(out=acc[:, c, sl], in0=bk[:], in1=acc[:, c, sl], op=ALU.add),
                             reads=[b_bk, b_acc[c]], writes=[b_acc[c]])
                    if stage == 2:
                        S.dma(G, y_d[c * 128:(c + 1) * 128, :], acc[:, c, :], reads=[b_acc[c]], writes=[b_y[c]])
                        return
                    rms_rstd(A, acc[:, c, :], b_acc[c], junkY[:], b_junkY, st_t, b_stt, float(D))
                    S.op(V, lambda e: e.scalar_tensor_tensor(out=xn3f[:], in0=acc[:, c, :], scalar=st_t[:, 2:3], in1=gmoe[:],
                                                             op0=ALU.mult, op1=ALU.mult),
                         reads=[b_acc[c], b_stt, b_gmoe], writes=[b_xn3f])
                    S.op(G, lambda e: e.tensor_copy(out=xn3B[:, c, :], in_=xn3f[:]), reads=[b_xn3f], writes=[b_xn3B[c]])
                    for half in range(2):
                        bk, b_bk = pp.next()
                        S.tr([(bk[:, k * 128:(k + 1) * 128], xn3f[:, (half * 4 + k) * 128:(half * 4 + k + 1) * 128]) for k in range(4)],
                             identf[:], reads=[b_xn3f, b_identf], writes=[b_bk])
                        S.op(V, lambda e: e.tensor_copy(out=xTf[:, half * 4:half * 4 + 4, :].rearrange("p a b -> p (a b)"), in_=bk[:]),
                             reads=[b_bk], writes=[b_xTf])
                    bk, b_bk = pp.next()
                    S.mm([(bk[:, 0:36], [(xTf[:, kc, :], Wr[:, kc, :]) for kc in range(8)])], reads=[b_xTf, b_wr], writes=[b_bk])
                    S.op(V, lambda e: e.tensor_tensor(out=lg[:], in0=bk[:, 0:36], in1=brt[:], op=ALU.add),
                         reads=[b_bk, b_brt], writes=[b_lg])
                    S.op(V, lambda e: e.tensor_reduce(out=rt[:, 0:1], in_=lg[:, 0:4], axis=AX.X, op=ALU.max), reads=[b_lg], writes=[b_rt])
                    S.op(V, lambda e: e.tensor_scalar(out=oh[:], in0=lg[:, 0:4], scalar1=rt[:, 0:1], scalar2=None, op0=ALU.is_ge),
                         reads=[b_lg, b_rt], writes=[b_oh])
                    S.op(V, lambda e: e.tensor_scalar(out=rt[:, 1:2], in0=rt[:, 0:1], scalar1=-1.0, scalar2=None, op0=ALU.mult),
                         reads=[b_rt], writes=[b_rt])
                    S.op(A, lambda e: e.activation(out=junk[:, 0:4], in_=lg[:, 0:4], func=AF.Exp, bias=rt[:, 1:2], accum_out=rt[:, 2:3]),
                         reads=[b_lg, b_rt], writes=[b_junk, b_rt])
                    S.op(V, lambda e: e.tensor_scalar(out=oh[:], in0=oh[:], scalar1=1e30, scalar2=-1e30, op0=ALU.mult, op1=ALU.add),
                         reads=[b_oh], writes=[b_oh])
                    S.op(V, lambda e: e.tensor_tensor(out=ml[:].rearrange("p (g j) -> p g j", g=4),
                                                      in0=lg[:, 4:36].rearrange("p (g j) -> p g j", g=4),
                                                      in1=oh[:].unsqueeze(2).to_broadcast([128, 4, 8]), op=ALU.add),
                         reads=[b_lg, b_oh], writes=[b_ml])
                    S.op(V, lambda e: e.tensor_reduce(out=rt[:, 3:4], in_=ml[:], axis=AX.X, op=ALU.max), reads=[b_ml], writes=[b_rt])
                    S.op(V, lambda e: e.tensor_scalar(out=ml2[:], in0=ml[:], scalar1=rt[:, 3:4], scalar2=-1e30, op0=ALU.is_ge, op1=ALU.mult),
                         reads=[b_ml, b_rt], writes=[b_ml2])
                    S.op(V, lambda e: e.tensor_tensor(out=ml2[:], in0=ml2[:], in1=ml[:], op=ALU.add), reads=[b_ml2, b_ml], writes=[b_ml2])
                    S.op(V, lambda e: e.tensor_reduce(out=rt[:, 4:5], in_=ml2[:], axis=AX.X, op=ALU.max), reads=[b_ml2], writes=[b_rt])
                    S.op(V, lambda e: e.tensor_scalar(out=msk[:], in0=ml[:], scalar1=rt[:, 4:5], scalar2=None, op0=ALU.is_ge),
                         reads=[b_ml, b_rt], writes=[b_msk])
                    S.op(V, lambda e: e.tensor_scalar(out=rt[:, 5:6], in0=rt[:, 3:4], scalar1=-1.0, scalar2=None, op0=ALU.mult),
                         reads=[b_rt], writes=[b_rt])
                    S.op(V, lambda e: e.tensor_scalar(out=ml2[:], in0=ml[:], scalar1=rt[:, 5:6], scalar2=-80.0, op0=ALU.add, op1=ALU.max),
                         reads=[b_ml, b_rt], writes=[b_ml2])
                    S.op(A, lambda e: e.activation(out=ml2[:], in_=ml2[:], func=AF.Exp), reads=[b_ml2], writes=[b_ml2])
                    S.op(V, lambda e: e.tensor_tensor(out=ml2[:], in0=ml2[:], in1=msk[:], op=ALU.mult), reads=[b_ml2, b_msk], writes=[b_ml2])
                    S.op(V, lambda e: e.tensor_reduce(out=rt[:, 6:7], in_=ml2[:], axis=AX.X, op=ALU.add), reads=[b_ml2], writes=[b_rt])
                    S.op(V, lambda e: e.tensor_tensor(out=rt[:, 7:8], in0=rt[:, 6:7], in1=rt[:, 2:3], op=ALU.mult), reads=[b_rt], writes=[b_rt])
                    S.op(V, lambda e: e.reciprocal(out=rt[:, 8:9], in_=rt[:, 7:8]), reads=[b_rt], writes=[b_rt])
                    S.op(V, lambda e: e.tensor_scalar(out=Gt[:, c, :], in0=ml2[:], scalar1=rt[:, 8:9], scalar2=None, op0=ALU.mult),
                         reads=[b_ml2, b_rt], writes=[b_Gt[c]])
                    S.op(V, lambda e: e.tensor_copy(out=mskA[:, c, :], in_=msk[:]), reads=[b_msk], writes=[b_mskA])
                    S.op(V, lambda e: e.tensor_copy(out=mskb[:], in_=msk[:]), reads=[b_msk], writes=[b_mskb])
                    bk, b_bk = pp.next()
                    S.mm([(bk[:, 0:32], [(trisb[:], mskb[:])]), (bk[:, 32:64], [(onesb[:], mskb[:])])],
                         reads=[b_trisb, b_onesb, b_mskb], writes=[b_bk])
                    S.op(V, lambda e: e.tensor_tensor(out=posA[:, c, :], in0=bk[:, 0:32], in1=runcnt[:], op=ALU.add),
                         reads=[b_bk, b_runcnt], writes=[b_posA])
                    S.op(V, lambda e: e.tensor_tensor(out=runcnt[:], in0=bk[:, 32:64], in1=runcnt[:], op=ALU.add),
                         reads=[b_bk, b_runcnt], writes=[b_runcnt])


                for rnd in range(NCH + 2):
                    fns = []
                    if rnd < NCH:
                        fns.append((lambda i=rnd: BX1(i), 1))
                    if 0 <= rnd - 1 < NCH:
                        fns.append((lambda i=rnd - 1: BX2(i), 1))
                    if 0 <= rnd - 2 < NCH:
                        fns.append((lambda i=rnd - 2: BY(i), 2))
                    S.weave(fns)

            if stage == 2:
                S.wait_all(G, b_y)
                return nc
            S.barrier()

            pd = ExitStack()
            with pd:
                def ld(name, shape, src):
                    t, bb = sbt(pd, name, shape, F32)
                    S.dma(SP, t[:], src, writes=[bb])
                    return t, bb
                thr, b_thr = ld("thr", [128, 16], thr_d)
                biota, b_biota = ld("biota", [128, NBLK], biota_d[:, 0:NBLK])
                pidx, b_pidx = ld("pidx", [128, 1], pidx_d)
                cmp1, b_cmp1 = sbt(pd, "cmp1", [128, NE, 16], F32)
                cmp2, b_cmp2 = sbt(pd, "cmp2", [128, NBLK, NE], F32)
                blocks, b_blocks = sbt(pd, "blocks", [128, NE], F32)
                pendb, b_pendb = sbt(pd, "pendb", [128, NE], F32)
                pstart, b_pstart = sbt(pd, "pstart", [128, NE], F32)
                ones32, b_ones32 = sbt(pd, "ones32", [128, NE], F32)
                ebf, b_ebf = sbt(pd, "ebf", [128, NBLK], F32)
                inv, b_inv = sbt(pd, "inv", [128, NBLK], F32)
                idxWf, b_idxWf = sbt(pd, "idxWf", [128, NBLK, 2], F32)
                incl, b_incl = sbt(pd, "incl", [128, NCH, NE], F32)
                m0, b_m0 = sbt(pd, "m0", [128, NCH, NE], F32)
                m1, b_m1 = sbt(pd, "m1", [128, NCH, NE], F32)
                dfull, b_dfull = sbt(pd, "dfull", [128, NCH, NE], F32)
                tmpd, b_tmpd = sbt(pd, "tmpd", [128, NCH, NE], F32)
                destf, b_destf = sbt(pd, "destf", [128, NCH, 2], F32)
                S.op(V, lambda e: e.tensor_tensor(out=cmp1[:], in0=runcnt[:].unsqueeze(2).to_broadcast([128, NE, 16]),
                                                  in1=thr[:].unsqueeze(1).to_broadcast([128, NE, 16]), op=ALU.is_gt),
                     reads=[b_runcnt, b_thr], writes=[b_cmp1])
                S.op(V, lambda e: e.tensor_reduce(out=blocks[:], in_=cmp1[:], axis=AX.X, op=ALU.add), reads=[b_cmp1], writes=[b_blocks])
                S.op(V, lambda e: e.memset(ones32[:], 1.0), writes=[b_ones32])
                S.op(V, lambda e: e.tensor_tensor_scan(out=pendb[:], data0=ones32[:], data1=blocks[:], initial=0.0,
                                                       op0=ALU.mult, op1=ALU.add),
                     reads=[b_ones32, b_blocks], writes=[b_pendb])
                S.op(V, lambda e: e.tensor_tensor(out=pstart[:], in0=pendb[:], in1=blocks[:], op=ALU.subtract),
                     reads=[b_pendb, b_blocks], writes=[b_pstart])
                S.op(V, lambda e: e.tensor_scalar(out=pstart[:], in0=pstart[:], scalar1=float(BS), scalar2=None, op0=ALU.mult),
                     reads=[b_pstart], writes=[b_pstart])
                S.op(V, lambda e: e.tensor_tensor(out=cmp2[:], in0=pendb[:].unsqueeze(1).to_broadcast([128, NBLK, NE]),
                                                  in1=biota[:].unsqueeze(2).to_broadcast([128, NBLK, NE]), op=ALU.is_le),
                     reads=[b_pendb, b_biota], writes=[b_cmp2])
                S.op(V, lambda e: e.tensor_reduce(out=ebf[:], in_=cmp2[:], axis=AX.X, op=ALU.add), reads=[b_cmp2], writes=[b_ebf])
                S.op(V, lambda e: e.tensor_scalar(out=ebf[:], in0=ebf[:], scalar1=float(NE - 1), scalar2=256.0, op0=ALU.min, op1=ALU.mult),
                     reads=[b_ebf], writes=[b_ebf])
                S.op(V, lambda e: e.tensor_scalar(out=inv[:], in0=biota[:], scalar1=pendb[:, NE - 1:NE], scalar2=1.0e6,
                                                  op0=ALU.is_ge, op1=ALU.mult), reads=[b_biota, b_pendb], writes=[b_inv])
                S.op(V, lambda e: e.tensor_tensor(out=ebf[:], in0=ebf[:], in1=inv[:], op=ALU.add), reads=[b_ebf, b_inv], writes=[b_ebf])
                S.op(V, lambda e: e.tensor_scalar(out=idxWf[:, :, 0], in0=ebf[:], scalar1=pidx[:, 0:1], scalar2=None, op0=ALU.add),
                     reads=[b_ebf, b_pidx], writes=[b_idxWf])
                S.op(V, lambda e: e.tensor_scalar(out=idxWf[:, :, 1], in0=idxWf[:, :, 0], scalar1=128.0, scalar2=None, op0=ALU.add),
                     reads=[b_idxWf], writes=[b_idxWf])
                S.op(V, lambda e: e.tensor_copy(out=idxWi[:].rearrange("p a b -> p (a b)"), in_=idxWf[:].rearrange("p a b -> p (a b)")),
                     reads=[b_idxWf], writes=[b_idxWi])
                for c in range(NCH):
                    S.op(V, lambda e: e.tensor_tensor_scan(out=incl[:, c, :], data0=ones32[:], data1=mskA[:, c, :], initial=0.0,
                                                           op0=ALU.mult, op1=ALU.add),
                         reads=[b_ones32, b_mskA], writes=[b_incl])
                fl = lambda t: t[:].rearrange("p a b -> p (a b)")
                S.op(V, lambda e: e.tensor_scalar(out=fl(m0), in0=fl(incl), scalar1=1.0, scalar2=None, op0=ALU.is_equal),
                     reads=[b_incl], writes=[b_m0])
                S.op(V, lambda e: e.tensor_tensor(out=fl(m0), in0=fl(m0), in1=fl(mskA), op=ALU.mult), reads=[b_m0, b_mskA], writes=[b_m0])
                S.op(V, lambda e: e.tensor_scalar(out=fl(m1), in0=fl(incl), scalar1=2.0, scalar2=None, op0=ALU.is_equal),
                     reads=[b_incl], writes=[b_m1])
                S.op(V, lambda e: e.tensor_tensor(out=fl(m1), in0=fl(m1), in1=fl(mskA), op=ALU.mult), reads=[b_m1, b_mskA], writes=[b_m1])
                S.op(V, lambda e: e.tensor_tensor(out=dfull[:], in0=posA[:], in1=pstart[:].unsqueeze(1).to_broadcast([128, NCH, NE]), op=ALU.add),
                     reads=[b_posA, b_pstart], writes=[b_dfull])
                for k, mk, b_mk in ((0, m0, b_m0), (1, m1, b_m1)):
                    S.op(V, lambda e: e.tensor_tensor(out=fl(tmpd), in0=fl(mk), in1=fl(dfull), op=ALU.mult), reads=[b_mk, b_dfull], writes=[b_tmpd])
                    S.op(V, lambda e: e.tensor_reduce(out=destf[:, :, k], in_=tmpd[:], axis=AX.X, op=ALU.add), reads=[b_tmpd], writes=[b_destf])
                    S.op(V, lambda e: e.tensor_tensor(out=fl(tmpd), in0=fl(mk), in1=Gt[:].rearrange("p a b -> p (a b)"), op=ALU.mult),
                         reads=[b_mk] + b_Gt, writes=[b_tmpd])
                    S.op(V, lambda e: e.tensor_reduce(out=gate2[:, :, k], in_=tmpd[:], axis=AX.X, op=ALU.add), reads=[b_tmpd], writes=[b_gate2])
                S.op(V, lambda e: e.tensor_copy(out=desti[:].rearrange("p a b -> p (a b)"), in_=destf[:].rearrange("p a b -> p (a b)")),
                     reads=[b_destf], writes=[b_desti])
                b_scat = []
                for c in range(NCH):
                    for k in range(2):
                        bb = Buf()
                        S.idma(G, reads=[b_xn3B[c], b_desti], writes=[bb], out=xbuf[:, :],
                               out_offset=bass.IndirectOffsetOnAxis(ap=desti[:, c, k:k + 1], axis=0),
                               in_=xn3B[:, c, :], in_offset=None)
                        b_scat.append(bb)
            S.barrier()

            pc = ExitStack()
            with pc:
                w1b = [sbt(pc, "w1b%d" % i, [128, 8, 512], BF16) for i in range(2)]
                w3b = [sbt(pc, "w3b%d" % i, [128, 8, 512], BF16) for i in range(2)]
                w2b = [sbt(pc, "w2b%d" % i, [128, 4, D], BF16) for i in range(2)]
                xb_r = [sbt(pc, "xb%d" % i, [128, NT, D], BF16) for i in range(2)]
                xbT_r = [sbt(pc, "xbT%d" % i, [128, 8, BS], BF16) for i in range(2)]
                hgT = [sbt(pc, "hgT%d" % i, [128, 4, BS], BF16) for i in range(2)]
                sl_t = [sbt(pc, "silu%d" % i, [128, 512], F32) for i in range(2)]
                ob_r = [sbt(pc, "ob%d" % i, [128, NT, D], F32) for i in range(1)]
                rg = [[sbt(pc, "rg%d_%d" % (i, k), [128, D], F32) for k in range(2)] for i in range(1)]
                yo = [sbt(pc, "yo%d" % i, [128, D], F32) for i in range(2)]
                st3, b_st3 = sbt(pc, "st3c", [128, 4], F32)
                pp = Pool(banks[0:7])
                bk_tr, b_tr = banks[7]
                trv = bk_tr[:].bitcast(BF16)
                b_ost = [Buf() for _ in range(NBLK)]

                bc_reg = G.eng.to_reg(NE * 256 - 1)

                def load_w(bi):
                    p = bi % 2
                    for j in range(2):
                        io = bass.IndirectOffsetOnAxis(ap=idxWi[:, bi, j:j + 1], axis=0)
                        S.idma(G, reads=[b_idxWi], writes=[w1b[p][1]], out=w1b[p][0][:, 4 * j:4 * j + 4, :].rearrange("p a b -> p (a b)"),
                               out_offset=None, in_=w1_d[:, :], in_offset=io, bounds_check=bc_reg, oob_is_err=False)
                        S.idma(G, reads=[b_idxWi], writes=[w3b[p][1]], out=w3b[p][0][:, 4 * j:4 * j + 4, :].rearrange("p a b -> p (a b)"),
                               out_offset=None, in_=w3_d[:, :], in_offset=io, bounds_check=bc_reg, oob_is_err=False)
                        S.idma(G, reads=[b_idxWi], writes=[w2b[p][1]], out=w2b[p][0][:, 2 * j:2 * j + 2, :].rearrange("p a b -> p (a b)"),
                               out_offset=None, in_=w2_d[:, :], in_offset=io, bounds_check=bc_reg, oob_is_err=False)

                def load_x(bi):
                    t, bb = xb_r[bi % 2]
                    S.dma(SP, t[:], xbuf[bi * BS:(bi + 1) * BS, :].rearrange("(t p) d -> p t d", p=128), reads=b_scat, writes=[bb])

                load_w(0)
                load_x(0)
                for bi in range(NBLK):
                    p = bi % 2
                    if bi + 1 < NBLK:
                        load_w(bi + 1)
                        load_x(bi + 1)
                    w1t, b_w1 = w1b[p]
                    w3t, b_w3 = w3b[p]
                    w2t, b_w2 = w2b[p]
                    xb_t, b_xb = xb_r[p]
                    xbT, b_xbT = xbT_r[p]
                    hg_t, b_hg = hgT[p]
                    ob, b_ob = ob_r[0]
                    for t in range(NT):
                        S.tr([(trv[:, k * 128:(k + 1) * 128], xb_t[:, t, k * 128:(k + 1) * 128]) for k in range(8)], identb[:],
                             reads=[b_xb, b_identb], writes=[b_tr])
                        S.op(A, lambda e_: e_.copy(out=xbT[:, :, t * 128:(t + 1) * 128], in_=trv.rearrange("p (k s) -> p k s", k=8)),
                             reads=[b_tr], writes=[b_xbT])
                    for fp_ in range(2):
                        bk1, b_bk1 = pp.next()
                        bk3, b_bk3 = pp.next()
                        S.mm([(bk1[:, q_ * BS:(q_ + 1) * BS],
                               [(w1t[:, kc, (2 * fp_ + q_) * 128:(2 * fp_ + q_ + 1) * 128], xbT[:, kc, :]) for kc in range(8)])
                              for q_ in range(2)], reads=[b_w1, b_xbT], writes=[b_bk1])
                        S.mm([(bk3[:, q_ * BS:(q_ + 1) * BS],
                               [(w3t[:, kc, (2 * fp_ + q_) * 128:(2 * fp_ + q_ + 1) * 128], xbT[:, kc, :]) for kc in range(8)])
                              for q_ in range(2)], reads=[b_w3, b_xbT], writes=[b_bk3])
                        s_t, b_sl = sl_t[fp_]
                        S.op(A, lambda e_: e_.activation(out=s_t[:], in_=bk1[:], func=AF.Silu), reads=[b_bk1], writes=[b_sl])
                        S.op(V, lambda e_: e_.tensor_tensor(out=hg_t[:, 2 * fp_:2 * fp_ + 2, :].rearrange("p a b -> p (a b)"), in0=bk3[:], in1=s_t[:],
                                                            op=ALU.mult), reads=[b_bk3, b_sl], writes=[b_hg])
                    for t in range(NT):
                        for half in range(2):
                            bk, b_bk = pp.next()
                            sl = slice(half * 512, (half + 1) * 512)
                            S.mm([(bk[:], [(hg_t[:, fc, t * 128:(t + 1) * 128], w2t[:, fc, sl]) for fc in range(4)])],
                                 reads=[b_hg, b_w2], writes=[b_bk])
                            if half == 0:
                                S.op(A, lambda e_: e_.copy(out=ob[:, t, sl], in_=bk[:]), reads=[b_bk], writes=[b_ob])
                            else:
                                S.op(V, lambda e_: e_.tensor_copy(out=ob[:, t, sl], in_=bk[:]), reads=[b_bk], writes=[b_ob])
                    S.dma(SP, obuf[bi * BS:(bi + 1) * BS, :].rearrange("(t p) d -> p t d", p=128), ob[:], reads=[b_ob], writes=[b_ost[bi]])
                for c in range(NCH):
                    for k in range(2):
                        r_t, b_r = rg[0][k]
                        S.idma(G, reads=b_ost + [b_desti], writes=[b_r], out=r_t[:, :], out_offset=None, in_=obuf[:, :],
                               in_offset=bass.IndirectOffsetOnAxis(ap=desti[:, c, k:k + 1], axis=0))
                        S.op(V, lambda e_: e_.scalar_tensor_tensor(out=acc[:, c, :], in0=r_t[:], scalar=gate2[:, c, k:k + 1],
                                                                   in1=acc[:, c, :], op0=ALU.mult, op1=ALU.add),
                             reads=[b_r, b_gate2, b_acc[c]], writes=[b_acc[c]])
                    yo_t, b_yo = yo[c % 2]
                    rms_rstd(A, acc[:, c, :], b_acc[c], junk[:], b_junk, st3, b_st3, float(D))
                    S.op(V, lambda e_: e_.scalar_tensor_tensor(out=yo_t[:], in0=acc[:, c, :], scalar=st3[:, 2:3], in1=gf[:],
                                                               op0=ALU.mult, op1=ALU.mult),
                         reads=[b_acc[c], b_st3, b_gf], writes=[b_yo])
                    S.dma(SP, y_d[c * 128:(c + 1) * 128, :], yo_t[:], reads=[b_yo], writes=[b_y[c]])
                S.wait_all(SP, b_y)
    return nc


def make_in_maps(inputs):
    f = lambda a: np.ascontiguousarray(a, dtype=np.float32)
    x = f(inputs["x"])
    mem = f(inputs["mem"])

    def bt(v, n=128):
        v = f(v).reshape(1, -1)
        return np.ascontiguousarray(np.broadcast_to(v, (n, v.shape[1])))

    b_if = f(inputs["b_if"][0])
    conv_w = f(inputs["conv_w"][0])
    conv_b = f(inputs["conv_b"][0])
    w_s = f(inputs["w_s"][0])
    b_s = f(inputs["b_s"][0])
    shared = {
        "w_in": f(inputs["w_in"][0]),
        "w_out": f(inputs["w_out"][0]),
        "w_xq": f(inputs["w_xq"][0]),
        "w_xkv": f(inputs["w_xkv"][0]),
        "w_xo": f(inputs["w_xo"][0]),
        "w_r": f(np.concatenate([inputs["w_rg"][0], inputs["w_re"][0]], axis=1)),
        "w1r": f(f(inputs["w1"][0]).reshape(NE, 2, 4, 128, 512).transpose(0, 1, 3, 2, 4).reshape(NE * 256, 2048)),
        "w3r": f(f(inputs["w3"][0]).reshape(NE, 2, 4, 128, 512).transpose(0, 1, 3, 2, 4).reshape(NE * 256, 2048)),
        "w2r": f(f(inputs["w2"][0]).reshape(NE, 2, 2, 128, 1024).transpose(0, 1, 3, 2, 4).reshape(NE * 256, 2048)),
        "thr16": bt(np.arange(16, dtype=np.float32) * 256.0),
        "biota": bt(np.arange(64, dtype=np.float32)),
        "pidx": np.arange(128, dtype=np.float32).reshape(128, 1),
        "ident": np.eye(128, dtype=np.float32),
        "tri": np.triu(np.ones((128, 128), dtype=np.float32)),
        "gmix_t": bt(inputs["norm_mix_g"][0]),
        "gx_t": bt(inputs["norm_x_g"][0]),
        "gmem_t": bt(inputs["norm_mem_g"][0]),
        "gmoe_t": bt(inputs["norm_moe_g"][0]),
        "gf_t": bt(inputs["norm_f_g"]),
        "mhg_t": bt(inputs["mh_norm_g"][0]),
        "sgug_t": bt(inputs["sgu_norm_g"][0]),
        "bs_t": f(np.repeat(b_s.T[:, :, None], 128, axis=2).reshape(128, 1024)),
        "bgate_t": bt(inputs["b_gate"][0]),
        "bif_t": bt(b_if),
        "br_t": bt(np.concatenate([inputs["b_rg"][0], inputs["b_re"][0]])),
        "convw_t": f(conv_w.reshape(4, 16, 128).transpose(2, 1, 0)),
        "convb_t": f(conv_b.reshape(16, 128).T),
        "wsT": f(w_s.transpose(2, 0, 1)),
    }
    maps = []
    for c in range(NCORES):
        b, j = divmod(c, 4)
        npad = NPRE - NCH * j
        xs = np.zeros((NIT * 128, D), dtype=np.float32)
        xs[npad * 128:] = x[b, 0:(j + 1) * NCH * 128]
        pm = np.zeros((128, NIT), dtype=np.float32)
        pm[:, npad:] = 1.0
        m = dict(shared)
        m["xseq"] = xs
        m["pmask"] = pm
        m["mem_b"] = mem[b]
        maps.append(m)
    return maps


def assemble(res):
    out = np.zeros((2, 8192, D), dtype=np.float32)
    for c in range(NCORES):
        b, j = divmod(c, 4)
        out[b, j * NCH * 128:(j + 1) * NCH * 128] = res.results[c]["y"]
    return out


def build2(stage=3):
    rec = []
    build(stage, None, rec)
    return build(stage, rec)


def kernel(**inputs):
    nc = build2(3)
    maps = make_in_maps(inputs)
    res = run_bass_kernel_spmd(nc, maps, core_ids=list(range(NCORES)))
    return assemble(res)
```
